# Optimizing a Trainium2 kernel written in Bass

```python
import math
import jax, jax.numpy as jnp
from jax import lax
import numpy as np

D_MODEL = 1024
BATCH = 2
SEQ = 8192
DEPTH = 4

MEM_LEN = 256
N_HEADS = 8
HEAD_DIM = 64
KV_LATENT = 128
IDX_HEADS = 8
IDX_DIM = 32
INDEX_TOPK = 256
Q_BLOCK = 128
CONV_WIDTH = 512
CONV_K = 3
SSM_WIDTH = 512
SSM_GROUP = 16
SSM_GROUPS = SSM_WIDTH // SSM_GROUP
SSM_STATE = 64
MEM_HEADS = 4
MEM_HEAD_DIM = 128
N_BRANCH = 4
BRANCH_WIDTH = 512
N_EXPERTS = 32
TOP_K = 4
D_EXPERT = 1024
SWIGLU_LIMIT = 7.0
SWIGLU_ALPHA = 1.702
EXPERT_BLOCK = 256
LN_EPS = 1e-5
DEEPNORM_ALPHA = (2 * DEPTH) ** 0.25
DEEPNORM_BETA = (8 * DEPTH) ** -0.25

SPLITS = (N_HEADS * HEAD_DIM, KV_LATENT, IDX_HEADS * IDX_DIM, IDX_DIM, IDX_HEADS,
          CONV_WIDTH, CONV_WIDTH, CONV_WIDTH, SSM_WIDTH, MEM_HEADS * MEM_HEAD_DIM, N_BRANCH * D_MODEL)
D_IN = sum(SPLITS)

kernel_name = 'hybrid_dsa_conv_s5_mem_moe_deepnorm'


def layer_norm(x, g, b):
    xf = x.astype(jnp.float32)
    mu = jnp.mean(xf, axis=-1, keepdims=True)
    var = jnp.mean(jnp.square(xf - mu), axis=-1, keepdims=True)
    return ((xf - mu) * lax.rsqrt(var + LN_EPS) * g.astype(jnp.float32) + b.astype(jnp.float32)).astype(x.dtype)


def rms_norm(x, g):
    xf = x.astype(jnp.float32)
    return (xf * lax.rsqrt(jnp.mean(jnp.square(xf), axis=-1, keepdims=True) + LN_EPS) * g.astype(jnp.float32)).astype(x.dtype)


def dsa_attention(q, ckv, q_idx, k_idx, w_idx, w_uk, w_uv):
    bsz, seq = ckv.shape[:2]
    topk = min(INDEX_TOPK, seq // 4)
    n_blk = seq // Q_BLOCK
    q_lat = jnp.einsum('bthd,chd->bthc', q, w_uk) * (HEAD_DIM ** -0.5)

    def to_blocks(a):
        return jnp.moveaxis(a.reshape((bsz, n_blk, Q_BLOCK) + a.shape[2:]), 1, 0)

    k_pos = jnp.arange(seq)
    gather = jax.vmap(lambda table, idx: table[idx])

    def block(args):
        i, qb, qib, wb = args
        q_pos = i * Q_BLOCK + jnp.arange(Q_BLOCK)
        rel = jax.nn.relu(jnp.einsum('bqhd,bsd->bqhs', qib, k_idx))
        score = jnp.einsum('bqhs,bqh->bqs', rel, wb).astype(jnp.float32)
        causal = k_pos[None, :] <= q_pos[:, None]
        score = jnp.where(causal[None], score, -jnp.inf)
        _, sel = lax.top_k(score, topk)
        valid = sel <= q_pos[None, :, None]
        kv = gather(ckv, sel)
        logits = jnp.einsum('bqhc,bqkc->bqhk', qb, kv).astype(jnp.float32)
        logits = jnp.where(valid[:, :, None, :], logits, -jnp.inf)
        p = jax.nn.softmax(logits, axis=-1).astype(kv.dtype)
        return jnp.einsum('bqhk,bqkc->bqhc', p, kv)

    o_lat = lax.map(block, (jnp.arange(n_blk), to_blocks(q_lat), to_blocks(q_idx), to_blocks(w_idx)))
    o_lat = jnp.moveaxis(o_lat, 0, 1).reshape(bsz, seq, N_HEADS, KV_LATENT)
    return jnp.einsum('bthc,chd->bthd', o_lat, w_uv).reshape(bsz, seq, N_HEADS * HEAD_DIM)


def short_conv(u, gate_b, gate_c, conv_w, conv_b):
    v = gate_c * u
    y = lax.conv_general_dilated(v, conv_w[:, None, :], window_strides=(1,), padding=[(CONV_K - 1, 0)],
                                 dimension_numbers=('NWC', 'WIO', 'NWC'), feature_group_count=CONV_WIDTH)
    return gate_b * (y + conv_b)


def s5_mixer(u, lam_re, lam_im, b_re, b_im, c_re, c_im, d_skip, log_dt, w_glu, b_glu):
    f32 = jnp.float32
    bsz, seq, _ = u.shape
    uf = u.astype(f32).reshape(bsz, seq, SSM_GROUPS, SSM_GROUP)
    lam = lax.complex(lam_re.astype(f32), lam_im.astype(f32))
    dt = jnp.exp(log_dt.astype(f32))[:, None]
    a_bar = jnp.exp(lam * dt)
    b_bar = ((a_bar - 1.0) / lam)[:, :, None] * lax.complex(b_re.astype(f32), b_im.astype(f32))
    bu = lax.complex(jnp.einsum('gpn,btgn->btgp', jnp.real(b_bar), uf),
                     jnp.einsum('gpn,btgn->btgp', jnp.imag(b_bar), uf))
    a = jnp.broadcast_to(a_bar, bu.shape)

    def combine(left, right):
        a_l, b_l = left
        a_r, b_r = right
        return a_r * a_l, a_r * b_l + b_r

    _, h = lax.associative_scan(combine, (a, bu), axis=1)
    y = (jnp.einsum('gnp,btgp->btgn', c_re.astype(f32), jnp.real(h))
         - jnp.einsum('gnp,btgp->btgn', c_im.astype(f32), jnp.imag(h))
         + d_skip.astype(f32) * uf)
    y = y.reshape(bsz, seq, SSM_WIDTH).astype(u.dtype)
    z = jax.nn.gelu(y)
    return z * jax.nn.sigmoid(z @ w_glu + b_glu)


def memory_attention(q, mem, w_mem_kv):
    bsz, seq = q.shape[:2]
    mlen = mem.shape[1]
    kv = (mem @ w_mem_kv).reshape(bsz, mlen, 2, MEM_HEADS, MEM_HEAD_DIM)
    k, v = kv[:, :, 0], kv[:, :, 1]
    logits = jnp.einsum('bthd,bmhd->bhtm', q, k).astype(jnp.float32) * (MEM_HEAD_DIM ** -0.5)
    p = jax.nn.softmax(logits, axis=-1).astype(v.dtype)
    return jnp.einsum('bhtm,bmhd->bthd', p, v).reshape(bsz, seq, MEM_HEADS * MEM_HEAD_DIM)


def moe(x, w_router, b_router, w_up, b_up, w_down, b_down):
    bsz, seq, d = x.shape
    n_tok = bsz * seq
    xf = x.reshape(n_tok, d)
    logits = (xf @ w_router + b_router).astype(jnp.float32)
    top_v, top_e = lax.top_k(logits, TOP_K)
    gates = jax.nn.softmax(top_v, axis=-1)
    n_asg = n_tok * TOP_K
    e_flat = top_e.reshape(n_asg).astype(jnp.int32)
    tok_flat = jnp.arange(n_asg, dtype=jnp.int32) // TOP_K
    g_flat = gates.reshape(n_asg)
    order = jnp.argsort(e_flat)
    e_sorted = e_flat[order]
    counts = jnp.bincount(e_flat, length=N_EXPERTS)
    starts = jnp.cumsum(counts) - counts
    nblk_per = (counts + EXPERT_BLOCK - 1) // EXPERT_BLOCK
    blk_end = jnp.cumsum(nblk_per)
    pstarts = (blk_end - nblk_per) * EXPERT_BLOCK
    dest = pstarts[e_sorted] + (jnp.arange(n_asg) - starts[e_sorted])
    n_blocks = -(-n_asg // EXPERT_BLOCK) + N_EXPERTS
    n_rows = n_blocks * EXPERT_BLOCK
    row_tok = jnp.full((n_rows,), n_tok, jnp.int32).at[dest].set(tok_flat[order])
    row_gate = jnp.zeros((n_rows,), jnp.float32).at[dest].set(g_flat[order])
    blk_expert = jnp.minimum(jnp.searchsorted(blk_end, jnp.arange(n_blocks), side='right'), N_EXPERTS - 1)
    x_pad = jnp.concatenate([xf, jnp.zeros((1, d), xf.dtype)], axis=0)
    xs = x_pad[row_tok].reshape(n_blocks, EXPERT_BLOCK, d)

    def expert_rows(args):
        xb, e = args
        hdn = xb @ w_up[e] + b_up[e]
        h_glu = jnp.minimum(hdn[..., ::2], SWIGLU_LIMIT)
        h_lin = jnp.clip(hdn[..., 1::2], -SWIGLU_LIMIT, SWIGLU_LIMIT)
        act = h_glu * jax.nn.sigmoid(SWIGLU_ALPHA * h_glu) * (h_lin + 1.0)
        return act @ w_down[e] + b_down[e]

    ys = lax.map(expert_rows, (xs, blk_expert)).reshape(n_rows, d)
    y = jax.ops.segment_sum(ys * row_gate[:, None].astype(ys.dtype), row_tok, num_segments=n_tok + 1)[:n_tok]
    return y.reshape(bsz, seq, d)


def setup_inputs(seed: int = 0) -> dict:
    key = jax.random.key(seed)
    ks = jax.random.split(key, 40)
    f32 = jnp.float32
    L, G, P, N = DEPTH, SSM_GROUPS, SSM_STATE, SSM_GROUP
    beta = DEEPNORM_BETA

    def nrm(i, shape, scale):
        return scale * jax.random.normal(ks[i], shape, f32)

    n_idx = jnp.arange(P, dtype=f32)
    return {
        'x': nrm(0, (BATCH, SEQ, D_MODEL), 1.0),
        'mem': nrm(1, (BATCH, MEM_LEN, D_MODEL), 1.0),
        'ln_in_g': 1.0 + nrm(2, (D_MODEL,), 0.02),
        'ln_in_b': nrm(3, (D_MODEL,), 0.02),
        'w_in': nrm(4, (L, D_MODEL, D_IN), D_MODEL ** -0.5),
        'kv_norm_g': 1.0 + nrm(5, (L, KV_LATENT), 0.02),
        'w_uk': nrm(6, (L, KV_LATENT, N_HEADS, HEAD_DIM), KV_LATENT ** -0.5),
        'w_uv': nrm(7, (L, KV_LATENT, N_HEADS, HEAD_DIM), KV_LATENT ** -0.5 * beta),
        'conv_w': nrm(8, (L, CONV_K, CONV_WIDTH), CONV_K ** -0.5),
        'conv_b': nrm(9, (L, CONV_WIDTH), 0.01),
        'lam_re': -0.5 + nrm(10, (L, G, P), 0.01),
        'lam_im': math.pi * n_idx + nrm(11, (L, G, P), 0.01),
        'b_re': nrm(12, (L, G, P, N), (2 * N) ** -0.5),
        'b_im': nrm(13, (L, G, P, N), (2 * N) ** -0.5),
        'c_re': nrm(14, (L, G, N, P), (2 * P) ** -0.5),
        'c_im': nrm(15, (L, G, N, P), (2 * P) ** -0.5),
        'd_skip': nrm(16, (L, G, N), 1.0),
        'log_dt': jax.random.uniform(ks[17], (L, G), f32, math.log(1e-3), math.log(1e-1)),
        'w_glu': nrm(18, (L, SSM_WIDTH, SSM_WIDTH), SSM_WIDTH ** -0.5),
        'b_glu': nrm(19, (L, SSM_WIDTH), 0.01),
        'w_mem_kv': nrm(20, (L, D_MODEL, 2 * MEM_HEADS * MEM_HEAD_DIM), D_MODEL ** -0.5),
        'w_branch': nrm(21, (L, N_BRANCH, BRANCH_WIDTH, D_MODEL), BRANCH_WIDTH ** -0.5),
        'w_o': nrm(22, (L, D_MODEL, D_MODEL), D_MODEL ** -0.5 * beta),
        'ln1_g': 1.0 + nrm(23, (L, D_MODEL), 0.02),
        'ln1_b': nrm(24, (L, D_MODEL), 0.02),
        'w_router': nrm(25, (L, D_MODEL, N_EXPERTS), D_MODEL ** -0.5),
        'b_router': nrm(26, (L, N_EXPERTS), 0.01),
        'w_up': nrm(27, (L, N_EXPERTS, D_MODEL, 2 * D_EXPERT), D_MODEL ** -0.5),
        'b_up': nrm(28, (L, N_EXPERTS, 2 * D_EXPERT), 0.01),
        'w_down': nrm(29, (L, N_EXPERTS, D_EXPERT, D_MODEL), D_EXPERT ** -0.5 * beta),
        'b_down': nrm(30, (L, N_EXPERTS, D_MODEL), 0.01),
        'ln2_g': 1.0 + nrm(31, (L, D_MODEL), 0.02),
        'ln2_b': nrm(32, (L, D_MODEL), 0.02),
    }


def reference(x, mem, ln_in_g, ln_in_b, w_in, kv_norm_g, w_uk, w_uv, conv_w, conv_b,
              lam_re, lam_im, b_re, b_im, c_re, c_im, d_skip, log_dt, w_glu, b_glu,
              w_mem_kv, w_branch, w_o, ln1_g, ln1_b, w_router, b_router, w_up, b_up,
              w_down, b_down, ln2_g, ln2_b):
    bsz, seq, _ = x.shape
    split_points = np.cumsum(SPLITS)[:-1].tolist()
    h = layer_norm(x, ln_in_g, ln_in_b)
    for l in range(DEPTH):
        proj = h @ w_in[l]
        (q, ckv, q_idx, k_idx, w_idx, conv_u, conv_gb, conv_gc,
         ssm_u, mem_q, gate_logits) = jnp.split(proj, split_points, axis=-1)
        att = dsa_attention(q.reshape(bsz, seq, N_HEADS, HEAD_DIM), rms_norm(ckv, kv_norm_g[l]),
                            q_idx.reshape(bsz, seq, IDX_HEADS, IDX_DIM), k_idx, w_idx, w_uk[l], w_uv[l])
        cnv = short_conv(conv_u, conv_gb, conv_gc, conv_w[l], conv_b[l])
        ssm = s5_mixer(ssm_u, lam_re[l], lam_im[l], b_re[l], b_im[l], c_re[l], c_im[l],
                       d_skip[l], log_dt[l], w_glu[l], b_glu[l])
        mem_o = memory_attention(mem_q.reshape(bsz, seq, MEM_HEADS, MEM_HEAD_DIM), mem, w_mem_kv[l])
        branches = jnp.stack([att, cnv, ssm, mem_o], axis=2)
        gates = jax.nn.sigmoid(gate_logits.reshape(bsz, seq, N_BRANCH, D_MODEL))
        merged = jnp.sum(jnp.einsum('btrc,rcd->btrd', branches, w_branch[l]) * gates, axis=2)
        h = layer_norm(DEEPNORM_ALPHA * h + merged @ w_o[l], ln1_g[l], ln1_b[l])
        ffn = moe(h, w_router[l], b_router[l], w_up[l], b_up[l], w_down[l], b_down[l])
        h = layer_norm(DEEPNORM_ALPHA * h + ffn, ln2_g[l], ln2_b[l])
    return h
```

```python
from contextlib import ExitStack
import math
import numpy as np
import concourse.bass as bass
import concourse.mybir as mybir
from concourse.bass_utils import run_bass_kernel_spmd

F32 = mybir.dt.float32
BF16 = mybir.dt.bfloat16
ALU = mybir.AluOpType
AF = mybir.ActivationFunctionType
AX = mybir.AxisListType

NCORES = 8
D = 1024
T = 8192
TOWN = 2048
WIN = 8192
PRE = WIN - TOWN
DEPTH = 4
D_IN = 7592
C_Q, C_CKV, C_QI, C_KI, C_WI, C_CU, C_GB, C_GC, C_SU, C_MQ, C_GATE = (
    0, 512, 640, 896, 928, 936, 1448, 1960, 2472, 2984, 3496)
LN_EPS = 1e-5
ALPHA = (2 * DEPTH) ** 0.25
NEG = -30000.0
BIG = 1.0e30
TWO_PI = 2.0 * math.pi
MAGIC = 12582912.0
NBIS = 22


class V:
    def __init__(self, t, ap):
        self.t = t
        self.ap = ap


class TT:
    def __init__(self, h, name):
        self.h = h
        self.name = name
        self.w = None
        self.r = []

    def __getitem__(self, idx):
        return V(self, self.h[idx])


def _ap(x):
    return x.ap if isinstance(x, V) else x


def _ts(*xs):
    return [x.t for x in xs if isinstance(x, V)]


class Prog:
    def __init__(self, nc, es):
        self.nc = nc
        self.es = es
        self.eng = {"pe": nc.tensor, "act": nc.scalar, "dve": nc.vector,
                    "pool": nc.gpsimd, "sp": nc.sync}
        self.sem = {}
        self.cnt = {}
        self.waited = {e: {} for e in self.eng}
        for e in self.eng:
            self.sem[e] = es.enter_context(nc.semaphore("s_" + e))
            self.cnt[e] = 0
        self.ninst = 0
        self.uid = 0

    def sb(self, name, shape, dt=F32):
        self.uid += 1
        return TT(self.es.enter_context(self.nc.sbuf_tensor(f"{name}_u{self.uid}", shape, dt)), name)

    def ps(self, name, shape, dt=F32):
        return TT(self.es.enter_context(self.nc.psum_tensor(name, shape, dt)), name)

    def dram(self, name, shape, dt=F32, kind="Internal"):
        return TT(self.nc.dram_tensor(name, shape, dt, kind=kind), name)

    def dsem(self, name):
        key = "d_" + name
        if key not in self.sem:
            self.sem[key] = self.es.enter_context(self.nc.semaphore(key))
            self.cnt[key] = 0
        return key

    def _wait(self, e, deps):
        need = {}
        for d in deps:
            if d is None:
                continue
            k, v = d
            if k == e and e == "pe":
                continue
            if v > need.get(k, 0):
                need[k] = v
        for k, v in need.items():
            if self.waited[e].get(k, 0) >= v:
                continue
            self.eng[e].wait_ge(self.sem[k], v)
            self.waited[e][k] = v

    def _deps(self, reads, writes):
        deps = []
        for t in reads:
            deps.append(t.w)
        for t in writes:
            deps.append(t.w)
            deps.extend(t.r)
        return deps

    def op(self, e, fn, reads=(), writes=()):
        self._wait(e, self._deps(reads, writes))
        ins = fn(self.eng[e])
        self.cnt[e] += 1
        ins.then_inc(self.sem[e], 1)
        d = (e, self.cnt[e])
        for t in reads:
            t.r.append(d)
        for t in writes:
            t.w = d
            t.r = []
        self.ninst += 1
        return d

    def dma(self, q, out, in_, sem=None, **kw):
        self._wait(q, self._deps([in_.t], [out.t]))
        key = self.dsem(sem or out.t.name)
        ins = self.eng[q].dma_start(out=out.ap, in_=in_.ap, **kw)
        self.cnt[key] += 16
        ins.then_inc(self.sem[key], 16)
        d = (key, self.cnt[key])
        in_.t.r.append(d)
        out.t.w = d
        out.t.r = []
        self.ninst += 1
        return d

    def allgather(self, out, in_, n=NCORES):
        self._wait("pool", self._deps([in_.t], [out.t]))
        key = self.dsem(out.t.name)
        ins = self.nc.gpsimd.collective_compute("AllGather", op=ALU.bypass, replica_groups=[list(range(n))],
                                                ins=[in_.ap], outs=[out.ap])
        self.cnt[key] += 16
        ins.then_inc(self.sem[key], 16)
        d = (key, self.cnt[key])
        in_.t.r.append(d)
        out.t.w = d
        out.t.r = []
        self.ninst += 1
        return d

    def finish(self, e, tensors):
        self._wait(e, [t.w for t in tensors])

    def mm(self, out, lhsT, rhs, start=True, stop=True):
        return self.op("pe", lambda e: e.matmul(out.ap, lhsT.ap, rhs.ap, start=start, stop=stop),
                       reads=[lhsT.t, rhs.t], writes=[out.t])

    def tr(self, out, in_, ident):
        return self.op("pe", lambda e: e.transpose(out.ap, in_.ap, ident.ap),
                       reads=[in_.t, ident.t], writes=[out.t])

    def act(self, out, in_, func, bias=0.0, scale=1.0, accum=None, e="act"):
        rd = _ts(in_, bias, scale)
        wr = _ts(out, accum)
        kw = {}
        if accum is not None:
            kw["accum_out"] = accum.ap
        return self.op("act", lambda g: g.activation(out=out.ap, in_=in_.ap, func=func,
                                                     bias=_ap(bias), scale=_ap(scale), **kw),
                       reads=rd, writes=wr)

    def ts(self, e, out, in0, s1, s2, op0, op1=None, accum=None):
        rd = _ts(in0, s1, s2)
        wr = _ts(out, accum)
        kw = {}
        if op1 is not None:
            kw["op1"] = op1
        if accum is not None:
            kw["accum_out"] = accum.ap
        return self.op(e, lambda g: g.tensor_scalar(out.ap, in0.ap, _ap(s1), _ap(s2), op0, **kw),
                       reads=rd, writes=wr)

    def tt(self, e, out, in0, in1, op):
        return self.op(e, lambda g: g.tensor_tensor(out.ap, in0.ap, in1.ap, op),
                       reads=_ts(in0, in1), writes=_ts(out))

    def stt(self, out, in0, s, in1, op0, op1):
        return self.op("dve", lambda g: g.scalar_tensor_tensor(out.ap, in0.ap, _ap(s), in1.ap, op0, op1),
                       reads=_ts(in0, s, in1), writes=_ts(out))

    def cp(self, e, out, in_):
        if e == "act":
            return self.act(out, in_, AF.Copy)
        return self.op(e, lambda g: g.tensor_copy(out.ap, in_.ap), reads=_ts(in_), writes=_ts(out))

    def red(self, out, in_, op, axis=AX.X):
        return self.op("dve", lambda g: g.tensor_reduce(out.ap, in_.ap, axis, op),
                       reads=_ts(in_), writes=_ts(out))

    def scan(self, out, d0, d1, init, op0, op1):
        return self.op("dve", lambda g: g.tensor_tensor_scan(out.ap, d0.ap, d1.ap, _ap(init), op0, op1),
                       reads=_ts(d0, d1, init), writes=_ts(out))

    def recip(self, out, in_):
        return self.op("dve", lambda g: g.reciprocal(out.ap, in_.ap), reads=_ts(in_), writes=_ts(out))

    def memset(self, e, out, val):
        return self.op(e, lambda g: g.memset(out.ap, val), reads=[], writes=_ts(out))

    def max8(self, out, in_):
        return self.op("dve", lambda g: g.max(out.ap, in_.ap), reads=_ts(in_), writes=_ts(out))


class Small:
    def __init__(self, P, name, n=64):
        self.P = P
        self.name = name
        self.k = 0

    def col(self, w=1):
        self.k += 1
        return self.P.sb(f"{self.name}_{self.k}", [128, w], F32)


def ln_tok(P, x, out, grep, brep, S, junk):
    msum = S.col(); negmean = S.col(); ss = S.col(); std = S.col(); rstd = S.col()
    P.red(msum[:, :], x, ALU.add)
    P.ts("dve", negmean[:, :], msum[:, :], -1.0 / D, None, ALU.mult)
    P.act(junk, x, AF.Square, bias=negmean[:, :], scale=1.0, accum=ss[:, :])
    P.ts("dve", std[:, :], ss[:, :], 1.0 / D, LN_EPS, ALU.mult, ALU.add)
    P.act(std[:, :], std[:, :], AF.Sqrt)
    P.recip(rstd[:, :], std[:, :])
    P.ts("dve", out, x, negmean[:, :], rstd[:, :], ALU.add, ALU.mult)
    P.tt("dve", out, out, grep, ALU.mult)
    P.tt("dve", out, out, brep, ALU.add)


def build_ln_in():
    nc = bass.Bass("TRN2", target_bir_lowering=False)
    es = ExitStack()
    P = Prog(nc, es)
    x = P.dram("x", [TOWN, D], F32, kind="ExternalInput")
    g = P.dram("g", [128, D], F32, kind="ExternalInput")
    b = P.dram("b", [128, D], F32, kind="ExternalInput")
    y = P.dram("y", [TOWN, D], F32, kind="ExternalOutput")
    gs = P.sb("gs", [128, D]); bs = P.sb("bs", [128, D])
    P.dma("sp", gs[:, :], g[:, :]); P.dma("sp", bs[:, :], b[:, :])
    S = Small(P, "lnin")
    junk = P.sb("junk", [128, D])
    xs = [P.sb(f"x{i}", [128, D]) for i in range(2)]
    os_ = [P.sb(f"o{i}", [128, D]) for i in range(2)]
    for i in range(TOWN // 128):
        xt = xs[i % 2]; ot = os_[i % 2]
        P.dma("sp", xt[:, :], x[i * 128:(i + 1) * 128, :])
        ln_tok(P, xt[:, :], ot[:, :], gs[:, :], bs[:, :], S, junk[:, :])
        P.dma("sp", y[i * 128:(i + 1) * 128, :], ot[:, :])
    P.finish("sp", [y])
    es.close()
    return nc


class Ctx:
    pass


def barrier(P):
    allk = [(k, v) for k, v in P.cnt.items() if v > 0]
    for e in P.eng:
        P._wait(e, allk)


class Stage:
    def __init__(self, P):
        self.P = P

    def __enter__(self):
        self.old = self.P.es
        self.es = ExitStack()
        self.P.es = self.es
        return self

    def __exit__(self, *a):
        barrier(self.P)
        self.P.es = self.old
        self.es.close()
        return False


def load_w(P, dst, src_ap_tt, q="pool"):
    return P.dma(q, dst, src_ap_tt)


def wview(w_in, c0, n):
    return V(w_in, w_in.h[:, c0:c0 + n].rearrange("(kt k) n -> k kt n", k=128))


def stage_window(P, C):
    pb = C.pb
    with Stage(P):
        wsu = P.sb("wsu", [128, 8, 512], BF16)
        wck = P.sb("wck", [128, 8, 128], BF16)
        wki = P.sb("wki", [128, 8, 32], BF16)
        P.dma("pool", wsu[:, :, :], wview(C.w_in, C_SU, 512))
        P.dma("pool", wck[:, :, :], wview(C.w_in, C_CKV, 128))
        P.dma("pool", wki[:, :, :], wview(C.w_in, C_KI, 32))
        kvg = P.sb("kvg_s", [128, 1]); P.dma("sp", kvg[:, :], C.kvg[:, :])
        kvgrow = P.sb("kvgrow_s", [128, 128]); P.dma("sp", kvgrow[:, :], C.kvgrow[:, :])
        hTc = [P.sb(f"hTc{i}", [128, 8, 512], BF16) for i in range(2)]
        ust = [P.sb(f"ust{i}", [128, 512]) for i in range(2)]
        sq = P.sb("sq", [128, 512], BF16)
        rst = P.sb("rst", [128, 512])
        junk = P.sb("wjunk", [128, 128])
        S = Small(P, "win")
        for g in range(WIN // 512):
            h = hTc[g % 2]
            P.dma("pool", h[:, :, :], V(C.hT, C.hT.h[:, C.off + g * 512:C.off + (g + 1) * 512].rearrange("(kt k) t -> k kt t", k=128)))
            for ct in range(4):
                ps = pb[ct % 2]
                for kt in range(8):
                    P.mm(ps[:, :], wsu[:, kt, ct * 128:(ct + 1) * 128], h[:, kt, :], start=(kt == 0), stop=(kt == 7))
                u = ust[ct % 2]
                P.cp("act", u[:, :], ps[:, :])
                P.dma("sp", C.uT[ct * 128:(ct + 1) * 128, g * 512:(g + 1) * 512], u[:, :])
            ps = pb[2]
            for kt in range(8):
                P.mm(ps[:, :], wck[:, kt, :], h[:, kt, :], start=(kt == 0), stop=(kt == 7))
            P.act(sq[:, :], ps[:, :], AF.Square)
            P.mm(pb[3][:, :], C.ones_bf[:, :], sq[:, :])
            P.ts("dve", rst[:, :], pb[3][:, :], 1.0 / 128, LN_EPS, ALU.mult, ALU.add)
            P.act(rst[:, :], rst[:, :], AF.Sqrt)
            P.recip(rst[:, :], rst[:, :])
            P.stt(C.ckvT[:, g * 512:(g + 1) * 512], ps[:, :], kvg[:, :], rst[:, :], ALU.mult, ALU.mult)
            ps = pb[4]
            for tt_ in range(4):
                for kt in range(8):
                    P.mm(ps[:, tt_ * 128:(tt_ + 1) * 128], h[:, kt, tt_ * 128:(tt_ + 1) * 128], wck[:, kt, :],
                         start=(kt == 0), stop=(kt == 7))
            for tt_ in range(4):
                ss = S.col(); rs = S.col()
                P.act(junk[:, :], ps[:, tt_ * 128:(tt_ + 1) * 128], AF.Square, accum=ss[:, :])
                P.ts("dve", rs[:, :], ss[:, :], 1.0 / 128, LN_EPS, ALU.mult, ALU.add)
                P.act(rs[:, :], rs[:, :], AF.Sqrt)
                P.recip(rs[:, :], rs[:, :])
                P.stt(C.ckv_tok[:, g * 4 + tt_, 0:128], ps[:, tt_ * 128:(tt_ + 1) * 128], rs[:, :], kvgrow[:, :],
                      ALU.mult, ALU.mult)
            ps = pb[5]
            for kt in range(8):
                P.mm(ps[0:32, :], wki[:, kt, :], h[:, kt, :], start=(kt == 0), stop=(kt == 7))
            P.cp("act", C.kidxT[0:32, g * 512:(g + 1) * 512], ps[0:32, :])


SEG = 512
NSEG = WIN // SEG
OWN0 = PRE // SEG


def reduce_turns(P, out_f, u, tmp):
    P.ts("dve", tmp, u, MAGIC, None, ALU.add)
    P.ts("dve", tmp, tmp, MAGIC, None, ALU.subtract)
    P.tt("dve", out_f, u, tmp, ALU.subtract)


def sincos(P, S_out, C_out, f, tmp):
    P.act(S_out, f, AF.Sin, scale=TWO_PI)
    P.act(tmp, f, AF.Abs)
    P.act(C_out, tmp, AF.Sin, scale=-TWO_PI, bias=C_halfpi)


C_halfpi = None


def stage_ssm(P, C):
    global C_halfpi
    pb = C.pb
    with Stage(P):
        halfpi = P.sb("halfpi", [128, 1]); P.memset("dve", halfpi[:, :], math.pi / 2)
        C_halfpi = halfpi[:, :]
        lhs_bu = P.sb("lhs_bu", [128, 16, 2, 128], BF16)
        lhs_c = P.sb("lhs_c", [128, 16, 2, 128], BF16)
        rcol = P.sb("rcol", [128, 16]); fturn = P.sb("fturn", [128, 16]); f0 = P.sb("f0", [128, 16, NSEG])
        dsk = P.sb("dsk", [128, 4]); P.dma("sp", dsk[:, :], C.dskip[:, :])
        bgl = P.sb("bgl", [128, 4]); P.dma("sp", bgl[:, :], C.bglu[:, :])
        with Stage(P):
            def ld(name, src, shape):
                t = P.sb(name, shape);
                P.dma("sp", t[tuple(slice(None) for _ in shape)], src[tuple(slice(None) for _ in shape)])
                return t
            lre = ld("lre", C.lamre, [128, 16]); lim = ld("lim", C.lamim, [128, 16]); ldt = ld("ldt", C.logdt, [128, 16])
            bre = ld("bre_s", C.bre, [128, 16, 16]); bim = ld("bim_s", C.bim, [128, 16, 16])
            cre = ld("cre_s", C.cre, [128, 16, 16]); cim = ld("cim_s", C.cim, [128, 16, 16])
            n16 = lambda nm: P.sb(nm, [128, 16])
            dt = n16("dt"); lnr = n16("lnr"); th = n16("th"); tmp = n16("tmp16"); ff = n16("ff")
            sn = n16("sn"); cs = n16("cs"); ar = n16("ar"); ai = n16("ai"); den = n16("den")
            kr = n16("kr"); ki = n16("ki"); nki = n16("nki"); t1 = n16("t1_16"); t2 = n16("t2_16")
            A = slice(None)
            P.act(dt[:, :], ldt[:, :], AF.Exp)
            P.tt("dve", lnr[:, :], lre[:, :], dt[:, :], ALU.mult)
            P.tt("dve", th[:, :], lim[:, :], dt[:, :], ALU.mult)
            P.ts("dve", fturn[:, :], th[:, :], 1.0 / TWO_PI, None, ALU.mult)
            P.act(rcol[:, :], lnr[:, :], AF.Exp)
            reduce_turns(P, ff[:, :], fturn[:, :], tmp[:, :])
            sincos(P, sn[:, :], cs[:, :], ff[:, :], tmp[:, :])
            P.tt("dve", ar[:, :], rcol[:, :], cs[:, :], ALU.mult)
            P.tt("dve", ai[:, :], rcol[:, :], sn[:, :], ALU.mult)
            P.ts("dve", ar[:, :], ar[:, :], -1.0, None, ALU.add)
            P.tt("dve", den[:, :], lre[:, :], lre[:, :], ALU.mult)
            P.tt("dve", t1[:, :], lim[:, :], lim[:, :], ALU.mult)
            P.tt("dve", den[:, :], den[:, :], t1[:, :], ALU.add)
            P.recip(den[:, :], den[:, :])
            P.tt("dve", t1[:, :], ar[:, :], lre[:, :], ALU.mult)
            P.tt("dve", t2[:, :], ai[:, :], lim[:, :], ALU.mult)
            P.tt("dve", kr[:, :], t1[:, :], t2[:, :], ALU.add)
            P.tt("dve", kr[:, :], kr[:, :], den[:, :], ALU.mult)
            P.tt("dve", t1[:, :], ai[:, :], lre[:, :], ALU.mult)
            P.tt("dve", t2[:, :], ar[:, :], lim[:, :], ALU.mult)
            P.tt("dve", ki[:, :], t1[:, :], t2[:, :], ALU.subtract)
            P.tt("dve", ki[:, :], ki[:, :], den[:, :], ALU.mult)
            P.ts("dve", nki[:, :], ki[:, :], -1.0, None, ALU.mult)
            for q in range(NSEG):
                P.ts("dve", f0[:, :, q], fturn[:, :], float(SEG * q), None, ALU.mult)
            ftmp = P.sb("ftmp", [128, 16, NSEG])
            reduce_turns(P, f0[:, :, :], f0[:, :, :], ftmp[:, :, :])
            bbr = P.sb("bbr", [128, 16, 16]); bbi = P.sb("bbi", [128, 16, 16]); tb = P.sb("tb", [128, 16])
            Sp = P.sb("Sp", [128, 32, 128])
            P.memset("pool", Sp[:, :, :], 0.0)
            lcf = P.sb("lcf", [128, 32, 128])
            P.memset("pool", lcf[:, :, :], 0.0)
            ncim = P.sb("ncim", [128, 16, 16])
            P.ts("dve", ncim[:, :, :], cim[:, :, :], -1.0, None, ALU.mult)
            for i in range(16):
                P.ts("dve", tb[:, :], bre[:, i, :], kr[:, i:i + 1], None, ALU.mult)
                P.stt(bbr[:, i, :], bim[:, i, :], nki[:, i:i + 1], tb[:, :], ALU.mult, ALU.add)
                P.ts("dve", tb[:, :], bim[:, i, :], kr[:, i:i + 1], None, ALU.mult)
                P.stt(bbi[:, i, :], bre[:, i, :], ki[:, i:i + 1], tb[:, :], ALU.mult, ALU.add)
                c0 = 32 * (i % 4)
                for gg in range(2):
                    rows = slice(64 * gg, 64 * gg + 64)
                    cols = slice(c0 + 16 * gg, c0 + 16 * gg + 16)
                    P.cp("dve", Sp[rows, 2 * i, cols], bbr[rows, i, :])
                    P.cp("dve", Sp[rows, 2 * i + 1, cols], bbi[rows, i, :])
                    P.cp("dve", lcf[rows, 2 * i, cols], cre[rows, i, :])
                    P.cp("dve", lcf[rows, 2 * i + 1, cols], ncim[rows, i, :])
            for i in range(16):
                for ri in range(2):
                    ps = pb[(2 * i + ri) % 4]
                    P.tr(ps[:, 0:128], Sp[:, 2 * i + ri, :], C.ident[:, :])
                    P.cp("act", lhs_bu[:, i, ri, :], ps[:, 0:128])
                    P.cp("pool", lhs_c[:, i, ri, :], lcf[:, 2 * i + ri, :])
        iota = P.sb("iota", [128, SEG])
        P.op("pool", lambda g: g.iota(iota.h[:, :], [[1, SEG]], 0, channel_multiplier=0, allow_small_or_imprecise_dtypes=True),
             reads=[], writes=[iota])
        ones = P.sb("ones_s", [128, SEG]); P.memset("dve", ones[:, :], 1.0)
        rbc = P.sb("rbc", [128, SEG])
        uTt = P.sb("uTt", [128, WIN], BF16)
        mk = lambda nm, dt_=F32: P.sb(nm, [128, SEG], dt_)
        tu = mk("tu"); tn = mk("tn"); tf = mk("tf"); tS = mk("tS"); tC = mk("tC")
        t1 = mk("r1"); t2 = mk("r2"); t3 = mk("r3"); t4 = mk("r4")
        zs = [[mk(f"zs{a}{b}") for b in range(2)] for a in range(2)]
        zro = P.sb("zro", [128, TOWN]); zio = P.sb("zio", [128, TOWN])
        hr = mk("hr", BF16); hi = mk("hi", BF16)
        zbf = P.sb("zbf", [128, 4, TOWN], BF16)
        u32 = P.sb("u32", [128, TOWN]); yy = P.sb("yy", [128, TOWN]); y2 = P.sb("y2", [128, TOWN])
        for i in range(16):
            ct = i // 4
            if i % 4 == 0:
                P.dma("pool", uTt[:, :], C.uT[ct * 128:(ct + 1) * 128, :])
                P.dma("sp", u32[:, :], C.uT[ct * 128:(ct + 1) * 128, PRE:WIN])
            P.ts("dve", rbc[:, :], ones[:, :], rcol[:, i:i + 1], None, ALU.mult)
            prev = None
            for q in range(NSEG):
                own = q >= OWN0
                P.ts("dve", tu[:, :], iota[:, :], fturn[:, i:i + 1], f0[:, i, q:q + 1], ALU.mult, ALU.add)
                reduce_turns(P, tf[:, :], tu[:, :], tn[:, :])
                sincos(P, tS[:, :], tC[:, :], tf[:, :], tn[:, :])
                pr = pb[q % 2]; pi_ = pb[2 + q % 2]
                P.mm(pr[:, :], lhs_bu[:, i, 0, :], uTt[:, q * SEG:(q + 1) * SEG])
                P.mm(pi_[:, :], lhs_bu[:, i, 1, :], uTt[:, q * SEG:(q + 1) * SEG])
                P.tt("dve", t1[:, :], tC[:, :], pr[:, :], ALU.mult)
                P.tt("dve", t2[:, :], tS[:, :], pi_[:, :], ALU.mult)
                P.tt("dve", t3[:, :], tC[:, :], pi_[:, :], ALU.mult)
                P.tt("dve", t4[:, :], tS[:, :], pr[:, :], ALU.mult)
                P.tt("pool", t1[:, :], t1[:, :], t2[:, :], ALU.add)
                P.tt("pool", t3[:, :], t3[:, :], t4[:, :], ALU.subtract)
                if own:
                    o = (q - OWN0) * SEG
                    zr_o = zro[:, o:o + SEG]; zi_o = zio[:, o:o + SEG]
                else:
                    zr_o = zs[0][q % 2][:, :]; zi_o = zs[1][q % 2][:, :]
                ir = 0.0 if prev is None else prev[0]
                ii = 0.0 if prev is None else prev[1]
                P.scan(zr_o, rbc[:, :], t1[:, :], ir, ALU.mult, ALU.add)
                P.scan(zi_o, rbc[:, :], t3[:, :], ii, ALU.mult, ALU.add)
                if own:
                    prev = (zro[:, o + SEG - 1:o + SEG], zio[:, o + SEG - 1:o + SEG])
                else:
                    prev = (zs[0][q % 2][:, SEG - 1:SEG], zs[1][q % 2][:, SEG - 1:SEG])
                if own:
                    P.tt("pool", t2[:, :], tC[:, :], zr_o, ALU.mult)
                    P.tt("pool", t4[:, :], tS[:, :], zi_o, ALU.mult)
                    P.tt("dve", hr[:, :], t2[:, :], t4[:, :], ALU.subtract)
                    P.tt("pool", t2[:, :], tS[:, :], zr_o, ALU.mult)
                    P.tt("pool", t4[:, :], tC[:, :], zi_o, ALU.mult)
                    P.tt("dve", hi[:, :], t2[:, :], t4[:, :], ALU.add)
                    py = pb[4 + (q - OWN0)]
                    P.mm(py[:, :], lhs_c[:, i, 0, :], hr[:, :], start=(i % 4 == 0), stop=False)
                    P.mm(py[:, :], lhs_c[:, i, 1, :], hi[:, :], start=False, stop=(i % 4 == 3))
            if i % 4 == 3:
                for s in range(4):
                    sl = slice(s * SEG, (s + 1) * SEG)
                    P.stt(yy[:, sl], u32[:, sl], dsk[:, ct:ct + 1], pb[4 + s][:, :], ALU.mult, ALU.add)
                P.tt("pool", y2[:, :], yy[:, :], yy[:, :], ALU.mult)
                P.ts("dve", y2[:, :], y2[:, :], 0.0713548163, 1.5957691216, ALU.mult, ALU.add)
                P.tt("dve", y2[:, :], y2[:, :], yy[:, :], ALU.mult)
                P.act(y2[:, :], y2[:, :], AF.Sigmoid)
                P.tt("dve", yy[:, :], yy[:, :], y2[:, :], ALU.mult)
                P.cp("act", zbf[:, ct, :], yy[:, :])
                P.dma("sp", C.z32[ct * 128:(ct + 1) * 128, :], yy[:, :])
        wgl = P.sb("wgl", [128, 4, 512], BF16)
        P.dma("pool", wgl[:, :, :], V(C.w_glu, C.w_glu.h[:, :].rearrange("(kt k) n -> k kt n", k=128)))
        for co in range(4):
            P.dma("sp", u32[:, :], C.z32[co * 128:(co + 1) * 128, :])
            for tg in range(4):
                ps = pb[tg % 4]
                sl = slice(tg * 512, (tg + 1) * 512)
                for kt in range(4):
                    P.mm(ps[:, :], wgl[:, kt, co * 128:(co + 1) * 128], zbf[:, kt, sl], start=(kt == 0), stop=(kt == 3))
                P.act(y2[:, sl], ps[:, :], AF.Sigmoid, bias=bgl[:, co:co + 1])
                P.tt("dve", hr[:, :], u32[:, sl], y2[:, sl], ALU.mult)
                P.dma("sp", C.brT[2, co * 128:(co + 1) * 128, sl], hr[:, :])


def proj_fm(P, C, ps, w, c0, n, tg, rows=None):
    for kt in range(8):
        P.mm(ps[0:n, :], w[:, kt, c0:c0 + n], C.hTo[:, kt, tg * 512:(tg + 1) * 512], start=(kt == 0), stop=(kt == 7))


def stage_conv(P, C):
    pb = C.pb
    with Stage(P):
        wcu = P.sb("wcu", [128, 8, 512], BF16); wgb = P.sb("wgb", [128, 8, 512], BF16); wgc = P.sb("wgc", [128, 8, 512], BF16)
        P.dma("pool", wcu[:, :, :], wview(C.w_in, C_CU, 512))
        P.dma("pool", wgb[:, :, :], wview(C.w_in, C_GB, 512))
        P.dma("pool", wgc[:, :, :], wview(C.w_in, C_GC, 512))
        cw = P.sb("cw", [128, 4, 3]); P.dma("sp", cw[:, :, :], C.convw[:, :, :])
        cb = P.sb("cb", [128, 4]); P.dma("sp", cb[:, :], C.convb[:, :])
        hh = P.sb("hhalo", [128, 8, 2], BF16)
        P.dma("pool", hh[:, :, :], V(C.hT, C.hT.h[:, C.off + PRE - 2:C.off + PRE].rearrange("(kt k) t -> k kt t", k=128)))
        v = P.sb("cv", [128, TOWN + 2]); us = P.sb("cus", [128, 512]); y = P.sb("cy", [128, TOWN])
        ob = P.sb("cob", [128, 512], BF16)
        for ct in range(4):
            cs = slice(ct * 128, (ct + 1) * 128)
            for kt in range(8):
                P.mm(pb[0][:, 0:2], wcu[:, kt, cs], hh[:, kt, :], start=(kt == 0), stop=(kt == 7))
            for kt in range(8):
                P.mm(pb[1][:, 0:2], wgc[:, kt, cs], hh[:, kt, :], start=(kt == 0), stop=(kt == 7))
            P.cp("act", us[:, 0:2], pb[0][:, 0:2])
            P.tt("dve", v[:, 0:2], us[:, 0:2], pb[1][:, 0:2], ALU.mult)
            for tg in range(4):
                proj_fm(P, C, pb[2], wcu, ct * 128, 128, tg)
                proj_fm(P, C, pb[3], wgc, ct * 128, 128, tg)
                P.cp("act", us[:, :], pb[2][:, :])
                P.tt("dve", v[:, 2 + tg * 512:2 + (tg + 1) * 512], us[:, :], pb[3][:, :], ALU.mult)
            P.ts("dve", y[:, :], v[:, 2:TOWN + 2], cw[:, ct, 2:3], cb[:, ct:ct + 1], ALU.mult, ALU.add)
            P.stt(y[:, :], v[:, 1:TOWN + 1], cw[:, ct, 1:2], y[:, :], ALU.mult, ALU.add)
            P.stt(y[:, :], v[:, 0:TOWN], cw[:, ct, 0:1], y[:, :], ALU.mult, ALU.add)
            for tg in range(4):
                proj_fm(P, C, pb[4 + tg % 2], wgb, ct * 128, 128, tg)
                P.tt("dve", ob[:, :], y[:, tg * 512:(tg + 1) * 512], pb[4 + tg % 2][:, :], ALU.mult)
                P.dma("sp", C.brT[1, cs, tg * 512:(tg + 1) * 512], ob[:, :])


def stage_mem(P, C):
    pb = C.pb
    with Stage(P):
        wmq = P.sb("wmq", [128, 8, 512], BF16)
        P.dma("pool", wmq[:, :, :], wview(C.w_in, C_MQ, 512))
        wkv = P.sb("wkv", [128, 8, 1024], BF16)
        P.dma("pool", wkv[:, :, :], V(C.w_mem, C.w_mem.h[:, :].rearrange("(kt k) n -> k kt n", k=128)))
        mT = P.sb("mT", [128, 8, 256], BF16)
        P.dma("pool", mT[:, :, :], V(C.memT, C.memT.h[:, :].rearrange("(kt k) m -> k kt m", k=128)))
        KT = P.sb("KT", [128, 4, 256], BF16)
        Vt = P.sb("Vt", [128, 2, 512], BF16)
        for h in range(4):
            for kt in range(8):
                P.mm(pb[0][:, 0:256], wkv[:, kt, h * 128:(h + 1) * 128], mT[:, kt, :], start=(kt == 0), stop=(kt == 7))
            P.cp("act", KT[:, h, :], pb[0][:, 0:256])
        for mt in range(2):
            for kt in range(8):
                P.mm(pb[1][:, :], mT[:, kt, mt * 128:(mt + 1) * 128], wkv[:, kt, 512:1024], start=(kt == 0), stop=(kt == 7))
            P.cp("act", Vt[:, mt, :], pb[1][:, :])
        mq = P.sb("mq", [128, 512], BF16); pT = P.sb("mpT", [128, 2, 512], BF16)
        rec = P.sb("mrec", [128, 512]); ob = P.sb("mob", [128, 512], BF16)
        for h in range(4):
            for tg in range(4):
                proj_fm(P, C, pb[2], wmq, h * 128, 128, tg)
                P.act(mq[:, :], pb[2][:, :], AF.Copy, scale=128.0 ** -0.5)
                for mt in range(2):
                    P.mm(pb[3 + mt][:, :], KT[:, h, mt * 128:(mt + 1) * 128], mq[:, :])
                    P.act(pT[:, mt, :], pb[3 + mt][:, :], AF.Exp)
                for mt in range(2):
                    P.mm(pb[5][:, :], Vt[:, mt, h * 128:(h + 1) * 128], pT[:, mt, :], start=(mt == 0), stop=(mt == 1))
                for mt in range(2):
                    P.mm(pb[6][:, :], C.ones_bf[:, :], pT[:, mt, :], start=(mt == 0), stop=(mt == 1))
                P.recip(rec[:, :], pb[6][:, :])
                P.tt("dve", ob[:, :], rec[:, :], pb[5][:, :], ALU.mult)
                P.dma("sp", C.brT[3, h * 128:(h + 1) * 128, tg * 512:(tg + 1) * 512], ob[:, :])


def stage_attproj(P, C):
    pb = C.pb
    with Stage(P):
        wq = P.sb("wq", [128, 8, 512], BF16); P.dma("pool", wq[:, :, :], wview(C.w_in, C_Q, 512))
        wqi = P.sb("wqi", [128, 8, 256], BF16); P.dma("pool", wqi[:, :, :], wview(C.w_in, C_QI, 256))
        wwi = P.sb("wwi", [128, 8, 8], BF16); P.dma("pool", wwi[:, :, :], wview(C.w_in, C_WI, 8))
        wuk = P.sb("wuk", [128, 512]); P.dma("sp", wuk[:, :], C.w_uk[:, :])
        wukT = P.sb("wukT", [128, 4, 128], BF16)
        for m in range(4):
            P.tr(pb[0][:, 0:128], wuk[:, m * 128:(m + 1) * 128], C.ident[:, :])
            P.cp("act", wukT[:, m, :], pb[0][:, 0:128])
        qT = P.sb("qT", [128, 4, TOWN], BF16)
        for m in range(4):
            for tg in range(4):
                proj_fm(P, C, pb[1 + tg % 2], wq, m * 128, 128, tg)
                P.cp("act", qT[:, m, tg * 512:(tg + 1) * 512], pb[1 + tg % 2][:, :])
        st = [P.sb(f"qst{i}", [128, 512], BF16) for i in range(2)]
        k = 0
        for h in range(8):
            m, hh = h // 2, h % 2
            rows = slice(64 * hh, 64 * hh + 64)
            for tg in range(4):
                ps = pb[3 + k % 2]; s = st[k % 2]; k += 1
                P.mm(ps[:, :], wukT[rows, m, :], qT[rows, m, tg * 512:(tg + 1) * 512])
                P.act(s[:, :], ps[:, :], AF.Copy, scale=0.125)
                P.dma("sp", C.qlat_d[:, h, tg * 512:(tg + 1) * 512], s[:, :])
        for h in range(8):
            for tg in range(4):
                ps = pb[5 + k % 2]; s = st[k % 2]; k += 1
                proj_fm(P, C, ps, wqi, h * 32, 32, tg)
                P.cp("act", s[0:32, :], ps[0:32, :])
                P.dma("sp", C.qidx_d[0:32, h, tg * 512:(tg + 1) * 512], s[0:32, :])
        for tt_ in range(16):
            for kt in range(8):
                P.mm(pb[7][:, 0:8], C.hTo[:, kt, tt_ * 128:(tt_ + 1) * 128], wwi[:, kt, :], start=(kt == 0), stop=(kt == 7))
            P.cp("act", C.widx[:, tt_, :], pb[7][:, 0:8])


def stage_att(P, C):
    pb = C.pb
    with Stage(P):
        acc = P.sb("acc", [128, WIN]); notsel = P.sb("notsel", [128, WIN], BF16)
        tmp = [P.sb(f"atmp{i}", [128, 512]) for i in range(2)]
        qi = P.sb("qi", [32, 8, 128], BF16); ql = P.sb("ql", [128, 8, 128], BF16)
        npad = P.sb("npad_s", [128, 1]); P.dma("sp", npad[:, :], C.npad[C.j, :, :])
        padc = P.sb("padc", [128, 64]); P.dma("sp", padc[:, :], C.padcol[C.j, :, :])
        tri = P.sb("tri_s", [128, 128]); P.dma("sp", tri[:, :], C.tri[:, :])
        negI = P.sb("negI", [128, 4, 128], BF16)
        for j in range(4):
            P.ts("dve", negI[:, j, :], C.ident[:, :], NEG, None, ALU.mult)
        wuv = P.sb("wuv", [128, 512]); P.dma("sp", wuv[:, :], C.w_uv[:, :])
        wuvp = P.sb("wuvp", [128, 8, 128], BF16)
        P.memset("pool", wuvp[:, :, :], 0.0)
        for h in range(8):
            P.cp("dve", wuvp[:, h, 64 * (h % 2):64 * (h % 2) + 64], wuv[:, h * 64:(h + 1) * 64])
        pTs = [P.sb(f"pT{i}", [128, 512], BF16) for i in range(3)]
        ol = P.sb("ol", [128, 8, 128]); olT = P.sb("olT", [128, 8, 128], BF16)
        ab = P.sb("ab", [128, 4, 128], BF16)
        S = Small(P, "att")
        lo = S.col(); hi = S.col(); mid = S.col(); cnt = S.col(); c2 = S.col(); ge = S.col(); d1 = S.col(); d2 = S.col()
        rec = S.col(8)
        kk = 0
        for qb in range(TOWN // 128):
            nk = PRE // 128 + qb + 1
            NK = nk * 128
            qs = slice(qb * 128, (qb + 1) * 128)
            P.dma("sp", qi[:, :, :], C.qidx_d[0:32, :, qs])
            P.dma("sp", ql[:, :, :], C.qlat_d[:, :, qs])
            nsp = (NK + 511) // 512
            for h in range(8):
                for s in range(nsp):
                    n = min(512, NK - 512 * s)
                    ks = slice(512 * s, 512 * s + n)
                    ps = pb[kk % 2]; t = tmp[kk % 2]; kk += 1
                    P.mm(ps[:, 0:n], qi[0:32, h, :], C.kidxT[0:32, ks])
                    P.act(t[:, 0:n], ps[:, 0:n], AF.Relu)
                    if h == 0:
                        P.ts("dve", acc[:, ks], t[:, 0:n], C.widx[:, qb, 0:1], None, ALU.mult)
                    else:
                        P.stt(acc[:, ks], t[:, 0:n], C.widx[:, qb, h:h + 1], acc[:, ks], ALU.mult, ALU.add)
            P.red(hi[:, :], acc[:, 0:NK], ALU.max)
            P.red(lo[:, :], acc[:, 0:NK], ALU.min)
            P.tt("dve", acc[:, NK - 128:NK], acc[:, NK - 128:NK], tri[:, :], ALU.add)
            for it in range(NBIS):
                P.ts("dve", mid[:, :], lo[:, :], hi[:, :], 0.5, ALU.add, ALU.mult)
                P.ts("dve", notsel[:, 0:NK], acc[:, 0:NK], mid[:, :], None, ALU.is_ge, op1=ALU.add, accum=cnt[:, :])
                P.ts("dve", c2[:, :], mid[:, :], 0.0, npad[:, :], ALU.is_le, ALU.mult)
                P.tt("dve", cnt[:, :], cnt[:, :], c2[:, :], ALU.subtract)
                P.ts("dve", ge[:, :], cnt[:, :], 256.0, None, ALU.is_ge)
                P.tt("dve", d1[:, :], mid[:, :], lo[:, :], ALU.subtract)
                P.tt("dve", d2[:, :], hi[:, :], mid[:, :], ALU.subtract)
                P.stt(lo[:, :], d1[:, :], ge[:, :], lo[:, :], ALU.mult, ALU.add)
                P.stt(hi[:, :], d2[:, :], ge[:, :], mid[:, :], ALU.mult, ALU.add)
            P.ts("dve", notsel[:, 0:NK], acc[:, 0:NK], lo[:, :], None, ALU.is_lt)
            for kc in range(nk):
                cs = slice(kc * 128, (kc + 1) * 128)
                for hg in range(2):
                    pl = pb[2 + kk % 2]; pT = pTs[kk % 3]; kk += 1
                    P.mm(pl[:, :], C.ckvT[:, cs], ql[:, 4 * hg:4 * hg + 4, :], start=True, stop=False)
                    P.mm(pl[:, :], notsel[:, cs], negI[:, :, :], start=False, stop=True)
                    P.act(pT[:, :], pl[:, :], AF.Exp, bias=padc[:, kc:kc + 1])
                    for h4 in range(4):
                        h = 4 * hg + h4
                        po = pb[4 + h // 3]
                        P.mm(po[:, (h % 3) * 129:(h % 3) * 129 + 129], pT[:, h4 * 128:(h4 + 1) * 128], C.ckv_tok[:, kc, :],
                             start=(kc == 0), stop=(kc == nk - 1))
            for h in range(8):
                po = pb[4 + h // 3]; o = (h % 3) * 129
                P.recip(rec[:, h:h + 1], po[:, o + 128:o + 129])
                P.ts("dve", ol[:, h, :], po[:, o:o + 128], rec[:, h:h + 1], None, ALU.mult)
            for h in range(8):
                P.tr(pb[7][:, (h % 4) * 128:(h % 4) * 128 + 128], ol[:, h, :], C.ident[:, :])
                P.cp("act", olT[:, h, :], pb[7][:, (h % 4) * 128:(h % 4) * 128 + 128])
            for m in range(4):
                ps = pb[kk % 2]; kk += 1
                P.mm(ps[:, 0:128], wuvp[:, 2 * m, :], olT[:, 2 * m, :], start=True, stop=False)
                P.mm(ps[:, 0:128], wuvp[:, 2 * m + 1, :], olT[:, 2 * m + 1, :], start=False, stop=True)
                P.cp("act", ab[:, m, :], ps[:, 0:128])
            P.dma("sp", V(C.brT, C.brT.h[0, :, qs].rearrange("(m p) q -> p m q", p=128)), ab[:, :, :])


def stage_merge_a(P, C):
    pb = C.pb
    with Stage(P):
        macc = P.sb("macc", [128, 8, TOWN])
        brt = P.sb("brt", [128, 4, TOWN], BF16)
        wbr = P.sb("wbr", [128, 4, D], BF16)
        wg = P.sb("wg", [128, 8, D], BF16)
        sg = [P.sb(f"sg{i}", [128, 512]) for i in range(2)]
        tm = [P.sb(f"mtm{i}", [128, 512]) for i in range(2)]
        k = 0
        for r in range(4):
            P.dma("sp", brt[:, :, :], V(C.brT, C.brT.h[r, :, :].rearrange("(kt k) t -> k kt t", k=128)))
            P.dma("pool", wbr[:, :, :], V(C.w_br, C.w_br.h[r, :, :].rearrange("(kt k) n -> k kt n", k=128)))
            P.dma("pool", wg[:, :, :], wview(C.w_in, C_GATE + r * D, D))
            for dt_ in range(8):
                ds_ = slice(dt_ * 128, (dt_ + 1) * 128)
                for tg in range(4):
                    ts_ = slice(tg * 512, (tg + 1) * 512)
                    pg = pb[k % 2]; pr = pb[2 + k % 2]; s = sg[k % 2]; t = tm[k % 2]; k += 1
                    for kt in range(8):
                        P.mm(pg[:, :], wg[:, kt, ds_], C.hTo[:, kt, ts_], start=(kt == 0), stop=(kt == 7))
                    for kt in range(4):
                        P.mm(pr[:, :], wbr[:, kt, ds_], brt[:, kt, ts_], start=(kt == 0), stop=(kt == 3))
                    P.act(s[:, :], pg[:, :], AF.Sigmoid)
                    if r == 0:
                        P.tt("dve", macc[:, dt_, ts_], s[:, :], pr[:, :], ALU.mult)
                    else:
                        P.tt("dve", t[:, :], s[:, :], pr[:, :], ALU.mult)
                        P.tt("pool", macc[:, dt_, ts_], macc[:, dt_, ts_], t[:, :], ALU.add)
        for dt_ in range(8):
            P.cp("act" if dt_ % 2 else "dve", brt[:, dt_ % 4, :], macc[:, dt_, :])
            P.dma("sp", C.mT_d[dt_ * 128:(dt_ + 1) * 128, :], brt[:, dt_ % 4, :])


def stage_merge_b(P, C):
    pb = C.pb
    with Stage(P):
        mT = P.sb("mTb", [128, 8, TOWN], BF16)
        P.dma("sp", mT[:, :, :], V(C.mT_d, C.mT_d.h[:, :].rearrange("(kt k) t -> k kt t", k=128)))
        wo = P.sb("wo", [128, 8, D], BF16)
        P.dma("pool", wo[:, :, :], V(C.w_o, C.w_o.h[:, :].rearrange("(kt k) n -> k kt n", k=128)))
        g1 = P.sb("g1", [128, D]); b1 = P.sb("b1", [128, D])
        P.dma("sp", g1[:, :], C.ln1g[:, :]); P.dma("sp", b1[:, :], C.ln1b[:, :])
        wr = P.sb("wr32", [128, 8, 32])
        P.dma("sp", wr[:, :, :], V(C.w_r, C.w_r.h[:, :].rearrange("(kt k) n -> k kt n", k=128)))
        wrh = P.sb("wrh", [128, 8, 32], BF16); wrl = P.sb("wrl", [128, 8, 32], BF16)
        P.cp("dve", wrh[:, :, :], wr[:, :, :])
        P.tt("dve", wrl[:, :, :], wr[:, :, :], wrh[:, :, :], ALU.subtract)
        brr = P.sb("brr", [128, 32]); P.dma("sp", brr[:, :], C.b_r[:, :])
        ho = [P.sb(f"ho{i}", [128, D]) for i in range(2)]
        xx = [P.sb(f"xx{i}", [128, D]) for i in range(2)]
        h1 = [P.sb(f"h1_{i}", [128, D]) for i in range(2)]
        h32 = [P.sb(f"h32_{i}", [128, 128], BF16) for i in range(3)]
        junk = P.sb("mjunk", [128, D])
        S = Small(P, "ln1")
        k = 0
        for tt_ in range(16):
            tsl = slice(tt_ * 128, (tt_ + 1) * 128)
            hot = ho[tt_ % 2]; x = xx[tt_ % 2]; h = h1[tt_ % 2]
            P.dma("sp", hot[:, :], C.hown[C.off + tt_ * 128:C.off + (tt_ + 1) * 128, :])
            for dh in range(2):
                ps = pb[dh]
                for kt in range(8):
                    P.mm(ps[:, :], mT[:, kt, tsl], wo[:, kt, dh * 512:(dh + 1) * 512], start=(kt == 0), stop=(kt == 7))
                P.stt(x[:, dh * 512:(dh + 1) * 512], hot[:, dh * 512:(dh + 1) * 512], ALPHA, ps[:, :], ALU.mult, ALU.add)
            ln_tok(P, x[:, :], h[:, :], g1[:, :], b1[:, :], S, junk[:, :])
            P.act(C.yacc[tt_][:, :], h[:, :], AF.Copy, scale=ALPHA)
            for kt in range(8):
                pt = pb[2 + k % 4]; hh = h32[k % 3]; k += 1
                P.tr(pt[:, 0:128], h[:, kt * 128:(kt + 1) * 128], C.ident[:, :])
                P.cp("act", C.h1T[:, kt, tsl], pt[:, 0:128])
                P.tt("dve", hh[:, :], pt[:, 0:128], C.h1T[:, kt, tsl], ALU.subtract)
                P.mm(pb[6][:, 0:32], C.h1T[:, kt, tsl], wrh[:, kt, :], start=(kt == 0), stop=False)
                P.mm(pb[6][:, 0:32], C.h1T[:, kt, tsl], wrl[:, kt, :], start=False, stop=False)
                P.mm(pb[6][:, 0:32], hh[:, :], wrh[:, kt, :], start=False, stop=(kt == 7))
            P.tt("dve", C.rlog[:, tt_, :], pb[6][:, 0:32], brr[:, :], ALU.add)


def emit_hT(P, C, o, dst, t0, k):
    if not hasattr(C, "tst") or C.tst_owner is not P.es:
        C.tst = [P.sb(f"tst{i}", [128, 8, 128]) for i in range(2)]
        C.tst_owner = P.es
    st = C.tst[k % 2]
    for kt in range(8):
        ps = C.pb[(kt // 4) + 2 * (k % 2)]
        P.tr(ps[:, (kt % 4) * 128:(kt % 4) * 128 + 128], o[:, kt * 128:(kt + 1) * 128], C.ident[:, :])
    for half in range(2):
        ps = C.pb[half + 2 * (k % 2)]
        P.cp("act" if half else "dve", st[:, 4 * half:4 * half + 4, :], ps[:, :])
    P.dma("sp", V(dst, dst.h[:, PRE + t0:PRE + t0 + 128].rearrange("(kt k) t -> k kt t", k=128)), st[:, :, :])


def stage_moe(P, C):
    pb = C.pb
    with Stage(P):
        S = Small(P, "moe")
        gates = P.sb("gates", [128, 16, 32])
        gT = P.sb("gT", [32, TOWN], BF16)
        bdn = P.sb("bdn", [32, D], BF16); P.dma("pool", bdn[:, :], C.b_dn[:, :])
        bup = P.sb("bup", [128, 32, 8, 2]); P.dma("sp", bup[:, :, :, :], C.b_up[:, :, :, :])
        top8 = P.sb("top8", [128, 8]); nmx = S.col(); ssum = S.col(); ee = P.sb("ree", [128, 32]); mk = P.sb("rmk", [128, 32])
        for tt_ in range(16):
            lg = C.rlog[:, tt_, :]
            P.max8(top8[:, :], lg)
            P.ts("dve", nmx[:, :], top8[:, 0:1], -1.0, None, ALU.mult)
            P.act(ee[:, :], lg, AF.Exp, bias=nmx[:, :])
            P.ts("dve", mk[:, :], lg, top8[:, 3:4], None, ALU.is_ge)
            P.tt("dve", ee[:, :], ee[:, :], mk[:, :], ALU.mult)
            P.red(ssum[:, :], ee[:, :], ALU.add)
            P.recip(ssum[:, :], ssum[:, :])
            P.ts("dve", gates[:, tt_, :], ee[:, :], ssum[:, :], None, ALU.mult)
            P.tr(pb[7][0:32, 0:128], gates[:, tt_, :], C.ident[:, :])
            P.cp("act", gT[0:32, tt_ * 128:(tt_ + 1) * 128], pb[7][0:32, 0:128])
        for tt_ in range(16):
            for dh in range(2):
                ps = pb[dh]
                P.mm(ps[:, :], gT[0:32, tt_ * 128:(tt_ + 1) * 128], bdn[0:32, dh * 512:(dh + 1) * 512])
                P.tt("dve", C.yacc[tt_][:, dh * 512:(dh + 1) * 512], C.yacc[tt_][:, dh * 512:(dh + 1) * 512], ps[:, :], ALU.add)
        with Stage(P):
            wup = [P.sb(f"wup{i}", [128, 8, 1024], BF16) for i in range(2)]
            wdn = [P.sb(f"wdn{i}", [128, 4, D], BF16) for i in range(2)]
            actT = P.sb("actT", [128, 4, TOWN], BF16)
            gg = [P.sb(f"gg{i}", [128, 512]) for i in range(2)]
            ll = [P.sb(f"ll{i}", [128, 512]) for i in range(2)]
            sgm = [P.sb(f"sgm{i}", [128, 512]) for i in range(2)]
            k = 0; kd = 0
            for e in range(32):
                for hf in range(2):
                    u = (2 * e + hf) % 2
                    wu = wup[u]; wd = wdn[u]
                    P.dma("pool", wu[:, :, :], V(C.w_up, C.w_up.h[e * D:(e + 1) * D, hf * 1024:(hf + 1) * 1024].rearrange("(kt k) n -> k kt n", k=128)))
                    P.dma("pool", wd[:, :, :], V(C.w_dn, C.w_dn.h[e * D + hf * 512:e * D + (hf + 1) * 512, :].rearrange("(ft f) n -> f ft n", f=128)))
                    for f4 in range(4):
                        ft = hf * 4 + f4
                        for tg in range(4):
                            ts_ = slice(tg * 512, (tg + 1) * 512)
                            pg = pb[(2 * k) % 6]; pl = pb[(2 * k) % 6 + 1]
                            g = gg[k % 2]; l = ll[k % 2]; s = sgm[k % 2]; k += 1
                            for kt in range(8):
                                P.mm(pg[:, :], wu[:, kt, f4 * 256:f4 * 256 + 256:2], C.h1T[:, kt, ts_], start=(kt == 0), stop=(kt == 7))
                            for kt in range(8):
                                P.mm(pl[:, :], wu[:, kt, f4 * 256 + 1:f4 * 256 + 256:2], C.h1T[:, kt, ts_], start=(kt == 0), stop=(kt == 7))
                            P.ts("dve", g[:, :], pg[:, :], bup[:, e, ft, 0:1], 7.0, ALU.add, ALU.min)
                            P.act(s[:, :], g[:, :], AF.Sigmoid, scale=1.702)
                            P.ts("dve", l[:, :], pl[:, :], bup[:, e, ft, 1:2], 7.0, ALU.add, ALU.min)
                            P.ts("dve", l[:, :], l[:, :], -7.0, 1.0, ALU.max, ALU.add)
                            P.tt("pool", g[:, :], g[:, :], s[:, :], ALU.mult)
                            P.tt("dve", actT[:, f4, ts_], g[:, :], l[:, :], ALU.mult)
                    for tt_ in range(16):
                        tsl = slice(tt_ * 128, (tt_ + 1) * 128)
                        for dh in range(2):
                            pd = pb[6 + kd % 2]; kd += 1
                            for f4 in range(4):
                                P.mm(pd[:, :], actT[:, f4, tsl], wd[:, f4, dh * 512:(dh + 1) * 512], start=(f4 == 0), stop=(f4 == 3))
                            P.stt(C.yacc[tt_][:, dh * 512:(dh + 1) * 512], pd[:, :], gates[:, tt_, e:e + 1],
                                  C.yacc[tt_][:, dh * 512:(dh + 1) * 512], ALU.mult, ALU.add)
        g2 = P.sb("g2", [128, D]); b2 = P.sb("b2", [128, D])
        P.dma("sp", g2[:, :], C.ln2g[:, :]); P.dma("sp", b2[:, :], C.ln2b[:, :])
        junk = P.sb("ojunk", [128, D])
        oo = [P.sb(f"oo{i}", [128, D]) for i in range(2)]
        for tt_ in range(16):
            o = oo[tt_ % 2]
            ln_tok(P, C.yacc[tt_][:, :], o[:, :], g2[:, :], b2[:, :], S, junk[:, :])
            P.dma("sp", C.out[C.off + tt_ * 128:C.off + (tt_ + 1) * 128, :], o[:, :])
            if getattr(C, "hT_next", None) is not None:
                emit_hT(P, C, o, C.hT_next, C.off + tt_ * 128, tt_)


STAGES_ALL = ("window", "ssm", "conv", "mem", "attproj", "att", "merge", "moe")
NQ = 4


def build_layer(stages=STAGES_ALL, dbg=(), quarters=(0, 1, 2, 3)):
    nc = bass.Bass("TRN2", target_bir_lowering=False)
    es = ExitStack()
    P = Prog(nc, es)
    C = Ctx()

    def inp(name, shape, dt=F32):
        t = P.dram(name, shape, dt, kind="ExternalInput")
        setattr(C, name, t)
        return t

    inp("hT", [D, PRE + T]); inp("hown", [T, D]); inp("npad", [4, 128, 1]); inp("padcol", [4, 128, 64])
    inp("memT", [D, 256]); inp("identin", [128, 128]); inp("tri", [128, 128])
    inp("w_in", [D, D_IN]); inp("kvg", [128, 1]); inp("kvgrow", [128, 128])
    inp("w_uk", [128, 512]); inp("w_uv", [128, 512]); inp("convw", [128, 4, 3]); inp("convb", [128, 4])
    inp("lamre", [128, 16]); inp("lamim", [128, 16]); inp("logdt", [128, 16])
    inp("bre", [128, 16, 16]); inp("bim", [128, 16, 16]); inp("cre", [128, 16, 16]); inp("cim", [128, 16, 16])
    inp("dskip", [128, 4]); inp("w_glu", [512, 512]); inp("bglu", [128, 4])
    inp("w_mem", [D, D]); inp("w_br", [4, 512, D]); inp("w_o", [D, D])
    inp("ln1g", [128, D]); inp("ln1b", [128, D]); inp("ln2g", [128, D]); inp("ln2b", [128, D])
    inp("w_r", [D, 32]); inp("b_r", [128, 32])
    inp("w_up", [32 * D, 2048]); inp("b_up", [128, 32, 8, 2]); inp("w_dn", [32 * D, D]); inp("b_dn", [32, D])
    C.out = P.dram("out", [T, D], F32, kind="ExternalOutput")

    def scratch(name, shape, dt):
        kind = "ExternalOutput" if name in dbg else "Internal"
        t = P.dram(name, shape, dt, kind=kind)
        setattr(C, name, t)
        return t

    scratch("uT", [512, WIN], F32); scratch("z32", [512, TOWN], F32); scratch("brT", [4, 512, TOWN], BF16)
    scratch("mT_d", [D, TOWN], BF16); scratch("qlat_d", [128, 8, TOWN], BF16); scratch("qidx_d", [32, 8, TOWN], BF16)

    C.pb = [P.ps(f"pb{i}", [128, 512], F32) for i in range(8)]
    C.ident = P.sb("ident", [128, 128]); P.dma("sp", C.ident[:, :], C.identin[:, :])
    C.ones_bf = P.sb("ones_bf", [128, 128], BF16); P.memset("dve", C.ones_bf[:, :], 1.0)

    for j in quarters:
        C.j = j
        C.off = j * TOWN
        with Stage(P):
            C.ckvT = P.sb(f"ckvT{j}", [128, WIN], BF16)
            C.ckv_tok = P.sb(f"ckv_tok{j}", [128, 64, 129], BF16)
            C.kidxT = P.sb(f"kidxT{j}", [32, WIN], BF16)
            C.widx = P.sb(f"widx{j}", [128, 16, 8])
            P.memset("dve", C.ckv_tok[:, :, 128:129], 1.0)
            if "window" in stages:
                stage_window(P, C)
            if "ssm" in stages:
                stage_ssm(P, C)
            with Stage(P):
                C.hTo = P.sb(f"hTo{j}", [128, 8, TOWN], BF16)
                P.dma("pool", C.hTo[:, :, :], V(C.hT, C.hT.h[:, C.off + PRE:C.off + WIN].rearrange("(kt k) t -> k kt t", k=128)))
                if "conv" in stages:
                    stage_conv(P, C)
                if "mem" in stages:
                    stage_mem(P, C)
                if "attproj" in stages:
                    stage_attproj(P, C)
            if "att" in stages:
                stage_att(P, C)
        with Stage(P):
            C.hTo = P.sb(f"hTo2{j}", [128, 8, TOWN], BF16)
            P.dma("pool", C.hTo[:, :, :], V(C.hT, C.hT.h[:, C.off + PRE:C.off + WIN].rearrange("(kt k) t -> k kt t", k=128)))
            if "merge" in stages or "merge_a" in stages:
                stage_merge_a(P, C)
        with Stage(P):
            C.yacc = [P.sb(f"yacc{j}_{i}", [128, D]) for i in range(16)]
            C.h1T = P.sb(f"h1T{j}", [128, 8, TOWN], BF16)
            C.rlog = P.sb(f"rlog{j}", [128, 16, 32])
            if "merge" in stages or "merge_b" in stages:
                stage_merge_b(P, C)
            if "moe" in stages:
                stage_moe(P, C)
    P.finish("sp", [C.out] + [getattr(C, n) for n in dbg])
    print("layer program instructions:", P.ninst, flush=True)
    es.close()
    return nc


def _rep(v, rows=128):
    return np.ascontiguousarray(np.broadcast_to(np.asarray(v, np.float32).reshape(1, -1), (rows, np.size(v))))


def _sm(a):
    a = np.asarray(a, np.float32)
    rest = a.shape[2:]
    return np.ascontiguousarray(a.reshape((16, 128) + rest).swapaxes(0, 1))


def layer_weights(inp, l):
    f = lambda k: np.asarray(inp[k][l], np.float32)
    w = {}
    w["w_in"] = np.ascontiguousarray(f("w_in"))
    w["kvg"] = np.ascontiguousarray(f("kv_norm_g").reshape(128, 1))
    w["kvgrow"] = _rep(f("kv_norm_g"))
    w["w_uk"] = np.ascontiguousarray(f("w_uk").reshape(128, 512))
    w["w_uv"] = np.ascontiguousarray(f("w_uv").reshape(128, 512))
    w["convw"] = np.ascontiguousarray(f("conv_w").T.reshape(4, 128, 3).transpose(1, 0, 2))
    w["convb"] = np.ascontiguousarray(f("conv_b").reshape(4, 128).T)
    w["lamre"] = _sm(f("lam_re")); w["lamim"] = _sm(f("lam_im"))
    w["logdt"] = _sm(np.broadcast_to(f("log_dt")[:, None], (32, 64)))
    w["bre"] = _sm(f("b_re")); w["bim"] = _sm(f("b_im"))
    w["cre"] = _sm(f("c_re").transpose(0, 2, 1)); w["cim"] = _sm(f("c_im").transpose(0, 2, 1))
    w["dskip"] = np.ascontiguousarray(f("d_skip").reshape(4, 128).T)
    w["w_glu"] = np.ascontiguousarray(f("w_glu"))
    w["bglu"] = np.ascontiguousarray(f("b_glu").reshape(4, 128).T)
    w["w_mem"] = np.ascontiguousarray(f("w_mem_kv"))
    w["w_br"] = np.ascontiguousarray(f("w_branch"))
    w["w_o"] = np.ascontiguousarray(f("w_o"))
    w["ln1g"] = _rep(f("ln1_g")); w["ln1b"] = _rep(f("ln1_b"))
    w["ln2g"] = _rep(f("ln2_g")); w["ln2b"] = _rep(f("ln2_b"))
    w["w_r"] = np.ascontiguousarray(f("w_router"))
    w["b_r"] = _rep(f("b_router"))
    w["w_up"] = np.ascontiguousarray(f("w_up").reshape(32 * D, 2048))
    w["b_up"] = np.ascontiguousarray(f("b_up").reshape(32, 8, 128, 2).transpose(2, 0, 1, 3))
    w["w_dn"] = np.ascontiguousarray(f("w_down").reshape(32 * D, D))
    w["b_dn"] = np.ascontiguousarray(f("b_down"))
    return w


def batch_inputs(h, mem, b):
    d = {}
    win = np.zeros((PRE + T, D), np.float32)
    win[PRE:] = h[b]
    d["hT"] = np.ascontiguousarray(win.T)
    d["hown"] = np.ascontiguousarray(h[b])
    npad = np.zeros((4, 128, 1), np.float32); padcol = np.zeros((4, 128, 64), np.float32)
    kidx = (np.arange(64)[None, :] * 128 + np.arange(128)[:, None])
    for j in range(4):
        pad = PRE - j * TOWN
        npad[j] = float(pad)
        padcol[j] = np.where(kidx < pad, NEG, 0.0)
    d["npad"] = npad; d["padcol"] = padcol
    d["memT"] = np.ascontiguousarray(np.asarray(mem[b], np.float32).T)
    d["identin"] = np.eye(128, dtype=np.float32)
    d["tri"] = np.where(np.arange(128)[None, :] > np.arange(128)[:, None], -BIG, 0.0).astype(np.float32)
    return d


def core_inputs(h, mem, c):
    b, j = c // 4, c % 4
    t0 = j * TOWN
    pad = PRE - t0
    win = np.zeros((WIN, D), np.float32)
    win[pad:] = h[b, 0:t0 + TOWN]
    d = {}
    d["hT"] = np.ascontiguousarray(win.T)
    d["hown"] = np.ascontiguousarray(h[b, t0:t0 + TOWN])
    d["npad"] = np.full((128, 1), float(pad), np.float32)
    kidx = (np.arange(64)[None, :] * 128 + np.arange(128)[:, None])
    d["padcol"] = np.where(kidx < pad, NEG, 0.0).astype(np.float32)
    d["memT"] = np.ascontiguousarray(np.asarray(mem[b], np.float32).T)
    d["identin"] = np.eye(128, dtype=np.float32)
    d["tri"] = np.where(np.arange(128)[None, :] > np.arange(128)[:, None], -BIG, 0.0).astype(np.float32)
    return d


_NC_CACHE = {}

W_NAMES = ("w_in", "kvg", "kvgrow", "w_uk", "w_uv", "convw", "convb", "lamre", "lamim", "logdt", "bre", "bim",
           "cre", "cim", "dskip", "w_glu", "bglu", "w_mem", "w_br", "w_o", "ln1g", "ln1b", "ln2g", "ln2b",
           "w_r", "b_r", "w_up", "b_up", "w_dn", "b_dn")
W_SHAPES = {"w_in": [D, D_IN], "kvg": [128, 1], "kvgrow": [128, 128], "w_uk": [128, 512], "w_uv": [128, 512],
            "convw": [128, 4, 3], "convb": [128, 4], "lamre": [128, 16], "lamim": [128, 16], "logdt": [128, 16],
            "bre": [128, 16, 16], "bim": [128, 16, 16], "cre": [128, 16, 16], "cim": [128, 16, 16],
            "dskip": [128, 4], "w_glu": [512, 512], "bglu": [128, 4], "w_mem": [D, D], "w_br": [4, 512, D],
            "w_o": [D, D], "ln1g": [128, D], "ln1b": [128, D], "ln2g": [128, D], "ln2b": [128, D],
            "w_r": [D, 32], "b_r": [128, 32], "w_up": [32 * D, 2048], "b_up": [128, 32, 8, 2],
            "w_dn": [32 * D, D], "b_dn": [32, D]}


def build_fused(depth=DEPTH, quarters=(0, 1, 2, 3)):
    nc = bass.Bass("TRN2", target_bir_lowering=False)
    es = ExitStack()
    P = Prog(nc, es)
    C = Ctx()
    x = P.dram("x", [T, D], F32, kind="ExternalInput")
    lng = P.dram("lng", [128, D], F32, kind="ExternalInput"); lnb = P.dram("lnb", [128, D], F32, kind="ExternalInput")
    C.npad = P.dram("npad", [4, 128, 1], F32, kind="ExternalInput")
    C.padcol = P.dram("padcol", [4, 128, 64], F32, kind="ExternalInput")
    C.memT = P.dram("memT", [D, 256], F32, kind="ExternalInput")
    C.identin = P.dram("identin", [128, 128], F32, kind="ExternalInput")
    C.tri = P.dram("tri", [128, 128], F32, kind="ExternalInput")
    WL = [{n: P.dram(f"{n}_{l}", W_SHAPES[n], F32, kind="ExternalInput") for n in W_NAMES} for l in range(depth)]
    out = P.dram("out", [T, D], F32, kind="ExternalOutput")
    hTb = [P.dram(f"hTbuf{i}", [D, PRE + T], F32) for i in range(2)]
    hb = [P.dram(f"hbuf{i}", [T, D], F32) for i in range(2)]
    for n, shp, dt in (("uT", [512, WIN], F32), ("z32", [512, TOWN], F32), ("brT", [4, 512, TOWN], BF16),
                       ("mT_d", [D, TOWN], BF16), ("qlat_d", [128, 8, TOWN], BF16), ("qidx_d", [32, 8, TOWN], BF16)):
        setattr(C, n, P.dram(n, shp, dt))
    C.pb = [P.ps(f"pb{i}", [128, 512], F32) for i in range(8)]
    C.ident = P.sb("ident", [128, 128]); P.dma("sp", C.ident[:, :], C.identin[:, :])
    C.ones_bf = P.sb("ones_bf", [128, 128], BF16); P.memset("dve", C.ones_bf[:, :], 1.0)
    with Stage(P):
        zt = P.sb("zt", [128, 2048]); P.memset("dve", zt[:, :], 0.0)
        for i in range(2):
            for r in range(8):
                for c in range(PRE // 2048):
                    P.dma("sp", hTb[i][r * 128:(r + 1) * 128, c * 2048:(c + 1) * 2048], zt[:, :])
        gs = P.sb("gs", [128, D]); bs = P.sb("bs", [128, D])
        P.dma("sp", gs[:, :], lng[:, :]); P.dma("sp", bs[:, :], lnb[:, :])
        S = Small(P, "lnin")
        junk = P.sb("junk", [128, D])
        xs = [P.sb(f"x{i}", [128, D]) for i in range(2)]
        os_ = [P.sb(f"o{i}", [128, D]) for i in range(2)]
        for i in range(T // 128):
            xt = xs[i % 2]; ot = os_[i % 2]
            P.dma("sp", xt[:, :], x[i * 128:(i + 1) * 128, :])
            ln_tok(P, xt[:, :], ot[:, :], gs[:, :], bs[:, :], S, junk[:, :])
            P.dma("sp", hb[0][i * 128:(i + 1) * 128, :], ot[:, :])
            emit_hT(P, C, ot, hTb[0], i * 128, i)
    for l in range(depth):
        for n in W_NAMES:
            setattr(C, n, WL[l][n])
        C.hT = hTb[l % 2]; C.hown = hb[l % 2]
        last = (l == depth - 1)
        C.out = out if last else hb[(l + 1) % 2]
        C.hT_next = None if last else hTb[(l + 1) % 2]
        for j in quarters:
            C.j = j
            C.off = j * TOWN
            with Stage(P):
                C.ckvT = P.sb("ckvT", [128, WIN], BF16)
                C.ckv_tok = P.sb("ckv_tok", [128, 64, 129], BF16)
                C.kidxT = P.sb("kidxT", [32, WIN], BF16)
                C.widx = P.sb("widx", [128, 16, 8])
                P.memset("dve", C.ckv_tok[:, :, 128:129], 1.0)
                stage_window(P, C)
                stage_ssm(P, C)
                with Stage(P):
                    C.hTo = P.sb("hTo", [128, 8, TOWN], BF16)
                    P.dma("pool", C.hTo[:, :, :], V(C.hT, C.hT.h[:, C.off + PRE:C.off + WIN].rearrange("(kt k) t -> k kt t", k=128)))
                    stage_conv(P, C)
                    stage_mem(P, C)
                    stage_attproj(P, C)
                stage_att(P, C)
            with Stage(P):
                C.hTo = P.sb("hTo2", [128, 8, TOWN], BF16)
                P.dma("pool", C.hTo[:, :, :], V(C.hT, C.hT.h[:, C.off + PRE:C.off + WIN].rearrange("(kt k) t -> k kt t", k=128)))
                stage_merge_a(P, C)
            with Stage(P):
                C.yacc = [P.sb(f"yacc{i}", [128, D]) for i in range(16)]
                C.h1T = P.sb("h1T", [128, 8, TOWN], BF16)
                C.rlog = P.sb("rlog", [128, 16, 32])
                stage_merge_b(P, C)
                stage_moe(P, C)
    P.finish("sp", [out])
    print("fused program instructions:", P.ninst, flush=True)
    es.close()
    return nc


def fused_inputs(inputs, b):
    x = np.asarray(inputs["x"], np.float32)
    d = {"x": np.ascontiguousarray(x[b]), "lng": _rep(inputs["ln_in_g"]), "lnb": _rep(inputs["ln_in_b"])}
    npad = np.zeros((4, 128, 1), np.float32); padcol = np.zeros((4, 128, 64), np.float32)
    kidx = (np.arange(64)[None, :] * 128 + np.arange(128)[:, None])
    for j in range(4):
        pad = PRE - j * TOWN
        npad[j] = float(pad)
        padcol[j] = np.where(kidx < pad, NEG, 0.0)
    d["npad"] = npad; d["padcol"] = padcol
    d["memT"] = np.ascontiguousarray(np.asarray(inputs["mem"], np.float32)[b].T)
    d["identin"] = np.eye(128, dtype=np.float32)
    d["tri"] = np.where(np.arange(128)[None, :] > np.arange(128)[:, None], -BIG, 0.0).astype(np.float32)
    return d


def kernel(**inputs):
    if "fused" not in _NC_CACHE:
        _NC_CACHE["fused"] = build_fused()
    maps = [fused_inputs(inputs, b) for b in range(2)]
    for l in range(DEPTH):
        w = layer_weights(inputs, l)
        for n in W_NAMES:
            for m in maps:
                m[f"{n}_{l}"] = w[n]
    res = run_bass_kernel_spmd(_NC_CACHE["fused"], maps, core_ids=[0, 1])
    return np.stack([np.asarray(r["out"]) for r in res.results]).astype(np.float32)
```

```python
from contextlib import ExitStack
import math
import numpy as np
import concourse.bass as bass
import concourse.mybir as mybir
from concourse.bass_utils import run_bass_kernel_spmd

F32 = mybir.dt.float32
BF16 = mybir.dt.bfloat16
ALU = mybir.AluOpType
AF = mybir.ActivationFunctionType
AX = mybir.AxisListType

NCORES = 8
D = 1024
T = 8192
TOWN = 2048
WIN = 8192
PRE = WIN - TOWN
DEPTH = 4
D_IN = 7592
C_Q, C_CKV, C_QI, C_KI, C_WI, C_CU, C_GB, C_GC, C_SU, C_MQ, C_GATE = (
    0, 512, 640, 896, 928, 936, 1448, 1960, 2472, 2984, 3496)
LN_EPS = 1e-5
ALPHA = (2 * DEPTH) ** 0.25
NEG = -30000.0
BIG = 1.0e30
TWO_PI = 2.0 * math.pi
MAGIC = 12582912.0
NBIS = 17


class V:
    def __init__(self, t, ap):
        self.t = t
        self.ap = ap


class TT:
    def __init__(self, h, name):
        self.h = h
        self.name = name
        self.w = None
        self.r = []

    def __getitem__(self, idx):
        return V(self, self.h[idx])


def _ap(x):
    return x.ap if isinstance(x, V) else x


def _ts(*xs):
    return [x.t for x in xs if isinstance(x, V)]


class Prog:
    def __init__(self, nc, es):
        self.nc = nc
        self.es = es
        self.eng = {"pe": nc.tensor, "act": nc.scalar, "dve": nc.vector,
                    "pool": nc.gpsimd, "sp": nc.sync}
        self.sem = {}
        self.cnt = {}
        self.waited = {e: {} for e in self.eng}
        for e in self.eng:
            self.sem[e] = es.enter_context(nc.semaphore("s_" + e))
            self.cnt[e] = 0
        self.ninst = 0
        self.uid = 0

    def sb(self, name, shape, dt=F32):
        self.uid += 1
        return TT(self.es.enter_context(self.nc.sbuf_tensor(f"{name}_u{self.uid}", shape, dt)), name)

    def ps(self, name, shape, dt=F32):
        return TT(self.es.enter_context(self.nc.psum_tensor(name, shape, dt)), name)

    def dram(self, name, shape, dt=F32, kind="Internal"):
        return TT(self.nc.dram_tensor(name, shape, dt, kind=kind), name)

    def dsem(self, name):
        key = "d_" + name
        if key not in self.sem:
            self.sem[key] = self.es.enter_context(self.nc.semaphore(key))
            self.cnt[key] = 0
        return key

    def _wait(self, e, deps):
        need = {}
        for d in deps:
            if d is None:
                continue
            k, v = d
            if k == e and e == "pe":
                continue
            if v > need.get(k, 0):
                need[k] = v
        for k, v in need.items():
            if self.waited[e].get(k, 0) >= v:
                continue
            self.eng[e].wait_ge(self.sem[k], v)
            self.waited[e][k] = v

    def _deps(self, reads, writes):
        deps = []
        for t in reads:
            deps.append(t.w)
        for t in writes:
            deps.append(t.w)
            deps.extend(t.r)
        return deps

    def op(self, e, fn, reads=(), writes=()):
        self._wait(e, self._deps(reads, writes))
        ins = fn(self.eng[e])
        self.cnt[e] += 1
        ins.then_inc(self.sem[e], 1)
        d = (e, self.cnt[e])
        for t in reads:
            t.r.append(d)
        for t in writes:
            t.w = d
            t.r = []
        self.ninst += 1
        return d

    def dma(self, q, out, in_, sem=None, **kw):
        self._wait(q, self._deps([in_.t], [out.t]))
        key = self.dsem(sem or out.t.name)
        ins = self.eng[q].dma_start(out=out.ap, in_=in_.ap, **kw)
        self.cnt[key] += 16
        ins.then_inc(self.sem[key], 16)
        d = (key, self.cnt[key])
        in_.t.r.append(d)
        out.t.w = d
        out.t.r = []
        self.ninst += 1
        return d

    def allgather(self, out, in_, n=NCORES):
        self._wait("pool", self._deps([in_.t], [out.t]))
        key = self.dsem(out.t.name)
        ins = self.nc.gpsimd.collective_compute("AllGather", op=ALU.bypass, replica_groups=[list(range(n))],
                                                ins=[in_.ap], outs=[out.ap])
        self.cnt[key] += 16
        ins.then_inc(self.sem[key], 16)
        d = (key, self.cnt[key])
        in_.t.r.append(d)
        out.t.w = d
        out.t.r = []
        self.ninst += 1
        return d

    def finish(self, e, tensors):
        self._wait(e, [t.w for t in tensors])

    def mm(self, out, lhsT, rhs, start=True, stop=True):
        return self.op("pe", lambda e: e.matmul(out.ap, lhsT.ap, rhs.ap, start=start, stop=stop),
                       reads=[lhsT.t, rhs.t], writes=[out.t])

    def tr(self, out, in_, ident):
        return self.op("pe", lambda e: e.transpose(out.ap, in_.ap, ident.ap),
                       reads=[in_.t, ident.t], writes=[out.t])

    def act(self, out, in_, func, bias=0.0, scale=1.0, accum=None, e="act"):
        rd = _ts(in_, bias, scale)
        wr = _ts(out, accum)
        kw = {}
        if accum is not None:
            kw["accum_out"] = accum.ap
        return self.op("act", lambda g: g.activation(out=out.ap, in_=in_.ap, func=func,
                                                     bias=_ap(bias), scale=_ap(scale), **kw),
                       reads=rd, writes=wr)

    def ts(self, e, out, in0, s1, s2, op0, op1=None, accum=None):
        rd = _ts(in0, s1, s2)
        wr = _ts(out, accum)
        kw = {}
        if op1 is not None:
            kw["op1"] = op1
        if accum is not None:
            kw["accum_out"] = accum.ap
        return self.op(e, lambda g: g.tensor_scalar(out.ap, in0.ap, _ap(s1), _ap(s2), op0, **kw),
                       reads=rd, writes=wr)

    def tt(self, e, out, in0, in1, op):
        return self.op(e, lambda g: g.tensor_tensor(out.ap, in0.ap, in1.ap, op),
                       reads=_ts(in0, in1), writes=_ts(out))

    def stt(self, out, in0, s, in1, op0, op1):
        return self.op("dve", lambda g: g.scalar_tensor_tensor(out.ap, in0.ap, _ap(s), in1.ap, op0, op1),
                       reads=_ts(in0, s, in1), writes=_ts(out))

    def cp(self, e, out, in_):
        if e == "act":
            return self.act(out, in_, AF.Copy)
        return self.op(e, lambda g: g.tensor_copy(out.ap, in_.ap), reads=_ts(in_), writes=_ts(out))

    def red(self, out, in_, op, axis=AX.X):
        return self.op("dve", lambda g: g.tensor_reduce(out.ap, in_.ap, axis, op),
                       reads=_ts(in_), writes=_ts(out))

    def scan(self, out, d0, d1, init, op0, op1):
        return self.op("dve", lambda g: g.tensor_tensor_scan(out.ap, d0.ap, d1.ap, _ap(init), op0, op1),
                       reads=_ts(d0, d1, init), writes=_ts(out))

    def recip(self, out, in_):
        return self.op("dve", lambda g: g.reciprocal(out.ap, in_.ap), reads=_ts(in_), writes=_ts(out))

    def memset(self, e, out, val):
        return self.op(e, lambda g: g.memset(out.ap, val), reads=[], writes=_ts(out))

    def max8(self, out, in_):
        return self.op("dve", lambda g: g.max(out.ap, in_.ap), reads=_ts(in_), writes=_ts(out))


class Small:
    def __init__(self, P, name, n=64):
        self.P = P
        self.name = name
        self.k = 0

    def col(self, w=1):
        self.k += 1
        return self.P.sb(f"{self.name}_{self.k}", [128, w], F32)


def ln_tok(P, x, out, grep, brep, S, junk):
    msum = S.col(); negmean = S.col(); ss = S.col(); std = S.col(); rstd = S.col()
    P.red(msum[:, :], x, ALU.add)
    P.ts("dve", negmean[:, :], msum[:, :], -1.0 / D, None, ALU.mult)
    P.act(junk, x, AF.Square, bias=negmean[:, :], scale=1.0, accum=ss[:, :])
    P.ts("dve", std[:, :], ss[:, :], 1.0 / D, LN_EPS, ALU.mult, ALU.add)
    P.act(std[:, :], std[:, :], AF.Sqrt)
    P.recip(rstd[:, :], std[:, :])
    P.ts("dve", out, x, negmean[:, :], rstd[:, :], ALU.add, ALU.mult)
    P.tt("dve", out, out, grep, ALU.mult)
    P.tt("dve", out, out, brep, ALU.add)


def build_ln_in():
    nc = bass.Bass("TRN2", target_bir_lowering=False)
    es = ExitStack()
    P = Prog(nc, es)
    x = P.dram("x", [TOWN, D], F32, kind="ExternalInput")
    g = P.dram("g", [128, D], F32, kind="ExternalInput")
    b = P.dram("b", [128, D], F32, kind="ExternalInput")
    y = P.dram("y", [TOWN, D], F32, kind="ExternalOutput")
    gs = P.sb("gs", [128, D]); bs = P.sb("bs", [128, D])
    P.dma("sp", gs[:, :], g[:, :]); P.dma("sp", bs[:, :], b[:, :])
    S = Small(P, "lnin")
    junk = P.sb("junk", [128, D])
    xs = [P.sb(f"x{i}", [128, D]) for i in range(2)]
    os_ = [P.sb(f"o{i}", [128, D]) for i in range(2)]
    for i in range(TOWN // 128):
        xt = xs[i % 2]; ot = os_[i % 2]
        P.dma("sp", xt[:, :], x[i * 128:(i + 1) * 128, :])
        ln_tok(P, xt[:, :], ot[:, :], gs[:, :], bs[:, :], S, junk[:, :])
        P.dma("sp", y[i * 128:(i + 1) * 128, :], ot[:, :])
    P.finish("sp", [y])
    es.close()
    return nc


class Ctx:
    pass


def barrier(P):
    allk = [(k, v) for k, v in P.cnt.items() if v > 0]
    for e in P.eng:
        P._wait(e, allk)


class Stage:
    def __init__(self, P):
        self.P = P

    def __enter__(self):
        self.old = self.P.es
        self.es = ExitStack()
        self.P.es = self.es
        return self

    def __exit__(self, *a):
        barrier(self.P)
        self.P.es = self.old
        self.es.close()
        return False


def load_w(P, dst, src_ap_tt, q="pool"):
    return P.dma(q, dst, src_ap_tt)


def wview(w_in, c0, n):
    return V(w_in, w_in.h[:, c0:c0 + n].rearrange("(kt k) n -> k kt n", k=128))


def stage_window(P, C):
    pb = C.pb
    with Stage(P):
        wsu = P.sb("wsu", [128, 8, 512], BF16)
        wck = P.sb("wck", [128, 8, 128], BF16)
        wki = P.sb("wki", [128, 8, 32], BF16)
        P.dma("pool", wsu[:, :, :], wview(C.w_in, C_SU, 512))
        P.dma("pool", wck[:, :, :], wview(C.w_in, C_CKV, 128))
        P.dma("pool", wki[:, :, :], wview(C.w_in, C_KI, 32))
        kvg = P.sb("kvg_s", [128, 1]); P.dma("sp", kvg[:, :], C.kvg[:, :])
        kvgrow = P.sb("kvgrow_s", [128, 128]); P.dma("sp", kvgrow[:, :], C.kvgrow[:, :])
        hTc = [P.sb(f"hTc{i}", [128, 8, 512], BF16) for i in range(2)]
        ust = [P.sb(f"ust{i}", [128, 512]) for i in range(2)]
        sq = P.sb("sq", [128, 512], BF16)
        rst = P.sb("rst", [128, 512])
        junk = P.sb("wjunk", [128, 128])
        S = Small(P, "win")
        for g in range(C.pad // 512, WIN // 512):
            h = hTc[g % 2]
            P.dma("pool", h[:, :, :], V(C.hT, C.hT.h[:, C.off + g * 512:C.off + (g + 1) * 512].rearrange("(kt k) t -> k kt t", k=128)))
            for ct in range(4):
                ps = pb[ct % 2]
                for kt in range(8):
                    P.mm(ps[:, :], wsu[:, kt, ct * 128:(ct + 1) * 128], h[:, kt, :], start=(kt == 0), stop=(kt == 7))
                u = ust[ct % 2]
                P.cp("act", u[:, :], ps[:, :])
                P.dma("sp", C.uT[ct * 128:(ct + 1) * 128, g * 512:(g + 1) * 512], u[:, :])
            ps = pb[2]
            for kt in range(8):
                P.mm(ps[:, :], wck[:, kt, :], h[:, kt, :], start=(kt == 0), stop=(kt == 7))
            P.act(sq[:, :], ps[:, :], AF.Square)
            P.mm(pb[3][:, :], C.ones_bf[:, :], sq[:, :])
            P.ts("dve", rst[:, :], pb[3][:, :], 1.0 / 128, LN_EPS, ALU.mult, ALU.add)
            P.act(rst[:, :], rst[:, :], AF.Sqrt)
            P.recip(rst[:, :], rst[:, :])
            P.stt(C.ckvT[:, g * 512:(g + 1) * 512], ps[:, :], kvg[:, :], rst[:, :], ALU.mult, ALU.mult)
            ps = pb[4]
            for tt_ in range(4):
                for kt in range(8):
                    P.mm(ps[:, tt_ * 128:(tt_ + 1) * 128], h[:, kt, tt_ * 128:(tt_ + 1) * 128], wck[:, kt, :],
                         start=(kt == 0), stop=(kt == 7))
            for tt_ in range(4):
                ss = S.col(); rs = S.col()
                P.act(junk[:, :], ps[:, tt_ * 128:(tt_ + 1) * 128], AF.Square, accum=ss[:, :])
                P.ts("dve", rs[:, :], ss[:, :], 1.0 / 128, LN_EPS, ALU.mult, ALU.add)
                P.act(rs[:, :], rs[:, :], AF.Sqrt)
                P.recip(rs[:, :], rs[:, :])
                P.stt(C.ckv_tok[:, g * 4 + tt_, 0:128], ps[:, tt_ * 128:(tt_ + 1) * 128], rs[:, :], kvgrow[:, :],
                      ALU.mult, ALU.mult)
            ps = pb[5]
            for kt in range(8):
                P.mm(ps[0:32, :], wki[:, kt, :], h[:, kt, :], start=(kt == 0), stop=(kt == 7))
            P.cp("act", C.kidxT[0:32, g * 512:(g + 1) * 512], ps[0:32, :])


SEG = 512
NSEG = WIN // SEG
OWN0 = PRE // SEG


def reduce_turns(P, out_f, u, tmp):
    P.ts("dve", tmp, u, MAGIC, None, ALU.add)
    P.ts("dve", tmp, tmp, MAGIC, None, ALU.subtract)
    P.tt("dve", out_f, u, tmp, ALU.subtract)


def sincos(P, S_out, C_out, f, tmp):
    P.act(S_out, f, AF.Sin, scale=TWO_PI)
    P.act(tmp, f, AF.Abs)
    P.act(C_out, tmp, AF.Sin, scale=-TWO_PI, bias=C_halfpi)


C_halfpi = None


def stage_ssm(P, C):
    global C_halfpi
    pb = C.pb
    with Stage(P):
        halfpi = P.sb("halfpi", [128, 1]); P.memset("dve", halfpi[:, :], math.pi / 2)
        C_halfpi = halfpi[:, :]
        lhs_bu = P.sb("lhs_bu", [128, 16, 2, 128], BF16)
        lhs_c = P.sb("lhs_c", [128, 16, 2, 128], BF16)
        rcol = P.sb("rcol", [128, 16]); fturn = P.sb("fturn", [128, 16]); f0 = P.sb("f0", [128, 16, NSEG])
        dsk = P.sb("dsk", [128, 4]); P.dma("sp", dsk[:, :], C.dskip[:, :])
        bgl = P.sb("bgl", [128, 4]); P.dma("sp", bgl[:, :], C.bglu[:, :])
        with Stage(P):
            def ld(name, src, shape):
                t = P.sb(name, shape);
                P.dma("sp", t[tuple(slice(None) for _ in shape)], src[tuple(slice(None) for _ in shape)])
                return t
            lre = ld("lre", C.lamre, [128, 16]); lim = ld("lim", C.lamim, [128, 16]); ldt = ld("ldt", C.logdt, [128, 16])
            bre = ld("bre_s", C.bre, [128, 16, 16]); bim = ld("bim_s", C.bim, [128, 16, 16])
            cre = ld("cre_s", C.cre, [128, 16, 16]); cim = ld("cim_s", C.cim, [128, 16, 16])
            n16 = lambda nm: P.sb(nm, [128, 16])
            dt = n16("dt"); lnr = n16("lnr"); th = n16("th"); tmp = n16("tmp16"); ff = n16("ff")
            sn = n16("sn"); cs = n16("cs"); ar = n16("ar"); ai = n16("ai"); den = n16("den")
            kr = n16("kr"); ki = n16("ki"); nki = n16("nki"); t1 = n16("t1_16"); t2 = n16("t2_16")
            A = slice(None)
            P.act(dt[:, :], ldt[:, :], AF.Exp)
            P.tt("dve", lnr[:, :], lre[:, :], dt[:, :], ALU.mult)
            P.tt("dve", th[:, :], lim[:, :], dt[:, :], ALU.mult)
            P.ts("dve", fturn[:, :], th[:, :], 1.0 / TWO_PI, None, ALU.mult)
            P.act(rcol[:, :], lnr[:, :], AF.Exp)
            reduce_turns(P, ff[:, :], fturn[:, :], tmp[:, :])
            sincos(P, sn[:, :], cs[:, :], ff[:, :], tmp[:, :])
            P.tt("dve", ar[:, :], rcol[:, :], cs[:, :], ALU.mult)
            P.tt("dve", ai[:, :], rcol[:, :], sn[:, :], ALU.mult)
            P.ts("dve", ar[:, :], ar[:, :], -1.0, None, ALU.add)
            P.tt("dve", den[:, :], lre[:, :], lre[:, :], ALU.mult)
            P.tt("dve", t1[:, :], lim[:, :], lim[:, :], ALU.mult)
            P.tt("dve", den[:, :], den[:, :], t1[:, :], ALU.add)
            P.recip(den[:, :], den[:, :])
            P.tt("dve", t1[:, :], ar[:, :], lre[:, :], ALU.mult)
            P.tt("dve", t2[:, :], ai[:, :], lim[:, :], ALU.mult)
            P.tt("dve", kr[:, :], t1[:, :], t2[:, :], ALU.add)
            P.tt("dve", kr[:, :], kr[:, :], den[:, :], ALU.mult)
            P.tt("dve", t1[:, :], ai[:, :], lre[:, :], ALU.mult)
            P.tt("dve", t2[:, :], ar[:, :], lim[:, :], ALU.mult)
            P.tt("dve", ki[:, :], t1[:, :], t2[:, :], ALU.subtract)
            P.tt("dve", ki[:, :], ki[:, :], den[:, :], ALU.mult)
            P.ts("dve", nki[:, :], ki[:, :], -1.0, None, ALU.mult)
            for q in range(NSEG):
                P.ts("dve", f0[:, :, q], fturn[:, :], float(SEG * q), None, ALU.mult)
            ftmp = P.sb("ftmp", [128, 16, NSEG])
            reduce_turns(P, f0[:, :, :], f0[:, :, :], ftmp[:, :, :])
            bbr = P.sb("bbr", [128, 16, 16]); bbi = P.sb("bbi", [128, 16, 16]); tb = P.sb("tb", [128, 16])
            Sp = P.sb("Sp", [128, 32, 128])
            P.memset("pool", Sp[:, :, :], 0.0)
            lcf = P.sb("lcf", [128, 32, 128])
            P.memset("pool", lcf[:, :, :], 0.0)
            ncim = P.sb("ncim", [128, 16, 16])
            P.ts("dve", ncim[:, :, :], cim[:, :, :], -1.0, None, ALU.mult)
            for i in range(16):
                P.ts("dve", tb[:, :], bre[:, i, :], kr[:, i:i + 1], None, ALU.mult)
                P.stt(bbr[:, i, :], bim[:, i, :], nki[:, i:i + 1], tb[:, :], ALU.mult, ALU.add)
                P.ts("dve", tb[:, :], bim[:, i, :], kr[:, i:i + 1], None, ALU.mult)
                P.stt(bbi[:, i, :], bre[:, i, :], ki[:, i:i + 1], tb[:, :], ALU.mult, ALU.add)
                c0 = 32 * (i % 4)
                for gg in range(2):
                    rows = slice(64 * gg, 64 * gg + 64)
                    cols = slice(c0 + 16 * gg, c0 + 16 * gg + 16)
                    P.cp("dve", Sp[rows, 2 * i, cols], bbr[rows, i, :])
                    P.cp("dve", Sp[rows, 2 * i + 1, cols], bbi[rows, i, :])
                    P.cp("dve", lcf[rows, 2 * i, cols], cre[rows, i, :])
                    P.cp("dve", lcf[rows, 2 * i + 1, cols], ncim[rows, i, :])
            for i in range(16):
                for ri in range(2):
                    ps = pb[(2 * i + ri) % 4]
                    P.tr(ps[:, 0:128], Sp[:, 2 * i + ri, :], C.ident[:, :])
                    P.cp("act", lhs_bu[:, i, ri, :], ps[:, 0:128])
                    P.cp("pool", lhs_c[:, i, ri, :], lcf[:, 2 * i + ri, :])
        iota = P.sb("iota", [128, SEG])
        P.op("pool", lambda g: g.iota(iota.h[:, :], [[1, SEG]], 0, channel_multiplier=0, allow_small_or_imprecise_dtypes=True),
             reads=[], writes=[iota])
        ones = P.sb("ones_s", [128, SEG]); P.memset("dve", ones[:, :], 1.0)
        rbc = P.sb("rbc", [128, SEG])
        uTt = P.sb("uTt", [128, WIN], BF16)
        mk = lambda nm, dt_=F32: P.sb(nm, [128, SEG], dt_)
        tu = mk("tu"); tn = mk("tn"); tf = mk("tf"); tS = mk("tS"); tC = mk("tC")
        t1 = mk("r1"); t2 = mk("r2"); t3 = mk("r3"); t4 = mk("r4")
        zs = [[mk(f"zs{a}{b}") for b in range(2)] for a in range(2)]
        zro = P.sb("zro", [128, TOWN]); zio = P.sb("zio", [128, TOWN])
        hr = mk("hr", BF16); hi = mk("hi", BF16)
        zbf = P.sb("zbf", [128, 4, TOWN], BF16)
        u32 = P.sb("u32", [128, TOWN]); yy = P.sb("yy", [128, TOWN]); y2 = P.sb("y2", [128, TOWN])
        for i in range(16):
            ct = i // 4
            if i % 4 == 0:
                P.dma("pool", uTt[:, C.pad:WIN], C.uT[ct * 128:(ct + 1) * 128, C.pad:WIN])
                P.dma("sp", u32[:, :], C.uT[ct * 128:(ct + 1) * 128, PRE:WIN])
            P.ts("dve", rbc[:, :], ones[:, :], rcol[:, i:i + 1], None, ALU.mult)
            prev = None
            for q in range(C.pad // SEG, NSEG):
                own = q >= OWN0
                P.ts("dve", tu[:, :], iota[:, :], fturn[:, i:i + 1], f0[:, i, q:q + 1], ALU.mult, ALU.add)
                reduce_turns(P, tf[:, :], tu[:, :], tn[:, :])
                sincos(P, tS[:, :], tC[:, :], tf[:, :], tn[:, :])
                pr = pb[q % 2]; pi_ = pb[2 + q % 2]
                P.mm(pr[:, :], lhs_bu[:, i, 0, :], uTt[:, q * SEG:(q + 1) * SEG])
                P.mm(pi_[:, :], lhs_bu[:, i, 1, :], uTt[:, q * SEG:(q + 1) * SEG])
                P.tt("dve", t1[:, :], tC[:, :], pr[:, :], ALU.mult)
                P.tt("dve", t2[:, :], tS[:, :], pi_[:, :], ALU.mult)
                P.tt("dve", t3[:, :], tC[:, :], pi_[:, :], ALU.mult)
                P.tt("dve", t4[:, :], tS[:, :], pr[:, :], ALU.mult)
                P.tt("pool", t1[:, :], t1[:, :], t2[:, :], ALU.add)
                P.tt("pool", t3[:, :], t3[:, :], t4[:, :], ALU.subtract)
                if own:
                    o = (q - OWN0) * SEG
                    zr_o = zro[:, o:o + SEG]; zi_o = zio[:, o:o + SEG]
                else:
                    zr_o = zs[0][q % 2][:, :]; zi_o = zs[1][q % 2][:, :]
                ir = 0.0 if prev is None else prev[0]
                ii = 0.0 if prev is None else prev[1]
                P.scan(zr_o, rbc[:, :], t1[:, :], ir, ALU.mult, ALU.add)
                P.scan(zi_o, rbc[:, :], t3[:, :], ii, ALU.mult, ALU.add)
                if own:
                    prev = (zro[:, o + SEG - 1:o + SEG], zio[:, o + SEG - 1:o + SEG])
                else:
                    prev = (zs[0][q % 2][:, SEG - 1:SEG], zs[1][q % 2][:, SEG - 1:SEG])
                if own:
                    P.tt("pool", t2[:, :], tC[:, :], zr_o, ALU.mult)
                    P.tt("pool", t4[:, :], tS[:, :], zi_o, ALU.mult)
                    P.tt("dve", hr[:, :], t2[:, :], t4[:, :], ALU.subtract)
                    P.tt("pool", t2[:, :], tS[:, :], zr_o, ALU.mult)
                    P.tt("pool", t4[:, :], tC[:, :], zi_o, ALU.mult)
                    P.tt("dve", hi[:, :], t2[:, :], t4[:, :], ALU.add)
                    py = pb[4 + (q - OWN0)]
                    P.mm(py[:, :], lhs_c[:, i, 0, :], hr[:, :], start=(i % 4 == 0), stop=False)
                    P.mm(py[:, :], lhs_c[:, i, 1, :], hi[:, :], start=False, stop=(i % 4 == 3))
            if i % 4 == 3:
                for s in range(4):
                    sl = slice(s * SEG, (s + 1) * SEG)
                    P.stt(yy[:, sl], u32[:, sl], dsk[:, ct:ct + 1], pb[4 + s][:, :], ALU.mult, ALU.add)
                P.tt("pool", y2[:, :], yy[:, :], yy[:, :], ALU.mult)
                P.ts("dve", y2[:, :], y2[:, :], 0.0713548163, 1.5957691216, ALU.mult, ALU.add)
                P.tt("dve", y2[:, :], y2[:, :], yy[:, :], ALU.mult)
                P.act(y2[:, :], y2[:, :], AF.Sigmoid)
                P.tt("dve", yy[:, :], yy[:, :], y2[:, :], ALU.mult)
                P.cp("act", zbf[:, ct, :], yy[:, :])
                P.dma("sp", C.z32[ct * 128:(ct + 1) * 128, :], yy[:, :])
        wgl = P.sb("wgl", [128, 4, 512], BF16)
        P.dma("pool", wgl[:, :, :], V(C.w_glu, C.w_glu.h[:, :].rearrange("(kt k) n -> k kt n", k=128)))
        for co in range(4):
            P.dma("sp", u32[:, :], C.z32[co * 128:(co + 1) * 128, :])
            for tg in range(4):
                ps = pb[tg % 4]
                sl = slice(tg * 512, (tg + 1) * 512)
                for kt in range(4):
                    P.mm(ps[:, :], wgl[:, kt, co * 128:(co + 1) * 128], zbf[:, kt, sl], start=(kt == 0), stop=(kt == 3))
                P.act(y2[:, sl], ps[:, :], AF.Sigmoid, bias=bgl[:, co:co + 1])
                P.tt("dve", hr[:, :], u32[:, sl], y2[:, sl], ALU.mult)
                P.dma("sp", C.brT[2, co * 128:(co + 1) * 128, sl], hr[:, :])


def proj_fm(P, C, ps, w, c0, n, tg, rows=None):
    for kt in range(8):
        P.mm(ps[0:n, :], w[:, kt, c0:c0 + n], C.hTo[:, kt, tg * 512:(tg + 1) * 512], start=(kt == 0), stop=(kt == 7))


def stage_conv(P, C):
    pb = C.pb
    with Stage(P):
        wcu = P.sb("wcu", [128, 8, 512], BF16); wgb = P.sb("wgb", [128, 8, 512], BF16); wgc = P.sb("wgc", [128, 8, 512], BF16)
        P.dma("pool", wcu[:, :, :], wview(C.w_in, C_CU, 512))
        P.dma("pool", wgb[:, :, :], wview(C.w_in, C_GB, 512))
        P.dma("pool", wgc[:, :, :], wview(C.w_in, C_GC, 512))
        cw = P.sb("cw", [128, 4, 3]); P.dma("sp", cw[:, :, :], C.convw[:, :, :])
        cb = P.sb("cb", [128, 4]); P.dma("sp", cb[:, :], C.convb[:, :])
        hh = P.sb("hhalo", [128, 8, 2], BF16)
        P.dma("pool", hh[:, :, :], V(C.hT, C.hT.h[:, C.off + PRE - 2:C.off + PRE].rearrange("(kt k) t -> k kt t", k=128)))
        v = P.sb("cv", [128, TOWN + 2]); us = P.sb("cus", [128, 512]); y = P.sb("cy", [128, TOWN])
        ob = P.sb("cob", [128, 512], BF16)
        for ct in range(4):
            cs = slice(ct * 128, (ct + 1) * 128)
            for kt in range(8):
                P.mm(pb[0][:, 0:2], wcu[:, kt, cs], hh[:, kt, :], start=(kt == 0), stop=(kt == 7))
            for kt in range(8):
                P.mm(pb[1][:, 0:2], wgc[:, kt, cs], hh[:, kt, :], start=(kt == 0), stop=(kt == 7))
            P.cp("act", us[:, 0:2], pb[0][:, 0:2])
            P.tt("dve", v[:, 0:2], us[:, 0:2], pb[1][:, 0:2], ALU.mult)
            for tg in range(4):
                proj_fm(P, C, pb[2], wcu, ct * 128, 128, tg)
                proj_fm(P, C, pb[3], wgc, ct * 128, 128, tg)
                P.cp("act", us[:, :], pb[2][:, :])
                P.tt("dve", v[:, 2 + tg * 512:2 + (tg + 1) * 512], us[:, :], pb[3][:, :], ALU.mult)
            P.ts("dve", y[:, :], v[:, 2:TOWN + 2], cw[:, ct, 2:3], cb[:, ct:ct + 1], ALU.mult, ALU.add)
            P.stt(y[:, :], v[:, 1:TOWN + 1], cw[:, ct, 1:2], y[:, :], ALU.mult, ALU.add)
            P.stt(y[:, :], v[:, 0:TOWN], cw[:, ct, 0:1], y[:, :], ALU.mult, ALU.add)
            for tg in range(4):
                proj_fm(P, C, pb[4 + tg % 2], wgb, ct * 128, 128, tg)
                P.tt("dve", ob[:, :], y[:, tg * 512:(tg + 1) * 512], pb[4 + tg % 2][:, :], ALU.mult)
                P.dma("sp", C.brT[1, cs, tg * 512:(tg + 1) * 512], ob[:, :])


def stage_mem(P, C):
    pb = C.pb
    with Stage(P):
        wmq = P.sb("wmq", [128, 8, 512], BF16)
        P.dma("pool", wmq[:, :, :], wview(C.w_in, C_MQ, 512))
        wkv = P.sb("wkv", [128, 8, 1024], BF16)
        P.dma("pool", wkv[:, :, :], V(C.w_mem, C.w_mem.h[:, :].rearrange("(kt k) n -> k kt n", k=128)))
        mT = P.sb("mT", [128, 8, 256], BF16)
        P.dma("pool", mT[:, :, :], V(C.memT, C.memT.h[:, :].rearrange("(kt k) m -> k kt m", k=128)))
        KT = P.sb("KT", [128, 4, 256], BF16)
        Vt = P.sb("Vt", [128, 2, 512], BF16)
        for h in range(4):
            for kt in range(8):
                P.mm(pb[0][:, 0:256], wkv[:, kt, h * 128:(h + 1) * 128], mT[:, kt, :], start=(kt == 0), stop=(kt == 7))
            P.cp("act", KT[:, h, :], pb[0][:, 0:256])
        for mt in range(2):
            for kt in range(8):
                P.mm(pb[1][:, :], mT[:, kt, mt * 128:(mt + 1) * 128], wkv[:, kt, 512:1024], start=(kt == 0), stop=(kt == 7))
            P.cp("act", Vt[:, mt, :], pb[1][:, :])
        mq = P.sb("mq", [128, 512], BF16); pT = P.sb("mpT", [128, 2, 512], BF16)
        rec = P.sb("mrec", [128, 512]); ob = P.sb("mob", [128, 512], BF16)
        for h in range(4):
            for tg in range(4):
                proj_fm(P, C, pb[2], wmq, h * 128, 128, tg)
                P.act(mq[:, :], pb[2][:, :], AF.Copy, scale=128.0 ** -0.5)
                for mt in range(2):
                    P.mm(pb[3 + mt][:, :], KT[:, h, mt * 128:(mt + 1) * 128], mq[:, :])
                    P.act(pT[:, mt, :], pb[3 + mt][:, :], AF.Exp)
                for mt in range(2):
                    P.mm(pb[5][:, :], Vt[:, mt, h * 128:(h + 1) * 128], pT[:, mt, :], start=(mt == 0), stop=(mt == 1))
                for mt in range(2):
                    P.mm(pb[6][:, :], C.ones_bf[:, :], pT[:, mt, :], start=(mt == 0), stop=(mt == 1))
                P.recip(rec[:, :], pb[6][:, :])
                P.tt("dve", ob[:, :], rec[:, :], pb[5][:, :], ALU.mult)
                P.dma("sp", C.brT[3, h * 128:(h + 1) * 128, tg * 512:(tg + 1) * 512], ob[:, :])


def stage_attproj(P, C):
    pb = C.pb
    with Stage(P):
        wq = P.sb("wq", [128, 8, 512], BF16); P.dma("pool", wq[:, :, :], wview(C.w_in, C_Q, 512))
        wqi = P.sb("wqi", [128, 8, 256], BF16); P.dma("pool", wqi[:, :, :], wview(C.w_in, C_QI, 256))
        wwi = P.sb("wwi", [128, 8, 8], BF16); P.dma("pool", wwi[:, :, :], wview(C.w_in, C_WI, 8))
        wuk = P.sb("wuk", [128, 512]); P.dma("sp", wuk[:, :], C.w_uk[:, :])
        wukT = P.sb("wukT", [128, 4, 128], BF16)
        for m in range(4):
            P.tr(pb[0][:, 0:128], wuk[:, m * 128:(m + 1) * 128], C.ident[:, :])
            P.cp("act", wukT[:, m, :], pb[0][:, 0:128])
        qT = P.sb("qT", [128, 4, TOWN], BF16)
        for m in range(4):
            for tg in range(4):
                proj_fm(P, C, pb[1 + tg % 2], wq, m * 128, 128, tg)
                P.cp("act", qT[:, m, tg * 512:(tg + 1) * 512], pb[1 + tg % 2][:, :])
        st = [P.sb(f"qst{i}", [128, 512], BF16) for i in range(2)]
        k = 0
        for h in range(8):
            m, hh = h // 2, h % 2
            rows = slice(64 * hh, 64 * hh + 64)
            for tg in range(4):
                ps = pb[3 + k % 2]; s = st[k % 2]; k += 1
                P.mm(ps[:, :], wukT[rows, m, :], qT[rows, m, tg * 512:(tg + 1) * 512])
                P.act(s[:, :], ps[:, :], AF.Copy, scale=0.125)
                P.dma("sp", C.qlat_d[:, h, tg * 512:(tg + 1) * 512], s[:, :])
        for h in range(8):
            for tg in range(4):
                ps = pb[5 + k % 2]; s = st[k % 2]; k += 1
                proj_fm(P, C, ps, wqi, h * 32, 32, tg)
                P.cp("act", s[0:32, :], ps[0:32, :])
                P.dma("sp", C.qidx_d[0:32, h, tg * 512:(tg + 1) * 512], s[0:32, :])
        for tt_ in range(16):
            for kt in range(8):
                P.mm(pb[7][:, 0:8], C.hTo[:, kt, tt_ * 128:(tt_ + 1) * 128], wwi[:, kt, :], start=(kt == 0), stop=(kt == 7))
            P.cp("act", C.widx[:, tt_, :], pb[7][:, 0:8])


def stage_att(P, C):
    pb = C.pb
    with Stage(P):
        acc = P.sb("acc", [128, WIN]); notsel = P.sb("notsel", [128, WIN], BF16); bj = P.sb("bj", [128, WIN], BF16)
        tmp = [P.sb(f"atmp{i}", [128, 512]) for i in range(2)]
        qi = P.sb("qi", [32, 8, 128], BF16); ql = P.sb("ql", [128, 8, 128], BF16)
        npad = P.sb("npad_s", [128, 1]); P.dma("sp", npad[:, :], C.npad[C.j, :, :])
        padc = P.sb("padc", [128, 64]); P.dma("sp", padc[:, :], C.padcol[C.j, :, :])
        tri = P.sb("tri_s", [128, 128]); P.dma("sp", tri[:, :], C.tri[:, :])
        negI = P.sb("negI", [128, 4, 128], BF16)
        for j in range(4):
            P.ts("dve", negI[:, j, :], C.ident[:, :], NEG, None, ALU.mult)
        wuv = P.sb("wuv", [128, 512]); P.dma("sp", wuv[:, :], C.w_uv[:, :])
        wuvp = P.sb("wuvp", [128, 8, 128], BF16)
        P.memset("pool", wuvp[:, :, :], 0.0)
        for h in range(8):
            P.cp("dve", wuvp[:, h, 64 * (h % 2):64 * (h % 2) + 64], wuv[:, h * 64:(h + 1) * 64])
        pTs = [P.sb(f"pT{i}", [128, 512], BF16) for i in range(3)]
        ol = P.sb("ol", [128, 8, 128]); olT = P.sb("olT", [128, 8, 128], BF16)
        ab = P.sb("ab", [128, 4, 128], BF16)
        S = Small(P, "att")
        lo = S.col(); hi = S.col(); mid = S.col(); cnt = S.col(); c2 = S.col(); ge = S.col(); d1 = S.col(); d2 = S.col()
        rec = S.col(8)
        kk = 0
        for qb in range(TOWN // 128):
            nk = PRE // 128 + qb + 1
            NK = nk * 128
            qs = slice(qb * 128, (qb + 1) * 128)
            P.dma("sp", qi[:, :, :], C.qidx_d[0:32, :, qs])
            P.dma("sp", ql[:, :, :], C.qlat_d[:, :, qs])
            pad = C.pad
            nsp = (NK - pad + 511) // 512
            for h in range(8):
                for s in range(nsp):
                    n = min(512, NK - pad - 512 * s)
                    ks = slice(pad + 512 * s, pad + 512 * s + n)
                    ps = pb[kk % 2]; t = tmp[kk % 2]; kk += 1
                    P.mm(ps[:, 0:n], qi[0:32, h, :], C.kidxT[0:32, ks])
                    P.act(t[:, 0:n], ps[:, 0:n], AF.Relu)
                    if h == 0:
                        P.ts("dve", acc[:, ks], t[:, 0:n], C.widx[:, qb, 0:1], None, ALU.mult)
                    else:
                        P.stt(acc[:, ks], t[:, 0:n], C.widx[:, qb, h:h + 1], acc[:, ks], ALU.mult, ALU.add)
            P.red(hi[:, :], acc[:, pad:NK], ALU.max)
            P.red(lo[:, :], acc[:, pad:NK], ALU.min)
            P.tt("dve", acc[:, NK - 128:NK], acc[:, NK - 128:NK], tri[:, :], ALU.add)
            P.tt("dve", d2[:, :], hi[:, :], lo[:, :], ALU.subtract)
            for it in range(NBIS):
                P.ts("dve", mid[:, :], d2[:, :], 0.5 ** (it + 1), lo[:, :], ALU.mult, ALU.add)
                P.ts("dve", bj[:, pad:NK], acc[:, pad:NK], mid[:, :], None, ALU.is_ge, op1=ALU.add, accum=cnt[:, :])
                P.ts("dve", ge[:, :], cnt[:, :], 256.0, None, ALU.is_ge)
                P.tt("dve", d1[:, :], mid[:, :], lo[:, :], ALU.subtract)
                P.stt(lo[:, :], d1[:, :], ge[:, :], lo[:, :], ALU.mult, ALU.add)
            P.ts("dve", notsel[:, pad:NK], acc[:, pad:NK], lo[:, :], None, ALU.is_lt)
            kc0 = pad // 128
            for kc in range(kc0, nk):
                cs = slice(kc * 128, (kc + 1) * 128)
                for hg in range(2):
                    pl = pb[2 + kk % 2]; pT = pTs[kk % 3]; kk += 1
                    P.mm(pl[:, :], C.ckvT[:, cs], ql[:, 4 * hg:4 * hg + 4, :], start=True, stop=False)
                    P.mm(pl[:, :], notsel[:, cs], negI[:, :, :], start=False, stop=True)
                    P.act(pT[:, :], pl[:, :], AF.Exp, bias=padc[:, kc:kc + 1])
                    for h4 in range(4):
                        h = 4 * hg + h4
                        po = pb[4 + h // 3]
                        P.mm(po[:, (h % 3) * 129:(h % 3) * 129 + 129], pT[:, h4 * 128:(h4 + 1) * 128], C.ckv_tok[:, kc, :],
                             start=(kc == kc0), stop=(kc == nk - 1))
            for h in range(8):
                po = pb[4 + h // 3]; o = (h % 3) * 129
                P.recip(rec[:, h:h + 1], po[:, o + 128:o + 129])
                P.ts("dve", ol[:, h, :], po[:, o:o + 128], rec[:, h:h + 1], None, ALU.mult)
            for h in range(8):
                P.tr(pb[7][:, (h % 4) * 128:(h % 4) * 128 + 128], ol[:, h, :], C.ident[:, :])
                P.cp("act", olT[:, h, :], pb[7][:, (h % 4) * 128:(h % 4) * 128 + 128])
            for m in range(4):
                ps = pb[kk % 2]; kk += 1
                P.mm(ps[:, 0:128], wuvp[:, 2 * m, :], olT[:, 2 * m, :], start=True, stop=False)
                P.mm(ps[:, 0:128], wuvp[:, 2 * m + 1, :], olT[:, 2 * m + 1, :], start=False, stop=True)
                P.cp("act", ab[:, m, :], ps[:, 0:128])
            P.dma("sp", V(C.brT, C.brT.h[0, :, qs].rearrange("(m p) q -> p m q", p=128)), ab[:, :, :])


def stage_merge_a(P, C):
    pb = C.pb
    with Stage(P):
        macc = P.sb("macc", [128, 8, TOWN])
        brt = P.sb("brt", [128, 4, TOWN], BF16)
        wbr = P.sb("wbr", [128, 4, D], BF16)
        wg = P.sb("wg", [128, 8, D], BF16)
        sg = [P.sb(f"sg{i}", [128, 512]) for i in range(2)]
        tm = [P.sb(f"mtm{i}", [128, 512]) for i in range(2)]
        k = 0
        for r in range(4):
            P.dma("sp", brt[:, :, :], V(C.brT, C.brT.h[r, :, :].rearrange("(kt k) t -> k kt t", k=128)))
            P.dma("pool", wbr[:, :, :], V(C.w_br, C.w_br.h[r, :, :].rearrange("(kt k) n -> k kt n", k=128)))
            P.dma("pool", wg[:, :, :], wview(C.w_in, C_GATE + r * D, D))
            for dt_ in range(8):
                ds_ = slice(dt_ * 128, (dt_ + 1) * 128)
                for tg in range(4):
                    ts_ = slice(tg * 512, (tg + 1) * 512)
                    pg = pb[k % 2]; pr = pb[2 + k % 2]; s = sg[k % 2]; t = tm[k % 2]; k += 1
                    for kt in range(8):
                        P.mm(pg[:, :], wg[:, kt, ds_], C.hTo[:, kt, ts_], start=(kt == 0), stop=(kt == 7))
                    for kt in range(4):
                        P.mm(pr[:, :], wbr[:, kt, ds_], brt[:, kt, ts_], start=(kt == 0), stop=(kt == 3))
                    P.act(s[:, :], pg[:, :], AF.Sigmoid)
                    if r == 0:
                        P.tt("dve", macc[:, dt_, ts_], s[:, :], pr[:, :], ALU.mult)
                    else:
                        P.tt("dve", t[:, :], s[:, :], pr[:, :], ALU.mult)
                        P.tt("pool", macc[:, dt_, ts_], macc[:, dt_, ts_], t[:, :], ALU.add)
        for dt_ in range(8):
            P.cp("act" if dt_ % 2 else "dve", brt[:, dt_ % 4, :], macc[:, dt_, :])
            P.dma("sp", C.mT_d[dt_ * 128:(dt_ + 1) * 128, :], brt[:, dt_ % 4, :])


def stage_merge_b(P, C):
    pb = C.pb
    with Stage(P):
        mT = P.sb("mTb", [128, 8, TOWN], BF16)
        P.dma("sp", mT[:, :, :], V(C.mT_d, C.mT_d.h[:, :].rearrange("(kt k) t -> k kt t", k=128)))
        wo = P.sb("wo", [128, 8, D], BF16)
        P.dma("pool", wo[:, :, :], V(C.w_o, C.w_o.h[:, :].rearrange("(kt k) n -> k kt n", k=128)))
        g1 = P.sb("g1", [128, D]); b1 = P.sb("b1", [128, D])
        P.dma("sp", g1[:, :], C.ln1g[:, :]); P.dma("sp", b1[:, :], C.ln1b[:, :])
        wr = P.sb("wr32", [128, 8, 32])
        P.dma("sp", wr[:, :, :], V(C.w_r, C.w_r.h[:, :].rearrange("(kt k) n -> k kt n", k=128)))
        wrh = P.sb("wrh", [128, 8, 32], BF16); wrl = P.sb("wrl", [128, 8, 32], BF16)
        P.cp("dve", wrh[:, :, :], wr[:, :, :])
        P.tt("dve", wrl[:, :, :], wr[:, :, :], wrh[:, :, :], ALU.subtract)
        brr = P.sb("brr", [128, 32]); P.dma("sp", brr[:, :], C.b_r[:, :])
        ho = [P.sb(f"ho{i}", [128, D]) for i in range(2)]
        xx = [P.sb(f"xx{i}", [128, D]) for i in range(2)]
        h1 = [P.sb(f"h1_{i}", [128, D]) for i in range(2)]
        h32 = [P.sb(f"h32_{i}", [128, 128], BF16) for i in range(3)]
        junk = P.sb("mjunk", [128, D])
        S = Small(P, "ln1")
        k = 0
        for tt_ in range(16):
            tsl = slice(tt_ * 128, (tt_ + 1) * 128)
            hot = ho[tt_ % 2]; x = xx[tt_ % 2]; h = h1[tt_ % 2]
            P.dma("sp", hot[:, :], C.hown[C.off + tt_ * 128:C.off + (tt_ + 1) * 128, :])
            for dh in range(2):
                ps = pb[dh]
                for kt in range(8):
                    P.mm(ps[:, :], mT[:, kt, tsl], wo[:, kt, dh * 512:(dh + 1) * 512], start=(kt == 0), stop=(kt == 7))
                P.stt(x[:, dh * 512:(dh + 1) * 512], hot[:, dh * 512:(dh + 1) * 512], ALPHA, ps[:, :], ALU.mult, ALU.add)
            ln_tok(P, x[:, :], h[:, :], g1[:, :], b1[:, :], S, junk[:, :])
            P.act(C.yacc[tt_][:, :], h[:, :], AF.Copy, scale=ALPHA)
            for kt in range(8):
                pt = pb[2 + k % 4]; hh = h32[k % 3]; k += 1
                P.tr(pt[:, 0:128], h[:, kt * 128:(kt + 1) * 128], C.ident[:, :])
                P.cp("act", C.h1T[:, kt, tsl], pt[:, 0:128])
                P.tt("dve", hh[:, :], pt[:, 0:128], C.h1T[:, kt, tsl], ALU.subtract)
                P.mm(pb[6][:, 0:32], C.h1T[:, kt, tsl], wrh[:, kt, :], start=(kt == 0), stop=False)
                P.mm(pb[6][:, 0:32], C.h1T[:, kt, tsl], wrl[:, kt, :], start=False, stop=False)
                P.mm(pb[6][:, 0:32], hh[:, :], wrh[:, kt, :], start=False, stop=(kt == 7))
            P.tt("dve", C.rlog[:, tt_, :], pb[6][:, 0:32], brr[:, :], ALU.add)


def emit_hT(P, C, o, dst, t0, k):
    if not hasattr(C, "tst") or C.tst_owner is not P.es:
        C.tst = [P.sb(f"tst{i}", [128, 8, 128]) for i in range(2)]
        C.tst_owner = P.es
    st = C.tst[k % 2]
    for kt in range(8):
        ps = C.pb[(kt // 4) + 2 * (k % 2)]
        P.tr(ps[:, (kt % 4) * 128:(kt % 4) * 128 + 128], o[:, kt * 128:(kt + 1) * 128], C.ident[:, :])
    for half in range(2):
        ps = C.pb[half + 2 * (k % 2)]
        P.cp("act" if half else "dve", st[:, 4 * half:4 * half + 4, :], ps[:, :])
    P.dma("sp", V(dst, dst.h[:, PRE + t0:PRE + t0 + 128].rearrange("(kt k) t -> k kt t", k=128)), st[:, :, :])


def stage_moe(P, C):
    pb = C.pb
    with Stage(P):
        S = Small(P, "moe")
        gates = P.sb("gates", [128, 16, 32])
        gT = P.sb("gT", [32, TOWN], BF16)
        bdn = P.sb("bdn", [32, D], BF16); P.dma("pool", bdn[:, :], C.b_dn[:, :])
        bup = P.sb("bup", [128, 32, 8, 2]); P.dma("sp", bup[:, :, :, :], C.b_up[:, :, :, :])
        top8 = P.sb("top8", [128, 8]); nmx = S.col(); ssum = S.col(); ee = P.sb("ree", [128, 32]); mk = P.sb("rmk", [128, 32])
        for tt_ in range(16):
            lg = C.rlog[:, tt_, :]
            P.max8(top8[:, :], lg)
            P.ts("dve", nmx[:, :], top8[:, 0:1], -1.0, None, ALU.mult)
            P.act(ee[:, :], lg, AF.Exp, bias=nmx[:, :])
            P.ts("dve", mk[:, :], lg, top8[:, 3:4], None, ALU.is_ge)
            P.tt("dve", ee[:, :], ee[:, :], mk[:, :], ALU.mult)
            P.red(ssum[:, :], ee[:, :], ALU.add)
            P.recip(ssum[:, :], ssum[:, :])
            P.ts("dve", gates[:, tt_, :], ee[:, :], ssum[:, :], None, ALU.mult)
            P.tr(pb[7][0:32, 0:128], gates[:, tt_, :], C.ident[:, :])
            P.cp("act", gT[0:32, tt_ * 128:(tt_ + 1) * 128], pb[7][0:32, 0:128])
        for tt_ in range(16):
            for dh in range(2):
                ps = pb[dh]
                P.mm(ps[:, :], gT[0:32, tt_ * 128:(tt_ + 1) * 128], bdn[0:32, dh * 512:(dh + 1) * 512])
                P.tt("dve", C.yacc[tt_][:, dh * 512:(dh + 1) * 512], C.yacc[tt_][:, dh * 512:(dh + 1) * 512], ps[:, :], ALU.add)
        with Stage(P):
            wup = [P.sb(f"wup{i}", [128, 8, 1024], BF16) for i in range(2)]
            wdn = [P.sb(f"wdn{i}", [128, 4, D], BF16) for i in range(2)]
            actT = P.sb("actT", [128, 4, TOWN], BF16)
            gg = [P.sb(f"gg{i}", [128, 512]) for i in range(2)]
            ll = [P.sb(f"ll{i}", [128, 512]) for i in range(2)]
            sgm = [P.sb(f"sgm{i}", [128, 512]) for i in range(2)]
            k = 0; kd = 0
            for e in range(32):
                for hf in range(2):
                    u = (2 * e + hf) % 2
                    wu = wup[u]; wd = wdn[u]
                    P.dma("pool", wu[:, :, :], V(C.w_up, C.w_up.h[e * D:(e + 1) * D, hf * 1024:(hf + 1) * 1024].rearrange("(kt k) n -> k kt n", k=128)))
                    P.dma("pool", wd[:, :, :], V(C.w_dn, C.w_dn.h[e * D + hf * 512:e * D + (hf + 1) * 512, :].rearrange("(ft f) n -> f ft n", f=128)))
                    for f4 in range(4):
                        ft = hf * 4 + f4
                        for tg in range(4):
                            ts_ = slice(tg * 512, (tg + 1) * 512)
                            pg = pb[(2 * k) % 6]; pl = pb[(2 * k) % 6 + 1]
                            g = gg[k % 2]; l = ll[k % 2]; s = sgm[k % 2]; k += 1
                            for kt in range(8):
                                P.mm(pg[:, :], wu[:, kt, f4 * 256:f4 * 256 + 256:2], C.h1T[:, kt, ts_], start=(kt == 0), stop=(kt == 7))
                            for kt in range(8):
                                P.mm(pl[:, :], wu[:, kt, f4 * 256 + 1:f4 * 256 + 256:2], C.h1T[:, kt, ts_], start=(kt == 0), stop=(kt == 7))
                            P.ts("dve", g[:, :], pg[:, :], bup[:, e, ft, 0:1], 7.0, ALU.add, ALU.min)
                            P.act(s[:, :], g[:, :], AF.Sigmoid, scale=1.702)
                            P.ts("dve", l[:, :], pl[:, :], bup[:, e, ft, 1:2], 7.0, ALU.add, ALU.min)
                            P.ts("dve", l[:, :], l[:, :], -7.0, 1.0, ALU.max, ALU.add)
                            P.tt("pool", g[:, :], g[:, :], s[:, :], ALU.mult)
                            P.tt("dve", actT[:, f4, ts_], g[:, :], l[:, :], ALU.mult)
                    for tt_ in range(16):
                        tsl = slice(tt_ * 128, (tt_ + 1) * 128)
                        for dh in range(2):
                            pd = pb[6 + kd % 2]; kd += 1
                            for f4 in range(4):
                                P.mm(pd[:, :], actT[:, f4, tsl], wd[:, f4, dh * 512:(dh + 1) * 512], start=(f4 == 0), stop=(f4 == 3))
                            P.stt(C.yacc[tt_][:, dh * 512:(dh + 1) * 512], pd[:, :], gates[:, tt_, e:e + 1],
                                  C.yacc[tt_][:, dh * 512:(dh + 1) * 512], ALU.mult, ALU.add)
        g2 = P.sb("g2", [128, D]); b2 = P.sb("b2", [128, D])
        P.dma("sp", g2[:, :], C.ln2g[:, :]); P.dma("sp", b2[:, :], C.ln2b[:, :])
        junk = P.sb("ojunk", [128, D])
        oo = [P.sb(f"oo{i}", [128, D]) for i in range(2)]
        for tt_ in range(16):
            o = oo[tt_ % 2]
            ln_tok(P, C.yacc[tt_][:, :], o[:, :], g2[:, :], b2[:, :], S, junk[:, :])
            P.dma("sp", C.out[C.off + tt_ * 128:C.off + (tt_ + 1) * 128, :], o[:, :])
            if getattr(C, "hT_next", None) is not None:
                emit_hT(P, C, o, C.hT_next, C.off + tt_ * 128, tt_)


STAGES_ALL = ("window", "ssm", "conv", "mem", "attproj", "att", "merge", "moe")
NQ = 4


def build_layer(stages=STAGES_ALL, dbg=(), quarters=(0, 1, 2, 3)):
    nc = bass.Bass("TRN2", target_bir_lowering=False)
    es = ExitStack()
    P = Prog(nc, es)
    C = Ctx()

    def inp(name, shape, dt=F32):
        t = P.dram(name, shape, dt, kind="ExternalInput")
        setattr(C, name, t)
        return t

    inp("hT", [D, PRE + T]); inp("hown", [T, D]); inp("npad", [4, 128, 1]); inp("padcol", [4, 128, 64])
    inp("memT", [D, 256]); inp("identin", [128, 128]); inp("tri", [128, 128])
    inp("w_in", [D, D_IN]); inp("kvg", [128, 1]); inp("kvgrow", [128, 128])
    inp("w_uk", [128, 512]); inp("w_uv", [128, 512]); inp("convw", [128, 4, 3]); inp("convb", [128, 4])
    inp("lamre", [128, 16]); inp("lamim", [128, 16]); inp("logdt", [128, 16])
    inp("bre", [128, 16, 16]); inp("bim", [128, 16, 16]); inp("cre", [128, 16, 16]); inp("cim", [128, 16, 16])
    inp("dskip", [128, 4]); inp("w_glu", [512, 512]); inp("bglu", [128, 4])
    inp("w_mem", [D, D]); inp("w_br", [4, 512, D]); inp("w_o", [D, D])
    inp("ln1g", [128, D]); inp("ln1b", [128, D]); inp("ln2g", [128, D]); inp("ln2b", [128, D])
    inp("w_r", [D, 32]); inp("b_r", [128, 32])
    inp("w_up", [32 * D, 2048]); inp("b_up", [128, 32, 8, 2]); inp("w_dn", [32 * D, D]); inp("b_dn", [32, D])
    C.out = P.dram("out", [T, D], F32, kind="ExternalOutput")

    def scratch(name, shape, dt):
        kind = "ExternalOutput" if name in dbg else "Internal"
        t = P.dram(name, shape, dt, kind=kind)
        setattr(C, name, t)
        return t

    scratch("uT", [512, WIN], F32); scratch("z32", [512, TOWN], F32); scratch("brT", [4, 512, TOWN], BF16)
    scratch("mT_d", [D, TOWN], BF16); scratch("qlat_d", [128, 8, TOWN], BF16); scratch("qidx_d", [32, 8, TOWN], BF16)

    C.pb = [P.ps(f"pb{i}", [128, 512], F32) for i in range(8)]
    C.ident = P.sb("ident", [128, 128]); P.dma("sp", C.ident[:, :], C.identin[:, :])
    C.ones_bf = P.sb("ones_bf", [128, 128], BF16); P.memset("dve", C.ones_bf[:, :], 1.0)

    for j in quarters:
        C.j = j
        C.off = j * TOWN
        C.pad = PRE - j * TOWN
        C.first_q = True
        with Stage(P):
            C.ckvT = P.sb(f"ckvT{j}", [128, WIN], BF16)
            C.ckv_tok = P.sb(f"ckv_tok{j}", [128, 64, 129], BF16)
            C.kidxT = P.sb(f"kidxT{j}", [32, WIN], BF16)
            C.widx = P.sb(f"widx{j}", [128, 16, 8])
            P.memset("dve", C.ckv_tok[:, :, 128:129], 1.0)
            if "window" in stages:
                stage_window(P, C)
            if "ssm" in stages:
                stage_ssm(P, C)
            with Stage(P):
                C.hTo = P.sb(f"hTo{j}", [128, 8, TOWN], BF16)
                P.dma("pool", C.hTo[:, :, :], V(C.hT, C.hT.h[:, C.off + PRE:C.off + WIN].rearrange("(kt k) t -> k kt t", k=128)))
                if "conv" in stages:
                    stage_conv(P, C)
                if "mem" in stages:
                    stage_mem(P, C)
                if "attproj" in stages:
                    stage_attproj(P, C)
            if "att" in stages:
                stage_att(P, C)
        with Stage(P):
            C.hTo = P.sb(f"hTo2{j}", [128, 8, TOWN], BF16)
            P.dma("pool", C.hTo[:, :, :], V(C.hT, C.hT.h[:, C.off + PRE:C.off + WIN].rearrange("(kt k) t -> k kt t", k=128)))
            if "merge" in stages or "merge_a" in stages:
                stage_merge_a(P, C)
        with Stage(P):
            C.yacc = [P.sb(f"yacc{j}_{i}", [128, D]) for i in range(16)]
            C.h1T = P.sb(f"h1T{j}", [128, 8, TOWN], BF16)
            C.rlog = P.sb(f"rlog{j}", [128, 16, 32])
            if "merge" in stages or "merge_b" in stages:
                stage_merge_b(P, C)
            if "moe" in stages:
                stage_moe(P, C)
    P.finish("sp", [C.out] + [getattr(C, n) for n in dbg])
    print("layer program instructions:", P.ninst, flush=True)
    es.close()
    return nc


def _rep(v, rows=128):
    return np.ascontiguousarray(np.broadcast_to(np.asarray(v, np.float32).reshape(1, -1), (rows, np.size(v))))


def _sm(a):
    a = np.asarray(a, np.float32)
    rest = a.shape[2:]
    return np.ascontiguousarray(a.reshape((16, 128) + rest).swapaxes(0, 1))


def layer_weights(inp, l):
    f = lambda k: np.asarray(inp[k][l], np.float32)
    w = {}
    w["w_in"] = np.ascontiguousarray(f("w_in"))
    w["kvg"] = np.ascontiguousarray(f("kv_norm_g").reshape(128, 1))
    w["kvgrow"] = _rep(f("kv_norm_g"))
    w["w_uk"] = np.ascontiguousarray(f("w_uk").reshape(128, 512))
    w["w_uv"] = np.ascontiguousarray(f("w_uv").reshape(128, 512))
    w["convw"] = np.ascontiguousarray(f("conv_w").T.reshape(4, 128, 3).transpose(1, 0, 2))
    w["convb"] = np.ascontiguousarray(f("conv_b").reshape(4, 128).T)
    w["lamre"] = _sm(f("lam_re")); w["lamim"] = _sm(f("lam_im"))
    w["logdt"] = _sm(np.broadcast_to(f("log_dt")[:, None], (32, 64)))
    w["bre"] = _sm(f("b_re")); w["bim"] = _sm(f("b_im"))
    w["cre"] = _sm(f("c_re").transpose(0, 2, 1)); w["cim"] = _sm(f("c_im").transpose(0, 2, 1))
    w["dskip"] = np.ascontiguousarray(f("d_skip").reshape(4, 128).T)
    w["w_glu"] = np.ascontiguousarray(f("w_glu"))
    w["bglu"] = np.ascontiguousarray(f("b_glu").reshape(4, 128).T)
    w["w_mem"] = np.ascontiguousarray(f("w_mem_kv"))
    w["w_br"] = np.ascontiguousarray(f("w_branch"))
    w["w_o"] = np.ascontiguousarray(f("w_o"))
    w["ln1g"] = _rep(f("ln1_g")); w["ln1b"] = _rep(f("ln1_b"))
    w["ln2g"] = _rep(f("ln2_g")); w["ln2b"] = _rep(f("ln2_b"))
    w["w_r"] = np.ascontiguousarray(f("w_router"))
    w["b_r"] = _rep(f("b_router"))
    w["w_up"] = np.ascontiguousarray(f("w_up").reshape(32 * D, 2048))
    w["b_up"] = np.ascontiguousarray(f("b_up").reshape(32, 8, 128, 2).transpose(2, 0, 1, 3))
    w["w_dn"] = np.ascontiguousarray(f("w_down").reshape(32 * D, D))
    w["b_dn"] = np.ascontiguousarray(f("b_down"))
    return w


def batch_inputs(h, mem, b):
    d = {}
    win = np.zeros((PRE + T, D), np.float32)
    win[PRE:] = h[b]
    d["hT"] = np.ascontiguousarray(win.T)
    d["hown"] = np.ascontiguousarray(h[b])
    npad = np.zeros((4, 128, 1), np.float32); padcol = np.zeros((4, 128, 64), np.float32)
    kidx = (np.arange(64)[None, :] * 128 + np.arange(128)[:, None])
    for j in range(4):
        pad = PRE - j * TOWN
        npad[j] = float(pad)
        padcol[j] = np.where(kidx < pad, NEG, 0.0)
    d["npad"] = npad; d["padcol"] = padcol
    d["memT"] = np.ascontiguousarray(np.asarray(mem[b], np.float32).T)
    d["identin"] = np.eye(128, dtype=np.float32)
    d["tri"] = np.where(np.arange(128)[None, :] > np.arange(128)[:, None], -BIG, 0.0).astype(np.float32)
    return d


def core_inputs(h, mem, c):
    b, j = c // 4, c % 4
    t0 = j * TOWN
    pad = PRE - t0
    win = np.zeros((WIN, D), np.float32)
    win[pad:] = h[b, 0:t0 + TOWN]
    d = {}
    d["hT"] = np.ascontiguousarray(win.T)
    d["hown"] = np.ascontiguousarray(h[b, t0:t0 + TOWN])
    d["npad"] = np.full((128, 1), float(pad), np.float32)
    kidx = (np.arange(64)[None, :] * 128 + np.arange(128)[:, None])
    d["padcol"] = np.where(kidx < pad, NEG, 0.0).astype(np.float32)
    d["memT"] = np.ascontiguousarray(np.asarray(mem[b], np.float32).T)
    d["identin"] = np.eye(128, dtype=np.float32)
    d["tri"] = np.where(np.arange(128)[None, :] > np.arange(128)[:, None], -BIG, 0.0).astype(np.float32)
    return d


_NC_CACHE = {}

W_NAMES = ("w_in", "kvg", "kvgrow", "w_uk", "w_uv", "convw", "convb", "lamre", "lamim", "logdt", "bre", "bim",
           "cre", "cim", "dskip", "w_glu", "bglu", "w_mem", "w_br", "w_o", "ln1g", "ln1b", "ln2g", "ln2b",
           "w_r", "b_r", "w_up", "b_up", "w_dn", "b_dn")
W_SHAPES = {"w_in": [D, D_IN], "kvg": [128, 1], "kvgrow": [128, 128], "w_uk": [128, 512], "w_uv": [128, 512],
            "convw": [128, 4, 3], "convb": [128, 4], "lamre": [128, 16], "lamim": [128, 16], "logdt": [128, 16],
            "bre": [128, 16, 16], "bim": [128, 16, 16], "cre": [128, 16, 16], "cim": [128, 16, 16],
            "dskip": [128, 4], "w_glu": [512, 512], "bglu": [128, 4], "w_mem": [D, D], "w_br": [4, 512, D],
            "w_o": [D, D], "ln1g": [128, D], "ln1b": [128, D], "ln2g": [128, D], "ln2b": [128, D],
            "w_r": [D, 32], "b_r": [128, 32], "w_up": [32 * D, 2048], "b_up": [128, 32, 8, 2],
            "w_dn": [32 * D, D], "b_dn": [32, D]}


def build_fused(depth=DEPTH, quarters=(0, 1, 2, 3)):
    nc = bass.Bass("TRN2", target_bir_lowering=False)
    es = ExitStack()
    P = Prog(nc, es)
    C = Ctx()
    x = P.dram("x", [T, D], F32, kind="ExternalInput")
    lng = P.dram("lng", [128, D], F32, kind="ExternalInput"); lnb = P.dram("lnb", [128, D], F32, kind="ExternalInput")
    C.npad = P.dram("npad", [4, 128, 1], F32, kind="ExternalInput")
    C.padcol = P.dram("padcol", [4, 128, 64], F32, kind="ExternalInput")
    C.memT = P.dram("memT", [D, 256], F32, kind="ExternalInput")
    C.identin = P.dram("identin", [128, 128], F32, kind="ExternalInput")
    C.tri = P.dram("tri", [128, 128], F32, kind="ExternalInput")
    WL = [{n: P.dram(f"{n}_{l}", W_SHAPES[n], F32, kind="ExternalInput") for n in W_NAMES} for l in range(depth)]
    out = P.dram("out", [T, D], F32, kind="ExternalOutput")
    hTb = [P.dram(f"hTbuf{i}", [D, PRE + T], F32) for i in range(2)]
    hb = [P.dram(f"hbuf{i}", [T, D], F32) for i in range(2)]
    for n, shp, dt in (("uT", [512, WIN], F32), ("z32", [512, TOWN], F32), ("brT", [4, 512, TOWN], BF16),
                       ("mT_d", [D, TOWN], BF16), ("qlat_d", [128, 8, TOWN], BF16), ("qidx_d", [32, 8, TOWN], BF16)):
        setattr(C, n, P.dram(n, shp, dt))
    C.wc_up = P.dram("wc_up", [64, 128, 8, 1024], BF16)
    C.wc_dn = P.dram("wc_dn", [64, 128, 4, D], BF16)
    C.pb = [P.ps(f"pb{i}", [128, 512], F32) for i in range(8)]
    C.ident = P.sb("ident", [128, 128]); P.dma("sp", C.ident[:, :], C.identin[:, :])
    C.ones_bf = P.sb("ones_bf", [128, 128], BF16); P.memset("dve", C.ones_bf[:, :], 1.0)
    with Stage(P):
        zt = P.sb("zt", [128, 2048]); P.memset("dve", zt[:, :], 0.0)
        for i in range(2):
            for r in range(8):
                for c in range(PRE // 2048):
                    P.dma("sp", hTb[i][r * 128:(r + 1) * 128, c * 2048:(c + 1) * 2048], zt[:, :])
        gs = P.sb("gs", [128, D]); bs = P.sb("bs", [128, D])
        P.dma("sp", gs[:, :], lng[:, :]); P.dma("sp", bs[:, :], lnb[:, :])
        S = Small(P, "lnin")
        junk = P.sb("junk", [128, D])
        xs = [P.sb(f"x{i}", [128, D]) for i in range(2)]
        os_ = [P.sb(f"o{i}", [128, D]) for i in range(2)]
        for i in range(T // 128):
            xt = xs[i % 2]; ot = os_[i % 2]
            P.dma("sp", xt[:, :], x[i * 128:(i + 1) * 128, :])
            ln_tok(P, xt[:, :], ot[:, :], gs[:, :], bs[:, :], S, junk[:, :])
            P.dma("sp", hb[0][i * 128:(i + 1) * 128, :], ot[:, :])
            emit_hT(P, C, ot, hTb[0], i * 128, i)
    for l in range(depth):
        for n in W_NAMES:
            setattr(C, n, WL[l][n])
        C.hT = hTb[l % 2]; C.hown = hb[l % 2]
        last = (l == depth - 1)
        C.out = out if last else hb[(l + 1) % 2]
        C.hT_next = None if last else hTb[(l + 1) % 2]
        for j in quarters:
            C.j = j
            C.off = j * TOWN
            C.pad = PRE - j * TOWN
            C.first_q = (j == quarters[0])
            with Stage(P):
                C.ckvT = P.sb("ckvT", [128, WIN], BF16)
                C.ckv_tok = P.sb("ckv_tok", [128, 64, 129], BF16)
                C.kidxT = P.sb("kidxT", [32, WIN], BF16)
                C.widx = P.sb("widx", [128, 16, 8])
                P.memset("dve", C.ckv_tok[:, :, 128:129], 1.0)
                stage_window(P, C)
                stage_ssm(P, C)
                with Stage(P):
                    C.hTo = P.sb("hTo", [128, 8, TOWN], BF16)
                    P.dma("pool", C.hTo[:, :, :], V(C.hT, C.hT.h[:, C.off + PRE:C.off + WIN].rearrange("(kt k) t -> k kt t", k=128)))
                    stage_conv(P, C)
                    stage_mem(P, C)
                    stage_attproj(P, C)
                stage_att(P, C)
            with Stage(P):
                C.hTo = P.sb("hTo2", [128, 8, TOWN], BF16)
                P.dma("pool", C.hTo[:, :, :], V(C.hT, C.hT.h[:, C.off + PRE:C.off + WIN].rearrange("(kt k) t -> k kt t", k=128)))
                stage_merge_a(P, C)
            with Stage(P):
                C.yacc = [P.sb(f"yacc{i}", [128, D]) for i in range(16)]
                C.h1T = P.sb("h1T", [128, 8, TOWN], BF16)
                C.rlog = P.sb("rlog", [128, 16, 32])
                stage_merge_b(P, C)
                stage_moe(P, C)
    P.finish("sp", [out])
    print("fused program instructions:", P.ninst, flush=True)
    es.close()
    return nc


def fused_inputs(inputs, b):
    x = np.asarray(inputs["x"], np.float32)
    d = {"x": np.ascontiguousarray(x[b]), "lng": _rep(inputs["ln_in_g"]), "lnb": _rep(inputs["ln_in_b"])}
    npad = np.zeros((4, 128, 1), np.float32); padcol = np.zeros((4, 128, 64), np.float32)
    kidx = (np.arange(64)[None, :] * 128 + np.arange(128)[:, None])
    for j in range(4):
        pad = PRE - j * TOWN
        npad[j] = float(pad)
        padcol[j] = np.where(kidx < pad, NEG, 0.0)
    d["npad"] = npad; d["padcol"] = padcol
    d["memT"] = np.ascontiguousarray(np.asarray(inputs["mem"], np.float32)[b].T)
    d["identin"] = np.eye(128, dtype=np.float32)
    d["tri"] = np.where(np.arange(128)[None, :] > np.arange(128)[:, None], -BIG, 0.0).astype(np.float32)
    return d


def kernel(**inputs):
    if "fused" not in _NC_CACHE:
        _NC_CACHE["fused"] = build_fused()
    maps = [fused_inputs(inputs, b) for b in range(2)]
    for l in range(DEPTH):
        w = layer_weights(inputs, l)
        for n in W_NAMES:
            for m in maps:
                m[f"{n}_{l}"] = w[n]
    res = run_bass_kernel_spmd(_NC_CACHE["fused"], maps, core_ids=[0, 1])
    return np.stack([np.asarray(r["out"]) for r in res.results]).astype(np.float32)
```

```python
from contextlib import ExitStack
import math
import numpy as np
import concourse.bass as bass
import concourse.mybir as mybir
from concourse.bass_utils import run_bass_kernel_spmd

F32 = mybir.dt.float32
BF16 = mybir.dt.bfloat16
ALU = mybir.AluOpType
AF = mybir.ActivationFunctionType
AX = mybir.AxisListType

NCORES = 8
D = 1024
T = 8192
TOWN = 2048
WIN = 8192
PRE = WIN - TOWN
DEPTH = 4
D_IN = 7592
C_Q, C_CKV, C_QI, C_KI, C_WI, C_CU, C_GB, C_GC, C_SU, C_MQ, C_GATE = (
    0, 512, 640, 896, 928, 936, 1448, 1960, 2472, 2984, 3496)
LN_EPS = 1e-5
ALPHA = (2 * DEPTH) ** 0.25
NEG = -30000.0
BIG = 1.0e30
TWO_PI = 2.0 * math.pi
MAGIC = 12582912.0
NBIS = 17


class V:
    def __init__(self, t, ap):
        self.t = t
        self.ap = ap


class TT:
    def __init__(self, h, name):
        self.h = h
        self.name = name
        self.w = None
        self.r = []

    def __getitem__(self, idx):
        return V(self, self.h[idx])


def _ap(x):
    return x.ap if isinstance(x, V) else x


def _ts(*xs):
    return [x.t for x in xs if isinstance(x, V)]


class Prog:
    def __init__(self, nc, es):
        self.nc = nc
        self.es = es
        self.eng = {"pe": nc.tensor, "act": nc.scalar, "dve": nc.vector,
                    "pool": nc.gpsimd, "sp": nc.sync}
        self.sem = {}
        self.cnt = {}
        self.waited = {e: {} for e in self.eng}
        for e in self.eng:
            self.sem[e] = es.enter_context(nc.semaphore("s_" + e))
            self.cnt[e] = 0
        self.ninst = 0
        self.uid = 0

    def sb(self, name, shape, dt=F32):
        self.uid += 1
        return TT(self.es.enter_context(self.nc.sbuf_tensor(f"{name}_u{self.uid}", shape, dt)), name)

    def ps(self, name, shape, dt=F32):
        return TT(self.es.enter_context(self.nc.psum_tensor(name, shape, dt)), name)

    def dram(self, name, shape, dt=F32, kind="Internal"):
        return TT(self.nc.dram_tensor(name, shape, dt, kind=kind), name)

    def dsem(self, name):
        key = "d_" + name
        if key not in self.sem:
            self.sem[key] = self.es.enter_context(self.nc.semaphore(key))
            self.cnt[key] = 0
        return key

    def _wait(self, e, deps):
        need = {}
        for d in deps:
            if d is None:
                continue
            k, v = d
            if k == e and e == "pe":
                continue
            if v > need.get(k, 0):
                need[k] = v
        for k, v in need.items():
            if self.waited[e].get(k, 0) >= v:
                continue
            self.eng[e].wait_ge(self.sem[k], v)
            self.waited[e][k] = v

    def _deps(self, reads, writes):
        deps = []
        for t in reads:
            deps.append(t.w)
        for t in writes:
            deps.append(t.w)
            deps.extend(t.r)
        return deps

    def op(self, e, fn, reads=(), writes=()):
        self._wait(e, self._deps(reads, writes))
        ins = fn(self.eng[e])
        self.cnt[e] += 1
        ins.then_inc(self.sem[e], 1)
        d = (e, self.cnt[e])
        for t in reads:
            t.r.append(d)
        for t in writes:
            t.w = d
            t.r = []
        self.ninst += 1
        return d

    def dma(self, q, out, in_, sem=None, **kw):
        self._wait(q, self._deps([in_.t], [out.t]))
        key = self.dsem(sem or out.t.name)
        ins = self.eng[q].dma_start(out=out.ap, in_=in_.ap, **kw)
        self.cnt[key] += 16
        ins.then_inc(self.sem[key], 16)
        d = (key, self.cnt[key])
        in_.t.r.append(d)
        out.t.w = d
        out.t.r = []
        self.ninst += 1
        return d

    def allgather(self, out, in_, n=NCORES):
        self._wait("pool", self._deps([in_.t], [out.t]))
        key = self.dsem(out.t.name)
        ins = self.nc.gpsimd.collective_compute("AllGather", op=ALU.bypass, replica_groups=[list(range(n))],
                                                ins=[in_.ap], outs=[out.ap])
        self.cnt[key] += 16
        ins.then_inc(self.sem[key], 16)
        d = (key, self.cnt[key])
        in_.t.r.append(d)
        out.t.w = d
        out.t.r = []
        self.ninst += 1
        return d

    def finish(self, e, tensors):
        self._wait(e, [t.w for t in tensors])

    def mm(self, out, lhsT, rhs, start=True, stop=True):
        return self.op("pe", lambda e: e.matmul(out.ap, lhsT.ap, rhs.ap, start=start, stop=stop),
                       reads=[lhsT.t, rhs.t], writes=[out.t])

    def tr(self, out, in_, ident):
        return self.op("pe", lambda e: e.transpose(out.ap, in_.ap, ident.ap),
                       reads=[in_.t, ident.t], writes=[out.t])

    def act(self, out, in_, func, bias=0.0, scale=1.0, accum=None, e="act"):
        rd = _ts(in_, bias, scale)
        wr = _ts(out, accum)
        kw = {}
        if accum is not None:
            kw["accum_out"] = accum.ap
        return self.op("act", lambda g: g.activation(out=out.ap, in_=in_.ap, func=func,
                                                     bias=_ap(bias), scale=_ap(scale), **kw),
                       reads=rd, writes=wr)

    def ts(self, e, out, in0, s1, s2, op0, op1=None, accum=None):
        rd = _ts(in0, s1, s2)
        wr = _ts(out, accum)
        kw = {}
        if op1 is not None:
            kw["op1"] = op1
        if accum is not None:
            kw["accum_out"] = accum.ap
        return self.op(e, lambda g: g.tensor_scalar(out.ap, in0.ap, _ap(s1), _ap(s2), op0, **kw),
                       reads=rd, writes=wr)

    def tt(self, e, out, in0, in1, op):
        return self.op(e, lambda g: g.tensor_tensor(out.ap, in0.ap, in1.ap, op),
                       reads=_ts(in0, in1), writes=_ts(out))

    def stt(self, out, in0, s, in1, op0, op1):
        return self.op("dve", lambda g: g.scalar_tensor_tensor(out.ap, in0.ap, _ap(s), in1.ap, op0, op1),
                       reads=_ts(in0, s, in1), writes=_ts(out))

    def cp(self, e, out, in_):
        if e == "act":
            return self.act(out, in_, AF.Copy)
        return self.op(e, lambda g: g.tensor_copy(out.ap, in_.ap), reads=_ts(in_), writes=_ts(out))

    def red(self, out, in_, op, axis=AX.X):
        return self.op("dve", lambda g: g.tensor_reduce(out.ap, in_.ap, axis, op),
                       reads=_ts(in_), writes=_ts(out))

    def scan(self, out, d0, d1, init, op0, op1):
        return self.op("dve", lambda g: g.tensor_tensor_scan(out.ap, d0.ap, d1.ap, _ap(init), op0, op1),
                       reads=_ts(d0, d1, init), writes=_ts(out))

    def recip(self, out, in_):
        return self.op("dve", lambda g: g.reciprocal(out.ap, in_.ap), reads=_ts(in_), writes=_ts(out))

    def memset(self, e, out, val):
        return self.op(e, lambda g: g.memset(out.ap, val), reads=[], writes=_ts(out))

    def max8(self, out, in_):
        return self.op("dve", lambda g: g.max(out.ap, in_.ap), reads=_ts(in_), writes=_ts(out))


class Small:
    def __init__(self, P, name, n=64):
        self.P = P
        self.name = name
        self.k = 0

    def col(self, w=1):
        self.k += 1
        return self.P.sb(f"{self.name}_{self.k}", [128, w], F32)


def ln_tok(P, x, out, grep, brep, S, junk):
    msum = S.col(); negmean = S.col(); ss = S.col(); std = S.col(); rstd = S.col()
    P.red(msum[:, :], x, ALU.add)
    P.ts("dve", negmean[:, :], msum[:, :], -1.0 / D, None, ALU.mult)
    P.act(junk, x, AF.Square, bias=negmean[:, :], scale=1.0, accum=ss[:, :])
    P.ts("dve", std[:, :], ss[:, :], 1.0 / D, LN_EPS, ALU.mult, ALU.add)
    P.act(std[:, :], std[:, :], AF.Sqrt)
    P.recip(rstd[:, :], std[:, :])
    P.ts("dve", out, x, negmean[:, :], rstd[:, :], ALU.add, ALU.mult)
    P.tt("dve", out, out, grep, ALU.mult)
    P.tt("dve", out, out, brep, ALU.add)


def build_ln_in():
    nc = bass.Bass("TRN2", target_bir_lowering=False)
    es = ExitStack()
    P = Prog(nc, es)
    x = P.dram("x", [TOWN, D], F32, kind="ExternalInput")
    g = P.dram("g", [128, D], F32, kind="ExternalInput")
    b = P.dram("b", [128, D], F32, kind="ExternalInput")
    y = P.dram("y", [TOWN, D], F32, kind="ExternalOutput")
    gs = P.sb("gs", [128, D]); bs = P.sb("bs", [128, D])
    P.dma("sp", gs[:, :], g[:, :]); P.dma("sp", bs[:, :], b[:, :])
    S = Small(P, "lnin")
    junk = P.sb("junk", [128, D])
    xs = [P.sb(f"x{i}", [128, D]) for i in range(2)]
    os_ = [P.sb(f"o{i}", [128, D]) for i in range(2)]
    for i in range(TOWN // 128):
        xt = xs[i % 2]; ot = os_[i % 2]
        P.dma("sp", xt[:, :], x[i * 128:(i + 1) * 128, :])
        ln_tok(P, xt[:, :], ot[:, :], gs[:, :], bs[:, :], S, junk[:, :])
        P.dma("sp", y[i * 128:(i + 1) * 128, :], ot[:, :])
    P.finish("sp", [y])
    es.close()
    return nc


class Ctx:
    pass


def barrier(P):
    allk = [(k, v) for k, v in P.cnt.items() if v > 0]
    for e in P.eng:
        P._wait(e, allk)


class Stage:
    def __init__(self, P):
        self.P = P

    def __enter__(self):
        self.old = self.P.es
        self.es = ExitStack()
        self.P.es = self.es
        return self

    def __exit__(self, *a):
        barrier(self.P)
        self.P.es = self.old
        self.es.close()
        return False


def load_w(P, dst, src_ap_tt, q="pool"):
    return P.dma(q, dst, src_ap_tt)


def wview(w_in, c0, n):
    return V(w_in, w_in.h[:, c0:c0 + n].rearrange("(kt k) n -> k kt n", k=128))


def stage_window(P, C):
    pb = C.pb
    with Stage(P):
        wsu = P.sb("wsu", [128, 8, 512], BF16)
        wck = P.sb("wck", [128, 8, 128], BF16)
        wki = P.sb("wki", [128, 8, 32], BF16)
        P.dma("pool", wsu[:, :, :], wview(C.w_in, C_SU, 512))
        P.dma("pool", wck[:, :, :], wview(C.w_in, C_CKV, 128))
        P.dma("pool", wki[:, :, :], wview(C.w_in, C_KI, 32))
        kvg = P.sb("kvg_s", [128, 1]); P.dma("sp", kvg[:, :], C.kvg[:, :])
        kvgrow = P.sb("kvgrow_s", [128, 128]); P.dma("sp", kvgrow[:, :], C.kvgrow[:, :])
        hTc = [P.sb(f"hTc{i}", [128, 8, 512], BF16) for i in range(2)]
        ust = [P.sb(f"ust{i}", [128, 512]) for i in range(2)]
        sq = P.sb("sq", [128, 512], BF16)
        rst = P.sb("rst", [128, 512])
        junk = P.sb("wjunk", [128, 128])
        S = Small(P, "win")
        for g in range(C.pad // 512, WIN // 512):
            h = hTc[g % 2]
            P.dma("pool", h[:, :, :], V(C.hT, C.hT.h[:, C.off + g * 512:C.off + (g + 1) * 512].rearrange("(kt k) t -> k kt t", k=128)))
            for ct in range(4):
                ps = pb[ct % 2]
                for kt in range(8):
                    P.mm(ps[:, :], wsu[:, kt, ct * 128:(ct + 1) * 128], h[:, kt, :], start=(kt == 0), stop=(kt == 7))
                u = ust[ct % 2]
                P.cp("act", u[:, :], ps[:, :])
                P.dma("sp", C.uT[ct * 128:(ct + 1) * 128, g * 512:(g + 1) * 512], u[:, :])
            ps = pb[2]
            for kt in range(8):
                P.mm(ps[:, :], wck[:, kt, :], h[:, kt, :], start=(kt == 0), stop=(kt == 7))
            P.act(sq[:, :], ps[:, :], AF.Square)
            P.mm(pb[3][:, :], C.ones_bf[:, :], sq[:, :])
            P.ts("dve", rst[:, :], pb[3][:, :], 1.0 / 128, LN_EPS, ALU.mult, ALU.add)
            P.act(rst[:, :], rst[:, :], AF.Sqrt)
            P.recip(rst[:, :], rst[:, :])
            P.stt(C.ckvT[:, g * 512:(g + 1) * 512], ps[:, :], kvg[:, :], rst[:, :], ALU.mult, ALU.mult)
            ps = pb[4]
            for tt_ in range(4):
                for kt in range(8):
                    P.mm(ps[:, tt_ * 128:(tt_ + 1) * 128], h[:, kt, tt_ * 128:(tt_ + 1) * 128], wck[:, kt, :],
                         start=(kt == 0), stop=(kt == 7))
            for tt_ in range(4):
                ss = S.col(); rs = S.col()
                P.act(junk[:, :], ps[:, tt_ * 128:(tt_ + 1) * 128], AF.Square, accum=ss[:, :])
                P.ts("dve", rs[:, :], ss[:, :], 1.0 / 128, LN_EPS, ALU.mult, ALU.add)
                P.act(rs[:, :], rs[:, :], AF.Sqrt)
                P.recip(rs[:, :], rs[:, :])
                P.stt(C.ckv_tok[:, g * 4 + tt_, 0:128], ps[:, tt_ * 128:(tt_ + 1) * 128], rs[:, :], kvgrow[:, :],
                      ALU.mult, ALU.mult)
            ps = pb[5]
            for kt in range(8):
                P.mm(ps[0:32, :], wki[:, kt, :], h[:, kt, :], start=(kt == 0), stop=(kt == 7))
            P.cp("act", C.kidxT[0:32, g * 512:(g + 1) * 512], ps[0:32, :])


SEG = 512
NSEG = WIN // SEG
OWN0 = PRE // SEG


def reduce_turns(P, out_f, u, tmp):
    P.ts("dve", tmp, u, MAGIC, None, ALU.add)
    P.ts("dve", tmp, tmp, MAGIC, None, ALU.subtract)
    P.tt("dve", out_f, u, tmp, ALU.subtract)


def sincos(P, S_out, C_out, f, tmp):
    P.act(S_out, f, AF.Sin, scale=TWO_PI)
    P.act(tmp, f, AF.Abs)
    P.act(C_out, tmp, AF.Sin, scale=-TWO_PI, bias=C_halfpi)


C_halfpi = None


def stage_ssm(P, C):
    global C_halfpi
    pb = C.pb
    with Stage(P):
        halfpi = P.sb("halfpi", [128, 1]); P.memset("dve", halfpi[:, :], math.pi / 2)
        C_halfpi = halfpi[:, :]
        lhs_bu = P.sb("lhs_bu", [128, 16, 2, 128], BF16)
        lhs_c = P.sb("lhs_c", [128, 16, 2, 128], BF16)
        rcol = P.sb("rcol", [128, 16]); fturn = P.sb("fturn", [128, 16]); f0 = P.sb("f0", [128, 16, NSEG])
        dsk = P.sb("dsk", [128, 4]); P.dma("sp", dsk[:, :], C.dskip[:, :])
        bgl = P.sb("bgl", [128, 4]); P.dma("sp", bgl[:, :], C.bglu[:, :])
        with Stage(P):
            def ld(name, src, shape):
                t = P.sb(name, shape);
                P.dma("sp", t[tuple(slice(None) for _ in shape)], src[tuple(slice(None) for _ in shape)])
                return t
            lre = ld("lre", C.lamre, [128, 16]); lim = ld("lim", C.lamim, [128, 16]); ldt = ld("ldt", C.logdt, [128, 16])
            bre = ld("bre_s", C.bre, [128, 16, 16]); bim = ld("bim_s", C.bim, [128, 16, 16])
            cre = ld("cre_s", C.cre, [128, 16, 16]); cim = ld("cim_s", C.cim, [128, 16, 16])
            n16 = lambda nm: P.sb(nm, [128, 16])
            dt = n16("dt"); lnr = n16("lnr"); th = n16("th"); tmp = n16("tmp16"); ff = n16("ff")
            sn = n16("sn"); cs = n16("cs"); ar = n16("ar"); ai = n16("ai"); den = n16("den")
            kr = n16("kr"); ki = n16("ki"); nki = n16("nki"); t1 = n16("t1_16"); t2 = n16("t2_16")
            A = slice(None)
            P.act(dt[:, :], ldt[:, :], AF.Exp)
            P.tt("dve", lnr[:, :], lre[:, :], dt[:, :], ALU.mult)
            P.tt("dve", th[:, :], lim[:, :], dt[:, :], ALU.mult)
            P.ts("dve", fturn[:, :], th[:, :], 1.0 / TWO_PI, None, ALU.mult)
            P.act(rcol[:, :], lnr[:, :], AF.Exp)
            reduce_turns(P, ff[:, :], fturn[:, :], tmp[:, :])
            sincos(P, sn[:, :], cs[:, :], ff[:, :], tmp[:, :])
            P.tt("dve", ar[:, :], rcol[:, :], cs[:, :], ALU.mult)
            P.tt("dve", ai[:, :], rcol[:, :], sn[:, :], ALU.mult)
            P.ts("dve", ar[:, :], ar[:, :], -1.0, None, ALU.add)
            P.tt("dve", den[:, :], lre[:, :], lre[:, :], ALU.mult)
            P.tt("dve", t1[:, :], lim[:, :], lim[:, :], ALU.mult)
            P.tt("dve", den[:, :], den[:, :], t1[:, :], ALU.add)
            P.recip(den[:, :], den[:, :])
            P.tt("dve", t1[:, :], ar[:, :], lre[:, :], ALU.mult)
            P.tt("dve", t2[:, :], ai[:, :], lim[:, :], ALU.mult)
            P.tt("dve", kr[:, :], t1[:, :], t2[:, :], ALU.add)
            P.tt("dve", kr[:, :], kr[:, :], den[:, :], ALU.mult)
            P.tt("dve", t1[:, :], ai[:, :], lre[:, :], ALU.mult)
            P.tt("dve", t2[:, :], ar[:, :], lim[:, :], ALU.mult)
            P.tt("dve", ki[:, :], t1[:, :], t2[:, :], ALU.subtract)
            P.tt("dve", ki[:, :], ki[:, :], den[:, :], ALU.mult)
            P.ts("dve", nki[:, :], ki[:, :], -1.0, None, ALU.mult)
            for q in range(NSEG):
                P.ts("dve", f0[:, :, q], fturn[:, :], float(SEG * q), None, ALU.mult)
            ftmp = P.sb("ftmp", [128, 16, NSEG])
            reduce_turns(P, f0[:, :, :], f0[:, :, :], ftmp[:, :, :])
            bbr = P.sb("bbr", [128, 16, 16]); bbi = P.sb("bbi", [128, 16, 16]); tb = P.sb("tb", [128, 16])
            Sp = P.sb("Sp", [128, 32, 128])
            P.memset("pool", Sp[:, :, :], 0.0)
            lcf = P.sb("lcf", [128, 32, 128])
            P.memset("pool", lcf[:, :, :], 0.0)
            ncim = P.sb("ncim", [128, 16, 16])
            P.ts("dve", ncim[:, :, :], cim[:, :, :], -1.0, None, ALU.mult)
            for i in range(16):
                P.ts("dve", tb[:, :], bre[:, i, :], kr[:, i:i + 1], None, ALU.mult)
                P.stt(bbr[:, i, :], bim[:, i, :], nki[:, i:i + 1], tb[:, :], ALU.mult, ALU.add)
                P.ts("dve", tb[:, :], bim[:, i, :], kr[:, i:i + 1], None, ALU.mult)
                P.stt(bbi[:, i, :], bre[:, i, :], ki[:, i:i + 1], tb[:, :], ALU.mult, ALU.add)
                c0 = 32 * (i % 4)
                for gg in range(2):
                    rows = slice(64 * gg, 64 * gg + 64)
                    cols = slice(c0 + 16 * gg, c0 + 16 * gg + 16)
                    P.cp("dve", Sp[rows, 2 * i, cols], bbr[rows, i, :])
                    P.cp("dve", Sp[rows, 2 * i + 1, cols], bbi[rows, i, :])
                    P.cp("dve", lcf[rows, 2 * i, cols], cre[rows, i, :])
                    P.cp("dve", lcf[rows, 2 * i + 1, cols], ncim[rows, i, :])
            for i in range(16):
                for ri in range(2):
                    ps = pb[(2 * i + ri) % 4]
                    P.tr(ps[:, 0:128], Sp[:, 2 * i + ri, :], C.ident[:, :])
                    P.cp("act", lhs_bu[:, i, ri, :], ps[:, 0:128])
                    P.cp("pool", lhs_c[:, i, ri, :], lcf[:, 2 * i + ri, :])
        iota = P.sb("iota", [128, SEG])
        P.op("pool", lambda g: g.iota(iota.h[:, :], [[1, SEG]], 0, channel_multiplier=0, allow_small_or_imprecise_dtypes=True),
             reads=[], writes=[iota])
        ones = P.sb("ones_s", [128, SEG]); P.memset("dve", ones[:, :], 1.0)
        rbc = P.sb("rbc", [128, SEG])
        uTt = P.sb("uTt", [128, WIN], BF16)
        mk = lambda nm, dt_=F32: P.sb(nm, [128, SEG], dt_)
        tu = mk("tu"); tn = mk("tn"); tf = mk("tf"); tS = mk("tS"); tC = mk("tC")
        t1 = mk("r1"); t2 = mk("r2"); t3 = mk("r3"); t4 = mk("r4")
        zs = [[mk(f"zs{a}{b}") for b in range(2)] for a in range(2)]
        zro = P.sb("zro", [128, TOWN]); zio = P.sb("zio", [128, TOWN])
        hr = mk("hr", BF16); hi = mk("hi", BF16)
        zbf = P.sb("zbf", [128, 4, TOWN], BF16)
        u32 = P.sb("u32", [128, TOWN]); yy = P.sb("yy", [128, TOWN]); y2 = P.sb("y2", [128, TOWN])
        for i in range(16):
            ct = i // 4
            if i % 4 == 0:
                P.dma("pool", uTt[:, C.pad:WIN], C.uT[ct * 128:(ct + 1) * 128, C.pad:WIN])
                P.dma("sp", u32[:, :], C.uT[ct * 128:(ct + 1) * 128, PRE:WIN])
            P.ts("dve", rbc[:, :], ones[:, :], rcol[:, i:i + 1], None, ALU.mult)
            prev = None
            for q in range(C.pad // SEG, NSEG):
                own = q >= OWN0
                P.ts("dve", tu[:, :], iota[:, :], fturn[:, i:i + 1], f0[:, i, q:q + 1], ALU.mult, ALU.add)
                reduce_turns(P, tf[:, :], tu[:, :], tn[:, :])
                sincos(P, tS[:, :], tC[:, :], tf[:, :], tn[:, :])
                pr = pb[q % 2]; pi_ = pb[2 + q % 2]
                P.mm(pr[:, :], lhs_bu[:, i, 0, :], uTt[:, q * SEG:(q + 1) * SEG])
                P.mm(pi_[:, :], lhs_bu[:, i, 1, :], uTt[:, q * SEG:(q + 1) * SEG])
                P.tt("dve", t1[:, :], tC[:, :], pr[:, :], ALU.mult)
                P.tt("dve", t2[:, :], tS[:, :], pi_[:, :], ALU.mult)
                P.tt("dve", t3[:, :], tC[:, :], pi_[:, :], ALU.mult)
                P.tt("dve", t4[:, :], tS[:, :], pr[:, :], ALU.mult)
                P.tt("pool", t1[:, :], t1[:, :], t2[:, :], ALU.add)
                P.tt("pool", t3[:, :], t3[:, :], t4[:, :], ALU.subtract)
                if own:
                    o = (q - OWN0) * SEG
                    zr_o = zro[:, o:o + SEG]; zi_o = zio[:, o:o + SEG]
                else:
                    zr_o = zs[0][q % 2][:, :]; zi_o = zs[1][q % 2][:, :]
                ir = 0.0 if prev is None else prev[0]
                ii = 0.0 if prev is None else prev[1]
                P.scan(zr_o, rbc[:, :], t1[:, :], ir, ALU.mult, ALU.add)
                P.scan(zi_o, rbc[:, :], t3[:, :], ii, ALU.mult, ALU.add)
                if own:
                    prev = (zro[:, o + SEG - 1:o + SEG], zio[:, o + SEG - 1:o + SEG])
                else:
                    prev = (zs[0][q % 2][:, SEG - 1:SEG], zs[1][q % 2][:, SEG - 1:SEG])
                if own:
                    P.tt("pool", t2[:, :], tC[:, :], zr_o, ALU.mult)
                    P.tt("pool", t4[:, :], tS[:, :], zi_o, ALU.mult)
                    P.tt("dve", hr[:, :], t2[:, :], t4[:, :], ALU.subtract)
                    P.tt("pool", t2[:, :], tS[:, :], zr_o, ALU.mult)
                    P.tt("pool", t4[:, :], tC[:, :], zi_o, ALU.mult)
                    P.tt("dve", hi[:, :], t2[:, :], t4[:, :], ALU.add)
                    py = pb[4 + (q - OWN0)]
                    P.mm(py[:, :], lhs_c[:, i, 0, :], hr[:, :], start=(i % 4 == 0), stop=False)
                    P.mm(py[:, :], lhs_c[:, i, 1, :], hi[:, :], start=False, stop=(i % 4 == 3))
            if i % 4 == 3:
                for s in range(4):
                    sl = slice(s * SEG, (s + 1) * SEG)
                    P.stt(yy[:, sl], u32[:, sl], dsk[:, ct:ct + 1], pb[4 + s][:, :], ALU.mult, ALU.add)
                P.tt("pool", y2[:, :], yy[:, :], yy[:, :], ALU.mult)
                P.ts("dve", y2[:, :], y2[:, :], 0.0713548163, 1.5957691216, ALU.mult, ALU.add)
                P.tt("dve", y2[:, :], y2[:, :], yy[:, :], ALU.mult)
                P.act(y2[:, :], y2[:, :], AF.Sigmoid)
                P.tt("dve", yy[:, :], yy[:, :], y2[:, :], ALU.mult)
                P.cp("act", zbf[:, ct, :], yy[:, :])
                P.dma("sp", C.z32[ct * 128:(ct + 1) * 128, :], yy[:, :])
        wgl = P.sb("wgl", [128, 4, 512], BF16)
        P.dma("pool", wgl[:, :, :], V(C.w_glu, C.w_glu.h[:, :].rearrange("(kt k) n -> k kt n", k=128)))
        for co in range(4):
            P.dma("sp", u32[:, :], C.z32[co * 128:(co + 1) * 128, :])
            for tg in range(4):
                ps = pb[tg % 4]
                sl = slice(tg * 512, (tg + 1) * 512)
                for kt in range(4):
                    P.mm(ps[:, :], wgl[:, kt, co * 128:(co + 1) * 128], zbf[:, kt, sl], start=(kt == 0), stop=(kt == 3))
                P.act(y2[:, sl], ps[:, :], AF.Sigmoid, bias=bgl[:, co:co + 1])
                P.tt("dve", hr[:, :], u32[:, sl], y2[:, sl], ALU.mult)
                P.dma("sp", C.brT[2, co * 128:(co + 1) * 128, sl], hr[:, :])


def proj_fm(P, C, ps, w, c0, n, tg, rows=None):
    for kt in range(8):
        P.mm(ps[0:n, :], w[:, kt, c0:c0 + n], C.hTo[:, kt, tg * 512:(tg + 1) * 512], start=(kt == 0), stop=(kt == 7))


def stage_conv(P, C):
    pb = C.pb
    with Stage(P):
        wcu = P.sb("wcu", [128, 8, 512], BF16); wgb = P.sb("wgb", [128, 8, 512], BF16); wgc = P.sb("wgc", [128, 8, 512], BF16)
        P.dma("pool", wcu[:, :, :], wview(C.w_in, C_CU, 512))
        P.dma("pool", wgb[:, :, :], wview(C.w_in, C_GB, 512))
        P.dma("pool", wgc[:, :, :], wview(C.w_in, C_GC, 512))
        cw = P.sb("cw", [128, 4, 3]); P.dma("sp", cw[:, :, :], C.convw[:, :, :])
        cb = P.sb("cb", [128, 4]); P.dma("sp", cb[:, :], C.convb[:, :])
        hh = P.sb("hhalo", [128, 8, 2], BF16)
        P.dma("pool", hh[:, :, :], V(C.hT, C.hT.h[:, C.off + PRE - 2:C.off + PRE].rearrange("(kt k) t -> k kt t", k=128)))
        v = P.sb("cv", [128, TOWN + 2]); us = P.sb("cus", [128, 512]); y = P.sb("cy", [128, TOWN])
        ob = P.sb("cob", [128, 512], BF16)
        for ct in range(4):
            cs = slice(ct * 128, (ct + 1) * 128)
            for kt in range(8):
                P.mm(pb[0][:, 0:2], wcu[:, kt, cs], hh[:, kt, :], start=(kt == 0), stop=(kt == 7))
            for kt in range(8):
                P.mm(pb[1][:, 0:2], wgc[:, kt, cs], hh[:, kt, :], start=(kt == 0), stop=(kt == 7))
            P.cp("act", us[:, 0:2], pb[0][:, 0:2])
            P.tt("dve", v[:, 0:2], us[:, 0:2], pb[1][:, 0:2], ALU.mult)
            for tg in range(4):
                proj_fm(P, C, pb[2], wcu, ct * 128, 128, tg)
                proj_fm(P, C, pb[3], wgc, ct * 128, 128, tg)
                P.cp("act", us[:, :], pb[2][:, :])
                P.tt("dve", v[:, 2 + tg * 512:2 + (tg + 1) * 512], us[:, :], pb[3][:, :], ALU.mult)
            P.ts("dve", y[:, :], v[:, 2:TOWN + 2], cw[:, ct, 2:3], cb[:, ct:ct + 1], ALU.mult, ALU.add)
            P.stt(y[:, :], v[:, 1:TOWN + 1], cw[:, ct, 1:2], y[:, :], ALU.mult, ALU.add)
            P.stt(y[:, :], v[:, 0:TOWN], cw[:, ct, 0:1], y[:, :], ALU.mult, ALU.add)
            for tg in range(4):
                proj_fm(P, C, pb[4 + tg % 2], wgb, ct * 128, 128, tg)
                P.tt("dve", ob[:, :], y[:, tg * 512:(tg + 1) * 512], pb[4 + tg % 2][:, :], ALU.mult)
                P.dma("sp", C.brT[1, cs, tg * 512:(tg + 1) * 512], ob[:, :])


def stage_mem(P, C):
    pb = C.pb
    with Stage(P):
        wmq = P.sb("wmq", [128, 8, 512], BF16)
        P.dma("pool", wmq[:, :, :], wview(C.w_in, C_MQ, 512))
        wkv = P.sb("wkv", [128, 8, 1024], BF16)
        P.dma("pool", wkv[:, :, :], V(C.w_mem, C.w_mem.h[:, :].rearrange("(kt k) n -> k kt n", k=128)))
        mT = P.sb("mT", [128, 8, 256], BF16)
        P.dma("pool", mT[:, :, :], V(C.memT, C.memT.h[:, :].rearrange("(kt k) m -> k kt m", k=128)))
        KT = P.sb("KT", [128, 4, 256], BF16)
        Vt = P.sb("Vt", [128, 2, 512], BF16)
        for h in range(4):
            for kt in range(8):
                P.mm(pb[0][:, 0:256], wkv[:, kt, h * 128:(h + 1) * 128], mT[:, kt, :], start=(kt == 0), stop=(kt == 7))
            P.cp("act", KT[:, h, :], pb[0][:, 0:256])
        for mt in range(2):
            for kt in range(8):
                P.mm(pb[1][:, :], mT[:, kt, mt * 128:(mt + 1) * 128], wkv[:, kt, 512:1024], start=(kt == 0), stop=(kt == 7))
            P.cp("act", Vt[:, mt, :], pb[1][:, :])
        mq = P.sb("mq", [128, 512], BF16); pT = P.sb("mpT", [128, 2, 512], BF16)
        rec = P.sb("mrec", [128, 512]); ob = P.sb("mob", [128, 512], BF16)
        for h in range(4):
            for tg in range(4):
                proj_fm(P, C, pb[2], wmq, h * 128, 128, tg)
                P.act(mq[:, :], pb[2][:, :], AF.Copy, scale=128.0 ** -0.5)
                for mt in range(2):
                    P.mm(pb[3 + mt][:, :], KT[:, h, mt * 128:(mt + 1) * 128], mq[:, :])
                    P.act(pT[:, mt, :], pb[3 + mt][:, :], AF.Exp)
                for mt in range(2):
                    P.mm(pb[5][:, :], Vt[:, mt, h * 128:(h + 1) * 128], pT[:, mt, :], start=(mt == 0), stop=(mt == 1))
                for mt in range(2):
                    P.mm(pb[6][:, :], C.ones_bf[:, :], pT[:, mt, :], start=(mt == 0), stop=(mt == 1))
                P.recip(rec[:, :], pb[6][:, :])
                P.tt("dve", ob[:, :], rec[:, :], pb[5][:, :], ALU.mult)
                P.dma("sp", C.brT[3, h * 128:(h + 1) * 128, tg * 512:(tg + 1) * 512], ob[:, :])


def stage_attproj(P, C):
    pb = C.pb
    with Stage(P):
        wq = P.sb("wq", [128, 8, 512], BF16); P.dma("pool", wq[:, :, :], wview(C.w_in, C_Q, 512))
        wqi = P.sb("wqi", [128, 8, 256], BF16); P.dma("pool", wqi[:, :, :], wview(C.w_in, C_QI, 256))
        wwi = P.sb("wwi", [128, 8, 8], BF16); P.dma("pool", wwi[:, :, :], wview(C.w_in, C_WI, 8))
        wuk = P.sb("wuk", [128, 512]); P.dma("sp", wuk[:, :], C.w_uk[:, :])
        wukT = P.sb("wukT", [128, 4, 128], BF16)
        for m in range(4):
            P.tr(pb[0][:, 0:128], wuk[:, m * 128:(m + 1) * 128], C.ident[:, :])
            P.cp("act", wukT[:, m, :], pb[0][:, 0:128])
        qT = P.sb("qT", [128, 4, TOWN], BF16)
        for m in range(4):
            for tg in range(4):
                proj_fm(P, C, pb[1 + tg % 2], wq, m * 128, 128, tg)
                P.cp("act", qT[:, m, tg * 512:(tg + 1) * 512], pb[1 + tg % 2][:, :])
        st = [P.sb(f"qst{i}", [128, 512], BF16) for i in range(2)]
        k = 0
        for h in range(8):
            m, hh = h // 2, h % 2
            rows = slice(64 * hh, 64 * hh + 64)
            for tg in range(4):
                ps = pb[3 + k % 2]; s = st[k % 2]; k += 1
                P.mm(ps[:, :], wukT[rows, m, :], qT[rows, m, tg * 512:(tg + 1) * 512])
                P.act(s[:, :], ps[:, :], AF.Copy, scale=0.125)
                P.dma("sp", C.qlat_d[:, h, tg * 512:(tg + 1) * 512], s[:, :])
        for h in range(8):
            for tg in range(4):
                ps = pb[5 + k % 2]; s = st[k % 2]; k += 1
                proj_fm(P, C, ps, wqi, h * 32, 32, tg)
                P.cp("act", s[0:32, :], ps[0:32, :])
                P.dma("sp", C.qidx_d[0:32, h, tg * 512:(tg + 1) * 512], s[0:32, :])
        for tt_ in range(16):
            for kt in range(8):
                P.mm(pb[7][:, 0:8], C.hTo[:, kt, tt_ * 128:(tt_ + 1) * 128], wwi[:, kt, :], start=(kt == 0), stop=(kt == 7))
            P.cp("act", C.widx[:, tt_, :], pb[7][:, 0:8])


def stage_att(P, C):
    pb = C.pb
    with Stage(P):
        acc = P.sb("acc", [128, WIN]); notsel = P.sb("notsel", [128, WIN], BF16); bj = P.sb("bj", [128, WIN], BF16)
        tmp = [P.sb(f"atmp{i}", [128, 512]) for i in range(2)]
        qi = P.sb("qi", [32, 8, 128], BF16); ql = P.sb("ql", [128, 8, 128], BF16)
        npad = P.sb("npad_s", [128, 1]); P.dma("sp", npad[:, :], C.npad[C.j, :, :])
        padc = P.sb("padc", [128, 64]); P.dma("sp", padc[:, :], C.padcol[C.j, :, :])
        tri = P.sb("tri_s", [128, 128]); P.dma("sp", tri[:, :], C.tri[:, :])
        negI = P.sb("negI", [128, 4, 128], BF16)
        for j in range(4):
            P.ts("dve", negI[:, j, :], C.ident[:, :], NEG, None, ALU.mult)
        wuv = P.sb("wuv", [128, 512]); P.dma("sp", wuv[:, :], C.w_uv[:, :])
        wuvp = P.sb("wuvp", [128, 8, 128], BF16)
        P.memset("pool", wuvp[:, :, :], 0.0)
        for h in range(8):
            P.cp("dve", wuvp[:, h, 64 * (h % 2):64 * (h % 2) + 64], wuv[:, h * 64:(h + 1) * 64])
        pTs = [P.sb(f"pT{i}", [128, 512], BF16) for i in range(3)]
        ol = P.sb("ol", [128, 8, 128]); olT = P.sb("olT", [128, 8, 128], BF16)
        ab = P.sb("ab", [128, 4, 128], BF16)
        S = Small(P, "att")
        lo = S.col(); hi = S.col(); mid = S.col(); cnt = S.col(); c2 = S.col(); ge = S.col(); d1 = S.col(); d2 = S.col()
        rec = S.col(8)
        kk = 0
        for qb in range(TOWN // 128):
            nk = PRE // 128 + qb + 1
            NK = nk * 128
            qs = slice(qb * 128, (qb + 1) * 128)
            P.dma("sp", qi[:, :, :], C.qidx_d[0:32, :, qs])
            P.dma("sp", ql[:, :, :], C.qlat_d[:, :, qs])
            pad = C.pad
            nsp = (NK - pad + 511) // 512
            for h in range(8):
                for s in range(nsp):
                    n = min(512, NK - pad - 512 * s)
                    ks = slice(pad + 512 * s, pad + 512 * s + n)
                    ps = pb[kk % 2]; t = tmp[kk % 2]; kk += 1
                    P.mm(ps[:, 0:n], qi[0:32, h, :], C.kidxT[0:32, ks])
                    P.act(t[:, 0:n], ps[:, 0:n], AF.Relu)
                    if h == 0:
                        P.ts("dve", acc[:, ks], t[:, 0:n], C.widx[:, qb, 0:1], None, ALU.mult)
                    else:
                        P.stt(acc[:, ks], t[:, 0:n], C.widx[:, qb, h:h + 1], acc[:, ks], ALU.mult, ALU.add)
            P.red(hi[:, :], acc[:, pad:NK], ALU.max)
            P.red(lo[:, :], acc[:, pad:NK], ALU.min)
            P.tt("dve", acc[:, NK - 128:NK], acc[:, NK - 128:NK], tri[:, :], ALU.add)
            P.tt("dve", d2[:, :], hi[:, :], lo[:, :], ALU.subtract)
            for it in range(NBIS):
                P.ts("dve", mid[:, :], d2[:, :], 0.5 ** (it + 1), lo[:, :], ALU.mult, ALU.add)
                P.ts("dve", bj[:, pad:NK], acc[:, pad:NK], mid[:, :], None, ALU.is_ge, op1=ALU.add, accum=cnt[:, :])
                P.ts("dve", ge[:, :], cnt[:, :], 256.0, None, ALU.is_ge)
                P.tt("dve", d1[:, :], mid[:, :], lo[:, :], ALU.subtract)
                P.stt(lo[:, :], d1[:, :], ge[:, :], lo[:, :], ALU.mult, ALU.add)
            P.ts("dve", notsel[:, pad:NK], acc[:, pad:NK], lo[:, :], None, ALU.is_lt)
            kc0 = pad // 128
            groups = [(kc, hg) for kc in range(kc0, nk) for hg in range(2)]

            def emit_qk(i):
                kc, hg = groups[i]
                cs = slice(kc * 128, (kc + 1) * 128)
                pl = pb[2 + i % 2]
                P.mm(pl[:, :], C.ckvT[:, cs], ql[:, 4 * hg:4 * hg + 4, :], start=True, stop=False)
                P.mm(pl[:, :], notsel[:, cs], negI[:, :, :], start=False, stop=True)

            def emit_exp_pv(i):
                kc, hg = groups[i]
                pl = pb[2 + i % 2]; pT = pTs[i % 3]
                P.act(pT[:, :], pl[:, :], AF.Exp)
                for h4 in range(4):
                    h = 4 * hg + h4
                    po = pb[4 + h // 3]
                    P.mm(po[:, (h % 3) * 129:(h % 3) * 129 + 129], pT[:, h4 * 128:(h4 + 1) * 128], C.ckv_tok[:, kc, :],
                         start=(kc == kc0), stop=(kc == nk - 1))

            emit_qk(0)
            for i in range(len(groups)):
                if i + 1 < len(groups):
                    emit_qk(i + 1)
                emit_exp_pv(i)
            for h in range(8):
                po = pb[4 + h // 3]; o = (h % 3) * 129
                P.recip(rec[:, h:h + 1], po[:, o + 128:o + 129])
                P.ts("dve", ol[:, h, :], po[:, o:o + 128], rec[:, h:h + 1], None, ALU.mult)
            for h in range(8):
                P.tr(pb[7][:, (h % 4) * 128:(h % 4) * 128 + 128], ol[:, h, :], C.ident[:, :])
                P.cp("act", olT[:, h, :], pb[7][:, (h % 4) * 128:(h % 4) * 128 + 128])
            for m in range(4):
                ps = pb[kk % 2]; kk += 1
                P.mm(ps[:, 0:128], wuvp[:, 2 * m, :], olT[:, 2 * m, :], start=True, stop=False)
                P.mm(ps[:, 0:128], wuvp[:, 2 * m + 1, :], olT[:, 2 * m + 1, :], start=False, stop=True)
                P.cp("act", ab[:, m, :], ps[:, 0:128])
            P.dma("sp", V(C.brT, C.brT.h[0, :, qs].rearrange("(m p) q -> p m q", p=128)), ab[:, :, :])


def stage_merge_a(P, C):
    pb = C.pb
    with Stage(P):
        macc = P.sb("macc", [128, 8, TOWN])
        brt = P.sb("brt", [128, 4, TOWN], BF16)
        wbr = P.sb("wbr", [128, 4, D], BF16)
        wg = P.sb("wg", [128, 8, D], BF16)
        sg = [P.sb(f"sg{i}", [128, 512]) for i in range(2)]
        tm = [P.sb(f"mtm{i}", [128, 512]) for i in range(2)]
        k = 0
        for r in range(4):
            P.dma("sp", brt[:, :, :], V(C.brT, C.brT.h[r, :, :].rearrange("(kt k) t -> k kt t", k=128)))
            P.dma("pool", wbr[:, :, :], V(C.w_br, C.w_br.h[r, :, :].rearrange("(kt k) n -> k kt n", k=128)))
            P.dma("pool", wg[:, :, :], wview(C.w_in, C_GATE + r * D, D))
            for dt_ in range(8):
                ds_ = slice(dt_ * 128, (dt_ + 1) * 128)
                for tg in range(4):
                    ts_ = slice(tg * 512, (tg + 1) * 512)
                    pg = pb[k % 2]; pr = pb[2 + k % 2]; s = sg[k % 2]; t = tm[k % 2]; k += 1
                    for kt in range(8):
                        P.mm(pg[:, :], wg[:, kt, ds_], C.hTo[:, kt, ts_], start=(kt == 0), stop=(kt == 7))
                    for kt in range(4):
                        P.mm(pr[:, :], wbr[:, kt, ds_], brt[:, kt, ts_], start=(kt == 0), stop=(kt == 3))
                    P.act(s[:, :], pg[:, :], AF.Sigmoid)
                    if r == 0:
                        P.tt("dve", macc[:, dt_, ts_], s[:, :], pr[:, :], ALU.mult)
                    else:
                        P.tt("dve", t[:, :], s[:, :], pr[:, :], ALU.mult)
                        P.tt("pool", macc[:, dt_, ts_], macc[:, dt_, ts_], t[:, :], ALU.add)
        for dt_ in range(8):
            P.cp("act" if dt_ % 2 else "dve", brt[:, dt_ % 4, :], macc[:, dt_, :])
            P.dma("sp", C.mT_d[dt_ * 128:(dt_ + 1) * 128, :], brt[:, dt_ % 4, :])


def stage_merge_b(P, C):
    pb = C.pb
    with Stage(P):
        mT = P.sb("mTb", [128, 8, TOWN], BF16)
        P.dma("sp", mT[:, :, :], V(C.mT_d, C.mT_d.h[:, :].rearrange("(kt k) t -> k kt t", k=128)))
        wo = P.sb("wo", [128, 8, D], BF16)
        P.dma("pool", wo[:, :, :], V(C.w_o, C.w_o.h[:, :].rearrange("(kt k) n -> k kt n", k=128)))
        g1 = P.sb("g1", [128, D]); b1 = P.sb("b1", [128, D])
        P.dma("sp", g1[:, :], C.ln1g[:, :]); P.dma("sp", b1[:, :], C.ln1b[:, :])
        wr = P.sb("wr32", [128, 8, 32])
        P.dma("sp", wr[:, :, :], V(C.w_r, C.w_r.h[:, :].rearrange("(kt k) n -> k kt n", k=128)))
        wrh = P.sb("wrh", [128, 8, 32], BF16); wrl = P.sb("wrl", [128, 8, 32], BF16)
        P.cp("dve", wrh[:, :, :], wr[:, :, :])
        P.tt("dve", wrl[:, :, :], wr[:, :, :], wrh[:, :, :], ALU.subtract)
        brr = P.sb("brr", [128, 32]); P.dma("sp", brr[:, :], C.b_r[:, :])
        ho = [P.sb(f"ho{i}", [128, D]) for i in range(2)]
        xx = [P.sb(f"xx{i}", [128, D]) for i in range(2)]
        h1 = [P.sb(f"h1_{i}", [128, D]) for i in range(2)]
        h32 = [P.sb(f"h32_{i}", [128, 128], BF16) for i in range(3)]
        junk = P.sb("mjunk", [128, D])
        S = Small(P, "ln1")
        k = 0
        for tt_ in range(16):
            tsl = slice(tt_ * 128, (tt_ + 1) * 128)
            hot = ho[tt_ % 2]; x = xx[tt_ % 2]; h = h1[tt_ % 2]
            P.dma("sp", hot[:, :], C.hown[C.off + tt_ * 128:C.off + (tt_ + 1) * 128, :])
            for dh in range(2):
                ps = pb[dh]
                for kt in range(8):
                    P.mm(ps[:, :], mT[:, kt, tsl], wo[:, kt, dh * 512:(dh + 1) * 512], start=(kt == 0), stop=(kt == 7))
                P.stt(x[:, dh * 512:(dh + 1) * 512], hot[:, dh * 512:(dh + 1) * 512], ALPHA, ps[:, :], ALU.mult, ALU.add)
            ln_tok(P, x[:, :], h[:, :], g1[:, :], b1[:, :], S, junk[:, :])
            P.act(C.yacc[tt_][:, :], h[:, :], AF.Copy, scale=ALPHA)
            for kt in range(8):
                pt = pb[2 + k % 4]; hh = h32[k % 3]; k += 1
                P.tr(pt[:, 0:128], h[:, kt * 128:(kt + 1) * 128], C.ident[:, :])
                P.cp("act", C.h1T[:, kt, tsl], pt[:, 0:128])
                P.tt("dve", hh[:, :], pt[:, 0:128], C.h1T[:, kt, tsl], ALU.subtract)
                P.mm(pb[6][:, 0:32], C.h1T[:, kt, tsl], wrh[:, kt, :], start=(kt == 0), stop=False)
                P.mm(pb[6][:, 0:32], C.h1T[:, kt, tsl], wrl[:, kt, :], start=False, stop=False)
                P.mm(pb[6][:, 0:32], hh[:, :], wrh[:, kt, :], start=False, stop=(kt == 7))
            P.tt("dve", C.rlog[:, tt_, :], pb[6][:, 0:32], brr[:, :], ALU.add)


def emit_hT(P, C, o, dst, t0, k):
    if not hasattr(C, "tst") or C.tst_owner is not P.es:
        C.tst = [P.sb(f"tst{i}", [128, 8, 128]) for i in range(2)]
        C.tst_owner = P.es
    st = C.tst[k % 2]
    for kt in range(8):
        ps = C.pb[(kt // 4) + 2 * (k % 2)]
        P.tr(ps[:, (kt % 4) * 128:(kt % 4) * 128 + 128], o[:, kt * 128:(kt + 1) * 128], C.ident[:, :])
    for half in range(2):
        ps = C.pb[half + 2 * (k % 2)]
        P.cp("act" if half else "dve", st[:, 4 * half:4 * half + 4, :], ps[:, :])
    P.dma("sp", V(dst, dst.h[:, PRE + t0:PRE + t0 + 128].rearrange("(kt k) t -> k kt t", k=128)), st[:, :, :])


def stage_moe(P, C):
    pb = C.pb
    with Stage(P):
        S = Small(P, "moe")
        gates = P.sb("gates", [128, 16, 32])
        gT = P.sb("gT", [32, TOWN], BF16)
        bdn = P.sb("bdn", [32, D], BF16); P.dma("pool", bdn[:, :], C.b_dn[:, :])
        bup = P.sb("bup", [128, 32, 8, 2]); P.dma("sp", bup[:, :, :, :], C.b_up[:, :, :, :])
        top8 = P.sb("top8", [128, 8]); nmx = S.col(); ssum = S.col(); ee = P.sb("ree", [128, 32]); mk = P.sb("rmk", [128, 32])
        for tt_ in range(16):
            lg = C.rlog[:, tt_, :]
            P.max8(top8[:, :], lg)
            P.ts("dve", nmx[:, :], top8[:, 0:1], -1.0, None, ALU.mult)
            P.act(ee[:, :], lg, AF.Exp, bias=nmx[:, :])
            P.ts("dve", mk[:, :], lg, top8[:, 3:4], None, ALU.is_ge)
            P.tt("dve", ee[:, :], ee[:, :], mk[:, :], ALU.mult)
            P.red(ssum[:, :], ee[:, :], ALU.add)
            P.recip(ssum[:, :], ssum[:, :])
            P.ts("dve", gates[:, tt_, :], ee[:, :], ssum[:, :], None, ALU.mult)
            P.tr(pb[7][0:32, 0:128], gates[:, tt_, :], C.ident[:, :])
            P.cp("act", gT[0:32, tt_ * 128:(tt_ + 1) * 128], pb[7][0:32, 0:128])
        for tt_ in range(16):
            for dh in range(2):
                ps = pb[dh]
                P.mm(ps[:, :], gT[0:32, tt_ * 128:(tt_ + 1) * 128], bdn[0:32, dh * 512:(dh + 1) * 512])
                P.tt("dve", C.yacc[tt_][:, dh * 512:(dh + 1) * 512], C.yacc[tt_][:, dh * 512:(dh + 1) * 512], ps[:, :], ALU.add)
        with Stage(P):
            wup = [P.sb(f"wup{i}", [128, 8, 1024], BF16) for i in range(2)]
            wdn = [P.sb(f"wdn{i}", [128, 4, D], BF16) for i in range(2)]
            actT = P.sb("actT", [128, 4, TOWN], BF16)
            gg = [P.sb(f"gg{i}", [128, 512]) for i in range(2)]
            ll = [P.sb(f"ll{i}", [128, 512]) for i in range(2)]
            sgm = [P.sb(f"sgm{i}", [128, 512]) for i in range(2)]
            k = 0; kd = 0
            for e in range(32):
                for hf in range(2):
                    u = (2 * e + hf) % 2
                    wu = wup[u]; wd = wdn[u]
                    P.dma("pool", wu[:, :, :], V(C.w_up, C.w_up.h[e * D:(e + 1) * D, hf * 1024:(hf + 1) * 1024].rearrange("(kt k) n -> k kt n", k=128)))
                    P.dma("pool", wd[:, :, :], V(C.w_dn, C.w_dn.h[e * D + hf * 512:e * D + (hf + 1) * 512, :].rearrange("(ft f) n -> f ft n", f=128)))
                    for f4 in range(4):
                        ft = hf * 4 + f4
                        for tg in range(4):
                            ts_ = slice(tg * 512, (tg + 1) * 512)
                            pg = pb[(2 * k) % 6]; pl = pb[(2 * k) % 6 + 1]
                            g = gg[k % 2]; l = ll[k % 2]; s = sgm[k % 2]; k += 1
                            for kt in range(8):
                                P.mm(pg[:, :], wu[:, kt, f4 * 256:f4 * 256 + 256:2], C.h1T[:, kt, ts_], start=(kt == 0), stop=(kt == 7))
                            for kt in range(8):
                                P.mm(pl[:, :], wu[:, kt, f4 * 256 + 1:f4 * 256 + 256:2], C.h1T[:, kt, ts_], start=(kt == 0), stop=(kt == 7))
                            P.ts("dve", g[:, :], pg[:, :], bup[:, e, ft, 0:1], 7.0, ALU.add, ALU.min)
                            P.act(s[:, :], g[:, :], AF.Sigmoid, scale=1.702)
                            P.ts("dve", l[:, :], pl[:, :], bup[:, e, ft, 1:2], 7.0, ALU.add, ALU.min)
                            P.ts("dve", l[:, :], l[:, :], -7.0, 1.0, ALU.max, ALU.add)
                            P.tt("pool", g[:, :], g[:, :], s[:, :], ALU.mult)
                            P.tt("dve", actT[:, f4, ts_], g[:, :], l[:, :], ALU.mult)
                    for tt_ in range(16):
                        tsl = slice(tt_ * 128, (tt_ + 1) * 128)
                        for dh in range(2):
                            pd = pb[6 + kd % 2]; kd += 1
                            for f4 in range(4):
                                P.mm(pd[:, :], actT[:, f4, tsl], wd[:, f4, dh * 512:(dh + 1) * 512], start=(f4 == 0), stop=(f4 == 3))
                            P.stt(C.yacc[tt_][:, dh * 512:(dh + 1) * 512], pd[:, :], gates[:, tt_, e:e + 1],
                                  C.yacc[tt_][:, dh * 512:(dh + 1) * 512], ALU.mult, ALU.add)
        g2 = P.sb("g2", [128, D]); b2 = P.sb("b2", [128, D])
        P.dma("sp", g2[:, :], C.ln2g[:, :]); P.dma("sp", b2[:, :], C.ln2b[:, :])
        junk = P.sb("ojunk", [128, D])
        oo = [P.sb(f"oo{i}", [128, D]) for i in range(2)]
        for tt_ in range(16):
            o = oo[tt_ % 2]
            ln_tok(P, C.yacc[tt_][:, :], o[:, :], g2[:, :], b2[:, :], S, junk[:, :])
            P.dma("sp", C.out[C.off + tt_ * 128:C.off + (tt_ + 1) * 128, :], o[:, :])
            if getattr(C, "hT_next", None) is not None:
                emit_hT(P, C, o, C.hT_next, C.off + tt_ * 128, tt_)


STAGES_ALL = ("window", "ssm", "conv", "mem", "attproj", "att", "merge", "moe")
NQ = 4


def build_layer(stages=STAGES_ALL, dbg=(), quarters=(0, 1, 2, 3)):
    nc = bass.Bass("TRN2", target_bir_lowering=False)
    es = ExitStack()
    P = Prog(nc, es)
    C = Ctx()

    def inp(name, shape, dt=F32):
        t = P.dram(name, shape, dt, kind="ExternalInput")
        setattr(C, name, t)
        return t

    inp("hT", [D, PRE + T]); inp("hown", [T, D]); inp("npad", [4, 128, 1]); inp("padcol", [4, 128, 64])
    inp("memT", [D, 256]); inp("identin", [128, 128]); inp("tri", [128, 128])
    inp("w_in", [D, D_IN]); inp("kvg", [128, 1]); inp("kvgrow", [128, 128])
    inp("w_uk", [128, 512]); inp("w_uv", [128, 512]); inp("convw", [128, 4, 3]); inp("convb", [128, 4])
    inp("lamre", [128, 16]); inp("lamim", [128, 16]); inp("logdt", [128, 16])
    inp("bre", [128, 16, 16]); inp("bim", [128, 16, 16]); inp("cre", [128, 16, 16]); inp("cim", [128, 16, 16])
    inp("dskip", [128, 4]); inp("w_glu", [512, 512]); inp("bglu", [128, 4])
    inp("w_mem", [D, D]); inp("w_br", [4, 512, D]); inp("w_o", [D, D])
    inp("ln1g", [128, D]); inp("ln1b", [128, D]); inp("ln2g", [128, D]); inp("ln2b", [128, D])
    inp("w_r", [D, 32]); inp("b_r", [128, 32])
    inp("w_up", [32 * D, 2048]); inp("b_up", [128, 32, 8, 2]); inp("w_dn", [32 * D, D]); inp("b_dn", [32, D])
    C.out = P.dram("out", [T, D], F32, kind="ExternalOutput")

    def scratch(name, shape, dt):
        kind = "ExternalOutput" if name in dbg else "Internal"
        t = P.dram(name, shape, dt, kind=kind)
        setattr(C, name, t)
        return t

    scratch("uT", [512, WIN], F32); scratch("z32", [512, TOWN], F32); scratch("brT", [4, 512, TOWN], BF16)
    scratch("mT_d", [D, TOWN], BF16); scratch("qlat_d", [128, 8, TOWN], BF16); scratch("qidx_d", [32, 8, TOWN], BF16)

    C.pb = [P.ps(f"pb{i}", [128, 512], F32) for i in range(8)]
    C.ident = P.sb("ident", [128, 128]); P.dma("sp", C.ident[:, :], C.identin[:, :])
    C.ones_bf = P.sb("ones_bf", [128, 128], BF16); P.memset("dve", C.ones_bf[:, :], 1.0)

    for j in quarters:
        C.j = j
        C.off = j * TOWN
        C.pad = PRE - j * TOWN
        C.first_q = True
        with Stage(P):
            C.ckvT = P.sb(f"ckvT{j}", [128, WIN], BF16)
            C.ckv_tok = P.sb(f"ckv_tok{j}", [128, 64, 129], BF16)
            C.kidxT = P.sb(f"kidxT{j}", [32, WIN], BF16)
            C.widx = P.sb(f"widx{j}", [128, 16, 8])
            P.memset("dve", C.ckv_tok[:, :, 128:129], 1.0)
            if "window" in stages:
                stage_window(P, C)
            if "ssm" in stages:
                stage_ssm(P, C)
            with Stage(P):
                C.hTo = P.sb(f"hTo{j}", [128, 8, TOWN], BF16)
                P.dma("pool", C.hTo[:, :, :], V(C.hT, C.hT.h[:, C.off + PRE:C.off + WIN].rearrange("(kt k) t -> k kt t", k=128)))
                if "conv" in stages:
                    stage_conv(P, C)
                if "mem" in stages:
                    stage_mem(P, C)
                if "attproj" in stages:
                    stage_attproj(P, C)
            if "att" in stages:
                stage_att(P, C)
        with Stage(P):
            C.hTo = P.sb(f"hTo2{j}", [128, 8, TOWN], BF16)
            P.dma("pool", C.hTo[:, :, :], V(C.hT, C.hT.h[:, C.off + PRE:C.off + WIN].rearrange("(kt k) t -> k kt t", k=128)))
            if "merge" in stages or "merge_a" in stages:
                stage_merge_a(P, C)
        with Stage(P):
            C.yacc = [P.sb(f"yacc{j}_{i}", [128, D]) for i in range(16)]
            C.h1T = P.sb(f"h1T{j}", [128, 8, TOWN], BF16)
            C.rlog = P.sb(f"rlog{j}", [128, 16, 32])
            if "merge" in stages or "merge_b" in stages:
                stage_merge_b(P, C)
            if "moe" in stages:
                stage_moe(P, C)
    P.finish("sp", [C.out] + [getattr(C, n) for n in dbg])
    print("layer program instructions:", P.ninst, flush=True)
    es.close()
    return nc


def _rep(v, rows=128):
    return np.ascontiguousarray(np.broadcast_to(np.asarray(v, np.float32).reshape(1, -1), (rows, np.size(v))))


def _sm(a):
    a = np.asarray(a, np.float32)
    rest = a.shape[2:]
    return np.ascontiguousarray(a.reshape((16, 128) + rest).swapaxes(0, 1))


def layer_weights(inp, l):
    f = lambda k: np.asarray(inp[k][l], np.float32)
    w = {}
    w["w_in"] = np.ascontiguousarray(f("w_in"))
    w["kvg"] = np.ascontiguousarray(f("kv_norm_g").reshape(128, 1))
    w["kvgrow"] = _rep(f("kv_norm_g"))
    w["w_uk"] = np.ascontiguousarray(f("w_uk").reshape(128, 512))
    w["w_uv"] = np.ascontiguousarray(f("w_uv").reshape(128, 512))
    w["convw"] = np.ascontiguousarray(f("conv_w").T.reshape(4, 128, 3).transpose(1, 0, 2))
    w["convb"] = np.ascontiguousarray(f("conv_b").reshape(4, 128).T)
    w["lamre"] = _sm(f("lam_re")); w["lamim"] = _sm(f("lam_im"))
    w["logdt"] = _sm(np.broadcast_to(f("log_dt")[:, None], (32, 64)))
    w["bre"] = _sm(f("b_re")); w["bim"] = _sm(f("b_im"))
    w["cre"] = _sm(f("c_re").transpose(0, 2, 1)); w["cim"] = _sm(f("c_im").transpose(0, 2, 1))
    w["dskip"] = np.ascontiguousarray(f("d_skip").reshape(4, 128).T)
    w["w_glu"] = np.ascontiguousarray(f("w_glu"))
    w["bglu"] = np.ascontiguousarray(f("b_glu").reshape(4, 128).T)
    w["w_mem"] = np.ascontiguousarray(f("w_mem_kv"))
    w["w_br"] = np.ascontiguousarray(f("w_branch"))
    w["w_o"] = np.ascontiguousarray(f("w_o"))
    w["ln1g"] = _rep(f("ln1_g")); w["ln1b"] = _rep(f("ln1_b"))
    w["ln2g"] = _rep(f("ln2_g")); w["ln2b"] = _rep(f("ln2_b"))
    w["w_r"] = np.ascontiguousarray(f("w_router"))
    w["b_r"] = _rep(f("b_router"))
    w["w_up"] = np.ascontiguousarray(f("w_up").reshape(32 * D, 2048))
    w["b_up"] = np.ascontiguousarray(f("b_up").reshape(32, 8, 128, 2).transpose(2, 0, 1, 3))
    w["w_dn"] = np.ascontiguousarray(f("w_down").reshape(32 * D, D))
    w["b_dn"] = np.ascontiguousarray(f("b_down"))
    return w


def batch_inputs(h, mem, b):
    d = {}
    win = np.zeros((PRE + T, D), np.float32)
    win[PRE:] = h[b]
    d["hT"] = np.ascontiguousarray(win.T)
    d["hown"] = np.ascontiguousarray(h[b])
    npad = np.zeros((4, 128, 1), np.float32); padcol = np.zeros((4, 128, 64), np.float32)
    kidx = (np.arange(64)[None, :] * 128 + np.arange(128)[:, None])
    for j in range(4):
        pad = PRE - j * TOWN
        npad[j] = float(pad)
        padcol[j] = np.where(kidx < pad, NEG, 0.0)
    d["npad"] = npad; d["padcol"] = padcol
    d["memT"] = np.ascontiguousarray(np.asarray(mem[b], np.float32).T)
    d["identin"] = np.eye(128, dtype=np.float32)
    d["tri"] = np.where(np.arange(128)[None, :] > np.arange(128)[:, None], -BIG, 0.0).astype(np.float32)
    return d


def core_inputs(h, mem, c):
    b, j = c // 4, c % 4
    t0 = j * TOWN
    pad = PRE - t0
    win = np.zeros((WIN, D), np.float32)
    win[pad:] = h[b, 0:t0 + TOWN]
    d = {}
    d["hT"] = np.ascontiguousarray(win.T)
    d["hown"] = np.ascontiguousarray(h[b, t0:t0 + TOWN])
    d["npad"] = np.full((128, 1), float(pad), np.float32)
    kidx = (np.arange(64)[None, :] * 128 + np.arange(128)[:, None])
    d["padcol"] = np.where(kidx < pad, NEG, 0.0).astype(np.float32)
    d["memT"] = np.ascontiguousarray(np.asarray(mem[b], np.float32).T)
    d["identin"] = np.eye(128, dtype=np.float32)
    d["tri"] = np.where(np.arange(128)[None, :] > np.arange(128)[:, None], -BIG, 0.0).astype(np.float32)
    return d


_NC_CACHE = {}

W_NAMES = ("w_in", "kvg", "kvgrow", "w_uk", "w_uv", "convw", "convb", "lamre", "lamim", "logdt", "bre", "bim",
           "cre", "cim", "dskip", "w_glu", "bglu", "w_mem", "w_br", "w_o", "ln1g", "ln1b", "ln2g", "ln2b",
           "w_r", "b_r", "w_up", "b_up", "w_dn", "b_dn")
W_SHAPES = {"w_in": [D, D_IN], "kvg": [128, 1], "kvgrow": [128, 128], "w_uk": [128, 512], "w_uv": [128, 512],
            "convw": [128, 4, 3], "convb": [128, 4], "lamre": [128, 16], "lamim": [128, 16], "logdt": [128, 16],
            "bre": [128, 16, 16], "bim": [128, 16, 16], "cre": [128, 16, 16], "cim": [128, 16, 16],
            "dskip": [128, 4], "w_glu": [512, 512], "bglu": [128, 4], "w_mem": [D, D], "w_br": [4, 512, D],
            "w_o": [D, D], "ln1g": [128, D], "ln1b": [128, D], "ln2g": [128, D], "ln2b": [128, D],
            "w_r": [D, 32], "b_r": [128, 32], "w_up": [32 * D, 2048], "b_up": [128, 32, 8, 2],
            "w_dn": [32 * D, D], "b_dn": [32, D]}


def build_fused(depth=DEPTH, quarters=(0, 1, 2, 3)):
    nc = bass.Bass("TRN2", target_bir_lowering=False)
    es = ExitStack()
    P = Prog(nc, es)
    C = Ctx()
    x = P.dram("x", [T, D], F32, kind="ExternalInput")
    lng = P.dram("lng", [128, D], F32, kind="ExternalInput"); lnb = P.dram("lnb", [128, D], F32, kind="ExternalInput")
    C.npad = P.dram("npad", [4, 128, 1], F32, kind="ExternalInput")
    C.padcol = P.dram("padcol", [4, 128, 64], F32, kind="ExternalInput")
    C.memT = P.dram("memT", [D, 256], F32, kind="ExternalInput")
    C.identin = P.dram("identin", [128, 128], F32, kind="ExternalInput")
    C.tri = P.dram("tri", [128, 128], F32, kind="ExternalInput")
    WL = [{n: P.dram(f"{n}_{l}", W_SHAPES[n], F32, kind="ExternalInput") for n in W_NAMES} for l in range(depth)]
    out = P.dram("out", [T, D], F32, kind="ExternalOutput")
    hTb = [P.dram(f"hTbuf{i}", [D, PRE + T], F32) for i in range(2)]
    hb = [P.dram(f"hbuf{i}", [T, D], F32) for i in range(2)]
    for n, shp, dt in (("uT", [512, WIN], F32), ("z32", [512, TOWN], F32), ("brT", [4, 512, TOWN], BF16),
                       ("mT_d", [D, TOWN], BF16), ("qlat_d", [128, 8, TOWN], BF16), ("qidx_d", [32, 8, TOWN], BF16)):
        setattr(C, n, P.dram(n, shp, dt))
    C.wc_up = P.dram("wc_up", [64, 128, 8, 1024], BF16)
    C.wc_dn = P.dram("wc_dn", [64, 128, 4, D], BF16)
    C.pb = [P.ps(f"pb{i}", [128, 512], F32) for i in range(8)]
    C.ident = P.sb("ident", [128, 128]); P.dma("sp", C.ident[:, :], C.identin[:, :])
    C.ones_bf = P.sb("ones_bf", [128, 128], BF16); P.memset("dve", C.ones_bf[:, :], 1.0)
    with Stage(P):
        zt = P.sb("zt", [128, 2048]); P.memset("dve", zt[:, :], 0.0)
        for i in range(2):
            for r in range(8):
                for c in range(PRE // 2048):
                    P.dma("sp", hTb[i][r * 128:(r + 1) * 128, c * 2048:(c + 1) * 2048], zt[:, :])
        gs = P.sb("gs", [128, D]); bs = P.sb("bs", [128, D])
        P.dma("sp", gs[:, :], lng[:, :]); P.dma("sp", bs[:, :], lnb[:, :])
        S = Small(P, "lnin")
        junk = P.sb("junk", [128, D])
        xs = [P.sb(f"x{i}", [128, D]) for i in range(2)]
        os_ = [P.sb(f"o{i}", [128, D]) for i in range(2)]
        for i in range(T // 128):
            xt = xs[i % 2]; ot = os_[i % 2]
            P.dma("sp", xt[:, :], x[i * 128:(i + 1) * 128, :])
            ln_tok(P, xt[:, :], ot[:, :], gs[:, :], bs[:, :], S, junk[:, :])
            P.dma("sp", hb[0][i * 128:(i + 1) * 128, :], ot[:, :])
            emit_hT(P, C, ot, hTb[0], i * 128, i)
    for l in range(depth):
        for n in W_NAMES:
            setattr(C, n, WL[l][n])
        C.hT = hTb[l % 2]; C.hown = hb[l % 2]
        last = (l == depth - 1)
        C.out = out if last else hb[(l + 1) % 2]
        C.hT_next = None if last else hTb[(l + 1) % 2]
        for j in quarters:
            C.j = j
            C.off = j * TOWN
            C.pad = PRE - j * TOWN
            C.first_q = (j == quarters[0])
            with Stage(P):
                C.ckvT = P.sb("ckvT", [128, WIN], BF16)
                C.ckv_tok = P.sb("ckv_tok", [128, 64, 129], BF16)
                C.kidxT = P.sb("kidxT", [32, WIN], BF16)
                C.widx = P.sb("widx", [128, 16, 8])
                P.memset("dve", C.ckv_tok[:, :, 128:129], 1.0)
                stage_window(P, C)
                stage_ssm(P, C)
                with Stage(P):
                    C.hTo = P.sb("hTo", [128, 8, TOWN], BF16)
                    P.dma("pool", C.hTo[:, :, :], V(C.hT, C.hT.h[:, C.off + PRE:C.off + WIN].rearrange("(kt k) t -> k kt t", k=128)))
                    stage_conv(P, C)
                    stage_mem(P, C)
                    stage_attproj(P, C)
                stage_att(P, C)
            with Stage(P):
                C.hTo = P.sb("hTo2", [128, 8, TOWN], BF16)
                P.dma("pool", C.hTo[:, :, :], V(C.hT, C.hT.h[:, C.off + PRE:C.off + WIN].rearrange("(kt k) t -> k kt t", k=128)))
                stage_merge_a(P, C)
            with Stage(P):
                C.yacc = [P.sb(f"yacc{i}", [128, D]) for i in range(16)]
                C.h1T = P.sb("h1T", [128, 8, TOWN], BF16)
                C.rlog = P.sb("rlog", [128, 16, 32])
                stage_merge_b(P, C)
                stage_moe(P, C)
    P.finish("sp", [out])
    print("fused program instructions:", P.ninst, flush=True)
    es.close()
    return nc


def fused_inputs(inputs, b):
    x = np.asarray(inputs["x"], np.float32)
    d = {"x": np.ascontiguousarray(x[b]), "lng": _rep(inputs["ln_in_g"]), "lnb": _rep(inputs["ln_in_b"])}
    npad = np.zeros((4, 128, 1), np.float32); padcol = np.zeros((4, 128, 64), np.float32)
    kidx = (np.arange(64)[None, :] * 128 + np.arange(128)[:, None])
    for j in range(4):
        pad = PRE - j * TOWN
        npad[j] = float(pad)
        padcol[j] = np.where(kidx < pad, NEG, 0.0)
    d["npad"] = npad; d["padcol"] = padcol
    d["memT"] = np.ascontiguousarray(np.asarray(inputs["mem"], np.float32)[b].T)
    d["identin"] = np.eye(128, dtype=np.float32)
    d["tri"] = np.where(np.arange(128)[None, :] > np.arange(128)[:, None], -BIG, 0.0).astype(np.float32)
    return d


def kernel(**inputs):
    if "fused" not in _NC_CACHE:
        _NC_CACHE["fused"] = build_fused()
    maps = [fused_inputs(inputs, b) for b in range(2)]
    for l in range(DEPTH):
        w = layer_weights(inputs, l)
        for n in W_NAMES:
            for m in maps:
                m[f"{n}_{l}"] = w[n]
    res = run_bass_kernel_spmd(_NC_CACHE["fused"], maps, core_ids=[0, 1])
    return np.stack([np.asarray(r["out"]) for r in res.results]).astype(np.float32)
```

```python
from contextlib import ExitStack
import math
import numpy as np
import concourse.bass as bass
import concourse.mybir as mybir
from concourse.bass_utils import run_bass_kernel_spmd

F32 = mybir.dt.float32
BF16 = mybir.dt.bfloat16
ALU = mybir.AluOpType
AF = mybir.ActivationFunctionType
AX = mybir.AxisListType

NCORES = 8
D = 1024
T = 8192
TOWN = 2048
WIN = 8192
PRE = WIN - TOWN
DEPTH = 4
D_IN = 7592
C_Q, C_CKV, C_QI, C_KI, C_WI, C_CU, C_GB, C_GC, C_SU, C_MQ, C_GATE = (
    0, 512, 640, 896, 928, 936, 1448, 1960, 2472, 2984, 3496)
LN_EPS = 1e-5
ALPHA = (2 * DEPTH) ** 0.25
NEG = -30000.0
BIG = 1.0e30
TWO_PI = 2.0 * math.pi
MAGIC = 12582912.0
NBIS = 17


class V:
    def __init__(self, t, ap):
        self.t = t
        self.ap = ap


class TT:
    def __init__(self, h, name):
        self.h = h
        self.name = name
        self.w = None
        self.r = []

    def __getitem__(self, idx):
        return V(self, self.h[idx])


def _ap(x):
    return x.ap if isinstance(x, V) else x


def _ts(*xs):
    return [x.t for x in xs if isinstance(x, V)]


class Prog:
    def __init__(self, nc, es):
        self.nc = nc
        self.es = es
        self.eng = {"pe": nc.tensor, "act": nc.scalar, "dve": nc.vector,
                    "pool": nc.gpsimd, "sp": nc.sync}
        self.sem = {}
        self.cnt = {}
        self.waited = {e: {} for e in self.eng}
        for e in self.eng:
            self.sem[e] = es.enter_context(nc.semaphore("s_" + e))
            self.cnt[e] = 0
        self.ninst = 0
        self.uid = 0

    def sb(self, name, shape, dt=F32):
        self.uid += 1
        return TT(self.es.enter_context(self.nc.sbuf_tensor(f"{name}_u{self.uid}", shape, dt)), name)

    def ps(self, name, shape, dt=F32):
        return TT(self.es.enter_context(self.nc.psum_tensor(name, shape, dt)), name)

    def dram(self, name, shape, dt=F32, kind="Internal"):
        return TT(self.nc.dram_tensor(name, shape, dt, kind=kind), name)

    def dsem(self, name):
        key = "d_" + name
        if key not in self.sem:
            self.sem[key] = self.es.enter_context(self.nc.semaphore(key))
            self.cnt[key] = 0
        return key

    def _wait(self, e, deps):
        need = {}
        for d in deps:
            if d is None:
                continue
            k, v = d
            if k == e and e == "pe":
                continue
            if v > need.get(k, 0):
                need[k] = v
        for k, v in need.items():
            if self.waited[e].get(k, 0) >= v:
                continue
            self.eng[e].wait_ge(self.sem[k], v)
            self.waited[e][k] = v

    def _deps(self, reads, writes):
        deps = []
        for t in reads:
            deps.append(t.w)
        for t in writes:
            deps.append(t.w)
            deps.extend(t.r)
        return deps

    def op(self, e, fn, reads=(), writes=()):
        self._wait(e, self._deps(reads, writes))
        ins = fn(self.eng[e])
        self.cnt[e] += 1
        ins.then_inc(self.sem[e], 1)
        d = (e, self.cnt[e])
        for t in reads:
            t.r.append(d)
        for t in writes:
            t.w = d
            t.r = []
        self.ninst += 1
        return d

    def dma(self, q, out, in_, sem=None, **kw):
        self._wait(q, self._deps([in_.t], [out.t]))
        key = self.dsem(sem or out.t.name)
        ins = self.eng[q].dma_start(out=out.ap, in_=in_.ap, **kw)
        self.cnt[key] += 16
        ins.then_inc(self.sem[key], 16)
        d = (key, self.cnt[key])
        in_.t.r.append(d)
        out.t.w = d
        out.t.r = []
        self.ninst += 1
        return d

    def allgather(self, out, in_, n=NCORES):
        self._wait("pool", self._deps([in_.t], [out.t]))
        key = self.dsem(out.t.name)
        ins = self.nc.gpsimd.collective_compute("AllGather", op=ALU.bypass, replica_groups=[list(range(n))],
                                                ins=[in_.ap], outs=[out.ap])
        self.cnt[key] += 16
        ins.then_inc(self.sem[key], 16)
        d = (key, self.cnt[key])
        in_.t.r.append(d)
        out.t.w = d
        out.t.r = []
        self.ninst += 1
        return d

    def finish(self, e, tensors):
        self._wait(e, [t.w for t in tensors])

    def mm(self, out, lhsT, rhs, start=True, stop=True):
        return self.op("pe", lambda e: e.matmul(out.ap, lhsT.ap, rhs.ap, start=start, stop=stop),
                       reads=[lhsT.t, rhs.t], writes=[out.t])

    def tr(self, out, in_, ident):
        return self.op("pe", lambda e: e.transpose(out.ap, in_.ap, ident.ap),
                       reads=[in_.t, ident.t], writes=[out.t])

    def act(self, out, in_, func, bias=0.0, scale=1.0, accum=None, e="act"):
        rd = _ts(in_, bias, scale)
        wr = _ts(out, accum)
        kw = {}
        if accum is not None:
            kw["accum_out"] = accum.ap
        return self.op("act", lambda g: g.activation(out=out.ap, in_=in_.ap, func=func,
                                                     bias=_ap(bias), scale=_ap(scale), **kw),
                       reads=rd, writes=wr)

    def ts(self, e, out, in0, s1, s2, op0, op1=None, accum=None):
        rd = _ts(in0, s1, s2)
        wr = _ts(out, accum)
        kw = {}
        if op1 is not None:
            kw["op1"] = op1
        if accum is not None:
            kw["accum_out"] = accum.ap
        return self.op(e, lambda g: g.tensor_scalar(out.ap, in0.ap, _ap(s1), _ap(s2), op0, **kw),
                       reads=rd, writes=wr)

    def tt(self, e, out, in0, in1, op):
        return self.op(e, lambda g: g.tensor_tensor(out.ap, in0.ap, in1.ap, op),
                       reads=_ts(in0, in1), writes=_ts(out))

    def stt(self, out, in0, s, in1, op0, op1):
        return self.op("dve", lambda g: g.scalar_tensor_tensor(out.ap, in0.ap, _ap(s), in1.ap, op0, op1),
                       reads=_ts(in0, s, in1), writes=_ts(out))

    def cp(self, e, out, in_):
        if e == "act":
            return self.act(out, in_, AF.Copy)
        return self.op(e, lambda g: g.tensor_copy(out.ap, in_.ap), reads=_ts(in_), writes=_ts(out))

    def red(self, out, in_, op, axis=AX.X):
        return self.op("dve", lambda g: g.tensor_reduce(out.ap, in_.ap, axis, op),
                       reads=_ts(in_), writes=_ts(out))

    def scan(self, out, d0, d1, init, op0, op1):
        return self.op("dve", lambda g: g.tensor_tensor_scan(out.ap, d0.ap, d1.ap, _ap(init), op0, op1),
                       reads=_ts(d0, d1, init), writes=_ts(out))

    def recip(self, out, in_):
        return self.op("dve", lambda g: g.reciprocal(out.ap, in_.ap), reads=_ts(in_), writes=_ts(out))

    def memset(self, e, out, val):
        return self.op(e, lambda g: g.memset(out.ap, val), reads=[], writes=_ts(out))

    def max8(self, out, in_):
        return self.op("dve", lambda g: g.max(out.ap, in_.ap), reads=_ts(in_), writes=_ts(out))


class Small:
    def __init__(self, P, name, n=64):
        self.P = P
        self.name = name
        self.k = 0

    def col(self, w=1):
        self.k += 1
        return self.P.sb(f"{self.name}_{self.k}", [128, w], F32)


def ln_tok(P, x, out, grep, brep, S, junk):
    msum = S.col(); negmean = S.col(); ss = S.col(); std = S.col(); rstd = S.col()
    P.red(msum[:, :], x, ALU.add)
    P.ts("dve", negmean[:, :], msum[:, :], -1.0 / D, None, ALU.mult)
    P.act(junk, x, AF.Square, bias=negmean[:, :], scale=1.0, accum=ss[:, :])
    P.ts("dve", std[:, :], ss[:, :], 1.0 / D, LN_EPS, ALU.mult, ALU.add)
    P.act(std[:, :], std[:, :], AF.Sqrt)
    P.recip(rstd[:, :], std[:, :])
    P.ts("dve", out, x, negmean[:, :], rstd[:, :], ALU.add, ALU.mult)
    P.tt("dve", out, out, grep, ALU.mult)
    P.tt("dve", out, out, brep, ALU.add)


def build_ln_in():
    nc = bass.Bass("TRN2", target_bir_lowering=False)
    es = ExitStack()
    P = Prog(nc, es)
    x = P.dram("x", [TOWN, D], F32, kind="ExternalInput")
    g = P.dram("g", [128, D], F32, kind="ExternalInput")
    b = P.dram("b", [128, D], F32, kind="ExternalInput")
    y = P.dram("y", [TOWN, D], F32, kind="ExternalOutput")
    gs = P.sb("gs", [128, D]); bs = P.sb("bs", [128, D])
    P.dma("sp", gs[:, :], g[:, :]); P.dma("sp", bs[:, :], b[:, :])
    S = Small(P, "lnin")
    junk = P.sb("junk", [128, D])
    xs = [P.sb(f"x{i}", [128, D]) for i in range(2)]
    os_ = [P.sb(f"o{i}", [128, D]) for i in range(2)]
    for i in range(TOWN // 128):
        xt = xs[i % 2]; ot = os_[i % 2]
        P.dma("sp", xt[:, :], x[i * 128:(i + 1) * 128, :])
        ln_tok(P, xt[:, :], ot[:, :], gs[:, :], bs[:, :], S, junk[:, :])
        P.dma("sp", y[i * 128:(i + 1) * 128, :], ot[:, :])
    P.finish("sp", [y])
    es.close()
    return nc


class Ctx:
    pass


def barrier(P):
    allk = [(k, v) for k, v in P.cnt.items() if v > 0]
    for e in P.eng:
        P._wait(e, allk)


class Stage:
    def __init__(self, P):
        self.P = P

    def __enter__(self):
        self.old = self.P.es
        self.es = ExitStack()
        self.P.es = self.es
        return self

    def __exit__(self, *a):
        barrier(self.P)
        self.P.es = self.old
        self.es.close()
        return False


def load_w(P, dst, src_ap_tt, q="pool"):
    return P.dma(q, dst, src_ap_tt)


def wview(w_in, c0, n):
    return V(w_in, w_in.h[:, c0:c0 + n].rearrange("(kt k) n -> k kt n", k=128))


def stage_window(P, C):
    pb = C.pb
    with Stage(P):
        wsu = P.sb("wsu", [128, 8, 512], BF16)
        wck = P.sb("wck", [128, 8, 128], BF16)
        wki = P.sb("wki", [128, 8, 32], BF16)
        P.dma("pool", wsu[:, :, :], wview(C.w_in, C_SU, 512))
        P.dma("pool", wck[:, :, :], wview(C.w_in, C_CKV, 128))
        P.dma("pool", wki[:, :, :], wview(C.w_in, C_KI, 32))
        kvg = P.sb("kvg_s", [128, 1]); P.dma("sp", kvg[:, :], C.kvg[:, :])
        kvgrow = P.sb("kvgrow_s", [128, 128]); P.dma("sp", kvgrow[:, :], C.kvgrow[:, :])
        hTc = [P.sb(f"hTc{i}", [128, 8, 512], BF16) for i in range(2)]
        ust = [P.sb(f"ust{i}", [128, 512]) for i in range(2)]
        sq = P.sb("sq", [128, 512], BF16)
        rst = P.sb("rst", [128, 512])
        junk = P.sb("wjunk", [128, 128])
        S = Small(P, "win")
        for g in range(C.pad // 512, WIN // 512):
            h = hTc[g % 2]
            P.dma("pool", h[:, :, :], V(C.hT, C.hT.h[:, C.off + g * 512:C.off + (g + 1) * 512].rearrange("(kt k) t -> k kt t", k=128)))
            for ct in range(4):
                ps = pb[ct % 2]
                for kt in range(8):
                    P.mm(ps[:, :], wsu[:, kt, ct * 128:(ct + 1) * 128], h[:, kt, :], start=(kt == 0), stop=(kt == 7))
                u = ust[ct % 2]
                P.cp("act", u[:, :], ps[:, :])
                P.dma("sp", C.uT[ct * 128:(ct + 1) * 128, g * 512:(g + 1) * 512], u[:, :])
            ps = pb[2]
            for kt in range(8):
                P.mm(ps[:, :], wck[:, kt, :], h[:, kt, :], start=(kt == 0), stop=(kt == 7))
            P.act(sq[:, :], ps[:, :], AF.Square)
            P.mm(pb[3][:, :], C.ones_bf[:, :], sq[:, :])
            P.ts("dve", rst[:, :], pb[3][:, :], 1.0 / 128, LN_EPS, ALU.mult, ALU.add)
            P.act(rst[:, :], rst[:, :], AF.Sqrt)
            P.recip(rst[:, :], rst[:, :])
            P.stt(C.ckvT[:, g * 512:(g + 1) * 512], ps[:, :], kvg[:, :], rst[:, :], ALU.mult, ALU.mult)
            ps = pb[4]
            for tt_ in range(4):
                for kt in range(8):
                    P.mm(ps[:, tt_ * 128:(tt_ + 1) * 128], h[:, kt, tt_ * 128:(tt_ + 1) * 128], wck[:, kt, :],
                         start=(kt == 0), stop=(kt == 7))
            for tt_ in range(4):
                ss = S.col(); rs = S.col()
                P.act(junk[:, :], ps[:, tt_ * 128:(tt_ + 1) * 128], AF.Square, accum=ss[:, :])
                P.ts("dve", rs[:, :], ss[:, :], 1.0 / 128, LN_EPS, ALU.mult, ALU.add)
                P.act(rs[:, :], rs[:, :], AF.Sqrt)
                P.recip(rs[:, :], rs[:, :])
                P.stt(C.ckv_tok[:, g * 4 + tt_, 0:128], ps[:, tt_ * 128:(tt_ + 1) * 128], rs[:, :], kvgrow[:, :],
                      ALU.mult, ALU.mult)
            ps = pb[5]
            for kt in range(8):
                P.mm(ps[0:32, :], wki[:, kt, :], h[:, kt, :], start=(kt == 0), stop=(kt == 7))
            P.cp("act", C.kidxT[0:32, g * 512:(g + 1) * 512], ps[0:32, :])


SEG = 512
NSEG = WIN // SEG
OWN0 = PRE // SEG


def reduce_turns(P, out_f, u, tmp):
    P.ts("dve", tmp, u, MAGIC, None, ALU.add)
    P.ts("dve", tmp, tmp, MAGIC, None, ALU.subtract)
    P.tt("dve", out_f, u, tmp, ALU.subtract)


def sincos(P, S_out, C_out, f, tmp):
    P.act(S_out, f, AF.Sin, scale=TWO_PI)
    P.act(tmp, f, AF.Abs)
    P.act(C_out, tmp, AF.Sin, scale=-TWO_PI, bias=C_halfpi)


C_halfpi = None


def stage_ssm(P, C):
    global C_halfpi
    pb = C.pb
    with Stage(P):
        halfpi = P.sb("halfpi", [128, 1]); P.memset("dve", halfpi[:, :], math.pi / 2)
        C_halfpi = halfpi[:, :]
        lhs_bu = P.sb("lhs_bu", [128, 16, 2, 128], BF16)
        lhs_c = P.sb("lhs_c", [128, 16, 2, 128], BF16)
        rcol = P.sb("rcol", [128, 16]); fturn = P.sb("fturn", [128, 16]); f0 = P.sb("f0", [128, 16, NSEG])
        dsk = P.sb("dsk", [128, 4]); P.dma("sp", dsk[:, :], C.dskip[:, :])
        bgl = P.sb("bgl", [128, 4]); P.dma("sp", bgl[:, :], C.bglu[:, :])
        with Stage(P):
            def ld(name, src, shape):
                t = P.sb(name, shape);
                P.dma("sp", t[tuple(slice(None) for _ in shape)], src[tuple(slice(None) for _ in shape)])
                return t
            lre = ld("lre", C.lamre, [128, 16]); lim = ld("lim", C.lamim, [128, 16]); ldt = ld("ldt", C.logdt, [128, 16])
            bre = ld("bre_s", C.bre, [128, 16, 16]); bim = ld("bim_s", C.bim, [128, 16, 16])
            cre = ld("cre_s", C.cre, [128, 16, 16]); cim = ld("cim_s", C.cim, [128, 16, 16])
            n16 = lambda nm: P.sb(nm, [128, 16])
            dt = n16("dt"); lnr = n16("lnr"); th = n16("th"); tmp = n16("tmp16"); ff = n16("ff")
            sn = n16("sn"); cs = n16("cs"); ar = n16("ar"); ai = n16("ai"); den = n16("den")
            kr = n16("kr"); ki = n16("ki"); nki = n16("nki"); t1 = n16("t1_16"); t2 = n16("t2_16")
            A = slice(None)
            P.act(dt[:, :], ldt[:, :], AF.Exp)
            P.tt("dve", lnr[:, :], lre[:, :], dt[:, :], ALU.mult)
            P.tt("dve", th[:, :], lim[:, :], dt[:, :], ALU.mult)
            P.ts("dve", fturn[:, :], th[:, :], 1.0 / TWO_PI, None, ALU.mult)
            P.act(rcol[:, :], lnr[:, :], AF.Exp)
            reduce_turns(P, ff[:, :], fturn[:, :], tmp[:, :])
            sincos(P, sn[:, :], cs[:, :], ff[:, :], tmp[:, :])
            P.tt("dve", ar[:, :], rcol[:, :], cs[:, :], ALU.mult)
            P.tt("dve", ai[:, :], rcol[:, :], sn[:, :], ALU.mult)
            P.ts("dve", ar[:, :], ar[:, :], -1.0, None, ALU.add)
            P.tt("dve", den[:, :], lre[:, :], lre[:, :], ALU.mult)
            P.tt("dve", t1[:, :], lim[:, :], lim[:, :], ALU.mult)
            P.tt("dve", den[:, :], den[:, :], t1[:, :], ALU.add)
            P.recip(den[:, :], den[:, :])
            P.tt("dve", t1[:, :], ar[:, :], lre[:, :], ALU.mult)
            P.tt("dve", t2[:, :], ai[:, :], lim[:, :], ALU.mult)
            P.tt("dve", kr[:, :], t1[:, :], t2[:, :], ALU.add)
            P.tt("dve", kr[:, :], kr[:, :], den[:, :], ALU.mult)
            P.tt("dve", t1[:, :], ai[:, :], lre[:, :], ALU.mult)
            P.tt("dve", t2[:, :], ar[:, :], lim[:, :], ALU.mult)
            P.tt("dve", ki[:, :], t1[:, :], t2[:, :], ALU.subtract)
            P.tt("dve", ki[:, :], ki[:, :], den[:, :], ALU.mult)
            P.ts("dve", nki[:, :], ki[:, :], -1.0, None, ALU.mult)
            for q in range(NSEG):
                P.ts("dve", f0[:, :, q], fturn[:, :], float(SEG * q), None, ALU.mult)
            ftmp = P.sb("ftmp", [128, 16, NSEG])
            reduce_turns(P, f0[:, :, :], f0[:, :, :], ftmp[:, :, :])
            bbr = P.sb("bbr", [128, 16, 16]); bbi = P.sb("bbi", [128, 16, 16]); tb = P.sb("tb", [128, 16])
            Sp = P.sb("Sp", [128, 32, 128])
            P.memset("pool", Sp[:, :, :], 0.0)
            lcf = P.sb("lcf", [128, 32, 128])
            P.memset("pool", lcf[:, :, :], 0.0)
            ncim = P.sb("ncim", [128, 16, 16])
            P.ts("dve", ncim[:, :, :], cim[:, :, :], -1.0, None, ALU.mult)
            for i in range(16):
                P.ts("dve", tb[:, :], bre[:, i, :], kr[:, i:i + 1], None, ALU.mult)
                P.stt(bbr[:, i, :], bim[:, i, :], nki[:, i:i + 1], tb[:, :], ALU.mult, ALU.add)
                P.ts("dve", tb[:, :], bim[:, i, :], kr[:, i:i + 1], None, ALU.mult)
                P.stt(bbi[:, i, :], bre[:, i, :], ki[:, i:i + 1], tb[:, :], ALU.mult, ALU.add)
                c0 = 32 * (i % 4)
                for gg in range(2):
                    rows = slice(64 * gg, 64 * gg + 64)
                    cols = slice(c0 + 16 * gg, c0 + 16 * gg + 16)
                    P.cp("dve", Sp[rows, 2 * i, cols], bbr[rows, i, :])
                    P.cp("dve", Sp[rows, 2 * i + 1, cols], bbi[rows, i, :])
                    P.cp("dve", lcf[rows, 2 * i, cols], cre[rows, i, :])
                    P.cp("dve", lcf[rows, 2 * i + 1, cols], ncim[rows, i, :])
            for i in range(16):
                for ri in range(2):
                    ps = pb[(2 * i + ri) % 4]
                    P.tr(ps[:, 0:128], Sp[:, 2 * i + ri, :], C.ident[:, :])
                    P.cp("act", lhs_bu[:, i, ri, :], ps[:, 0:128])
                    P.cp("pool", lhs_c[:, i, ri, :], lcf[:, 2 * i + ri, :])
        iota = P.sb("iota", [128, SEG])
        P.op("pool", lambda g: g.iota(iota.h[:, :], [[1, SEG]], 0, channel_multiplier=0, allow_small_or_imprecise_dtypes=True),
             reads=[], writes=[iota])
        ones = P.sb("ones_s", [128, SEG]); P.memset("dve", ones[:, :], 1.0)
        rbc = P.sb("rbc", [128, SEG])
        uTt = P.sb("uTt", [128, WIN], BF16)
        mk = lambda nm, dt_=F32: P.sb(nm, [128, SEG], dt_)
        tu = mk("tu"); tn = mk("tn"); tf = mk("tf"); tS = mk("tS"); tC = mk("tC")
        t1 = mk("r1"); t2 = mk("r2"); t3 = mk("r3"); t4 = mk("r4")
        zs = [[mk(f"zs{a}{b}") for b in range(2)] for a in range(2)]
        zro = P.sb("zro", [128, TOWN]); zio = P.sb("zio", [128, TOWN])
        hr = mk("hr", BF16); hi = mk("hi", BF16)
        zbf = P.sb("zbf", [128, 4, TOWN], BF16)
        u32 = P.sb("u32", [128, TOWN]); yy = P.sb("yy", [128, TOWN]); y2 = P.sb("y2", [128, TOWN])
        for i in range(16):
            ct = i // 4
            if i % 4 == 0:
                P.dma("pool", uTt[:, C.pad:WIN], C.uT[ct * 128:(ct + 1) * 128, C.pad:WIN])
                P.dma("sp", u32[:, :], C.uT[ct * 128:(ct + 1) * 128, PRE:WIN])
            P.ts("dve", rbc[:, :], ones[:, :], rcol[:, i:i + 1], None, ALU.mult)
            prev = None
            for q in range(C.pad // SEG, NSEG):
                own = q >= OWN0
                P.ts("dve", tu[:, :], iota[:, :], fturn[:, i:i + 1], f0[:, i, q:q + 1], ALU.mult, ALU.add)
                reduce_turns(P, tf[:, :], tu[:, :], tn[:, :])
                sincos(P, tS[:, :], tC[:, :], tf[:, :], tn[:, :])
                pr = pb[q % 2]; pi_ = pb[2 + q % 2]
                P.mm(pr[:, :], lhs_bu[:, i, 0, :], uTt[:, q * SEG:(q + 1) * SEG])
                P.mm(pi_[:, :], lhs_bu[:, i, 1, :], uTt[:, q * SEG:(q + 1) * SEG])
                P.tt("dve", t1[:, :], tC[:, :], pr[:, :], ALU.mult)
                P.tt("dve", t2[:, :], tS[:, :], pi_[:, :], ALU.mult)
                P.tt("dve", t3[:, :], tC[:, :], pi_[:, :], ALU.mult)
                P.tt("dve", t4[:, :], tS[:, :], pr[:, :], ALU.mult)
                P.tt("pool", t1[:, :], t1[:, :], t2[:, :], ALU.add)
                P.tt("pool", t3[:, :], t3[:, :], t4[:, :], ALU.subtract)
                if own:
                    o = (q - OWN0) * SEG
                    zr_o = zro[:, o:o + SEG]; zi_o = zio[:, o:o + SEG]
                else:
                    zr_o = zs[0][q % 2][:, :]; zi_o = zs[1][q % 2][:, :]
                ir = 0.0 if prev is None else prev[0]
                ii = 0.0 if prev is None else prev[1]
                P.scan(zr_o, rbc[:, :], t1[:, :], ir, ALU.mult, ALU.add)
                P.scan(zi_o, rbc[:, :], t3[:, :], ii, ALU.mult, ALU.add)
                if own:
                    prev = (zro[:, o + SEG - 1:o + SEG], zio[:, o + SEG - 1:o + SEG])
                else:
                    prev = (zs[0][q % 2][:, SEG - 1:SEG], zs[1][q % 2][:, SEG - 1:SEG])
                if own:
                    P.tt("pool", t2[:, :], tC[:, :], zr_o, ALU.mult)
                    P.tt("pool", t4[:, :], tS[:, :], zi_o, ALU.mult)
                    P.tt("dve", hr[:, :], t2[:, :], t4[:, :], ALU.subtract)
                    P.tt("pool", t2[:, :], tS[:, :], zr_o, ALU.mult)
                    P.tt("pool", t4[:, :], tC[:, :], zi_o, ALU.mult)
                    P.tt("dve", hi[:, :], t2[:, :], t4[:, :], ALU.add)
                    py = pb[4 + (q - OWN0)]
                    P.mm(py[:, :], lhs_c[:, i, 0, :], hr[:, :], start=(i % 4 == 0), stop=False)
                    P.mm(py[:, :], lhs_c[:, i, 1, :], hi[:, :], start=False, stop=(i % 4 == 3))
            if i % 4 == 3:
                for s in range(4):
                    sl = slice(s * SEG, (s + 1) * SEG)
                    P.stt(yy[:, sl], u32[:, sl], dsk[:, ct:ct + 1], pb[4 + s][:, :], ALU.mult, ALU.add)
                P.tt("pool", y2[:, :], yy[:, :], yy[:, :], ALU.mult)
                P.ts("dve", y2[:, :], y2[:, :], 0.0713548163, 1.5957691216, ALU.mult, ALU.add)
                P.tt("dve", y2[:, :], y2[:, :], yy[:, :], ALU.mult)
                P.act(y2[:, :], y2[:, :], AF.Sigmoid)
                P.tt("dve", yy[:, :], yy[:, :], y2[:, :], ALU.mult)
                P.cp("act", zbf[:, ct, :], yy[:, :])
                P.dma("sp", C.z32[ct * 128:(ct + 1) * 128, :], yy[:, :])
        wgl = P.sb("wgl", [128, 4, 512], BF16)
        P.dma("pool", wgl[:, :, :], V(C.w_glu, C.w_glu.h[:, :].rearrange("(kt k) n -> k kt n", k=128)))
        for co in range(4):
            P.dma("sp", u32[:, :], C.z32[co * 128:(co + 1) * 128, :])
            for tg in range(4):
                ps = pb[tg % 4]
                sl = slice(tg * 512, (tg + 1) * 512)
                for kt in range(4):
                    P.mm(ps[:, :], wgl[:, kt, co * 128:(co + 1) * 128], zbf[:, kt, sl], start=(kt == 0), stop=(kt == 3))
                P.act(y2[:, sl], ps[:, :], AF.Sigmoid, bias=bgl[:, co:co + 1])
                P.tt("dve", hr[:, :], u32[:, sl], y2[:, sl], ALU.mult)
                P.dma("sp", C.brT[2, co * 128:(co + 1) * 128, sl], hr[:, :])


def proj_fm(P, C, ps, w, c0, n, tg, rows=None):
    for kt in range(8):
        P.mm(ps[0:n, :], w[:, kt, c0:c0 + n], C.hTo[:, kt, tg * 512:(tg + 1) * 512], start=(kt == 0), stop=(kt == 7))


def stage_conv(P, C):
    pb = C.pb
    with Stage(P):
        wcu = P.sb("wcu", [128, 8, 512], BF16); wgb = P.sb("wgb", [128, 8, 512], BF16); wgc = P.sb("wgc", [128, 8, 512], BF16)
        P.dma("pool", wcu[:, :, :], wview(C.w_in, C_CU, 512))
        P.dma("pool", wgb[:, :, :], wview(C.w_in, C_GB, 512))
        P.dma("pool", wgc[:, :, :], wview(C.w_in, C_GC, 512))
        cw = P.sb("cw", [128, 4, 3]); P.dma("sp", cw[:, :, :], C.convw[:, :, :])
        cb = P.sb("cb", [128, 4]); P.dma("sp", cb[:, :], C.convb[:, :])
        hh = P.sb("hhalo", [128, 8, 2], BF16)
        P.dma("pool", hh[:, :, :], V(C.hT, C.hT.h[:, C.off + PRE - 2:C.off + PRE].rearrange("(kt k) t -> k kt t", k=128)))
        v = P.sb("cv", [128, TOWN + 2]); us = P.sb("cus", [128, 512]); y = P.sb("cy", [128, TOWN])
        ob = P.sb("cob", [128, 512], BF16)
        for ct in range(4):
            cs = slice(ct * 128, (ct + 1) * 128)
            for kt in range(8):
                P.mm(pb[0][:, 0:2], wcu[:, kt, cs], hh[:, kt, :], start=(kt == 0), stop=(kt == 7))
            for kt in range(8):
                P.mm(pb[1][:, 0:2], wgc[:, kt, cs], hh[:, kt, :], start=(kt == 0), stop=(kt == 7))
            P.cp("act", us[:, 0:2], pb[0][:, 0:2])
            P.tt("dve", v[:, 0:2], us[:, 0:2], pb[1][:, 0:2], ALU.mult)
            for tg in range(4):
                proj_fm(P, C, pb[2], wcu, ct * 128, 128, tg)
                proj_fm(P, C, pb[3], wgc, ct * 128, 128, tg)
                P.cp("act", us[:, :], pb[2][:, :])
                P.tt("dve", v[:, 2 + tg * 512:2 + (tg + 1) * 512], us[:, :], pb[3][:, :], ALU.mult)
            P.ts("dve", y[:, :], v[:, 2:TOWN + 2], cw[:, ct, 2:3], cb[:, ct:ct + 1], ALU.mult, ALU.add)
            P.stt(y[:, :], v[:, 1:TOWN + 1], cw[:, ct, 1:2], y[:, :], ALU.mult, ALU.add)
            P.stt(y[:, :], v[:, 0:TOWN], cw[:, ct, 0:1], y[:, :], ALU.mult, ALU.add)
            for tg in range(4):
                proj_fm(P, C, pb[4 + tg % 2], wgb, ct * 128, 128, tg)
                P.tt("dve", ob[:, :], y[:, tg * 512:(tg + 1) * 512], pb[4 + tg % 2][:, :], ALU.mult)
                P.dma("sp", C.brT[1, cs, tg * 512:(tg + 1) * 512], ob[:, :])


def stage_mem(P, C):
    pb = C.pb
    with Stage(P):
        wmq = P.sb("wmq", [128, 8, 512], BF16)
        P.dma("pool", wmq[:, :, :], wview(C.w_in, C_MQ, 512))
        wkv = P.sb("wkv", [128, 8, 1024], BF16)
        P.dma("pool", wkv[:, :, :], V(C.w_mem, C.w_mem.h[:, :].rearrange("(kt k) n -> k kt n", k=128)))
        mT = P.sb("mT", [128, 8, 256], BF16)
        P.dma("pool", mT[:, :, :], V(C.memT, C.memT.h[:, :].rearrange("(kt k) m -> k kt m", k=128)))
        KT = P.sb("KT", [128, 4, 256], BF16)
        Vt = P.sb("Vt", [128, 2, 512], BF16)
        for h in range(4):
            for kt in range(8):
                P.mm(pb[0][:, 0:256], wkv[:, kt, h * 128:(h + 1) * 128], mT[:, kt, :], start=(kt == 0), stop=(kt == 7))
            P.cp("act", KT[:, h, :], pb[0][:, 0:256])
        for mt in range(2):
            for kt in range(8):
                P.mm(pb[1][:, :], mT[:, kt, mt * 128:(mt + 1) * 128], wkv[:, kt, 512:1024], start=(kt == 0), stop=(kt == 7))
            P.cp("act", Vt[:, mt, :], pb[1][:, :])
        mq = P.sb("mq", [128, 512], BF16); pT = P.sb("mpT", [128, 2, 512], BF16)
        rec = P.sb("mrec", [128, 512]); ob = P.sb("mob", [128, 512], BF16)
        for h in range(4):
            for tg in range(4):
                proj_fm(P, C, pb[2], wmq, h * 128, 128, tg)
                P.act(mq[:, :], pb[2][:, :], AF.Copy, scale=128.0 ** -0.5)
                for mt in range(2):
                    P.mm(pb[3 + mt][:, :], KT[:, h, mt * 128:(mt + 1) * 128], mq[:, :])
                    P.act(pT[:, mt, :], pb[3 + mt][:, :], AF.Exp)
                for mt in range(2):
                    P.mm(pb[5][:, :], Vt[:, mt, h * 128:(h + 1) * 128], pT[:, mt, :], start=(mt == 0), stop=(mt == 1))
                for mt in range(2):
                    P.mm(pb[6][:, :], C.ones_bf[:, :], pT[:, mt, :], start=(mt == 0), stop=(mt == 1))
                P.recip(rec[:, :], pb[6][:, :])
                P.tt("dve", ob[:, :], rec[:, :], pb[5][:, :], ALU.mult)
                P.dma("sp", C.brT[3, h * 128:(h + 1) * 128, tg * 512:(tg + 1) * 512], ob[:, :])


def stage_attproj(P, C):
    pb = C.pb
    with Stage(P):
        wq = P.sb("wq", [128, 8, 512], BF16); P.dma("pool", wq[:, :, :], wview(C.w_in, C_Q, 512))
        wqi = P.sb("wqi", [128, 8, 256], BF16); P.dma("pool", wqi[:, :, :], wview(C.w_in, C_QI, 256))
        wwi = P.sb("wwi", [128, 8, 8], BF16); P.dma("pool", wwi[:, :, :], wview(C.w_in, C_WI, 8))
        wuk = P.sb("wuk", [128, 512]); P.dma("sp", wuk[:, :], C.w_uk[:, :])
        wukT = P.sb("wukT", [128, 4, 128], BF16)
        for m in range(4):
            P.tr(pb[0][:, 0:128], wuk[:, m * 128:(m + 1) * 128], C.ident[:, :])
            P.cp("act", wukT[:, m, :], pb[0][:, 0:128])
        qT = P.sb("qT", [128, 4, TOWN], BF16)
        for m in range(4):
            for tg in range(4):
                proj_fm(P, C, pb[1 + tg % 2], wq, m * 128, 128, tg)
                P.cp("act", qT[:, m, tg * 512:(tg + 1) * 512], pb[1 + tg % 2][:, :])
        st = [P.sb(f"qst{i}", [128, 512], BF16) for i in range(2)]
        k = 0
        for h in range(8):
            m, hh = h // 2, h % 2
            rows = slice(64 * hh, 64 * hh + 64)
            for tg in range(4):
                ps = pb[3 + k % 2]; s = st[k % 2]; k += 1
                P.mm(ps[:, :], wukT[rows, m, :], qT[rows, m, tg * 512:(tg + 1) * 512])
                P.act(s[:, :], ps[:, :], AF.Copy, scale=0.125)
                P.dma("sp", C.qlat_d[:, h, tg * 512:(tg + 1) * 512], s[:, :])
        for h in range(8):
            for tg in range(4):
                ps = pb[5 + k % 2]; s = st[k % 2]; k += 1
                proj_fm(P, C, ps, wqi, h * 32, 32, tg)
                P.cp("act", s[0:32, :], ps[0:32, :])
                P.dma("sp", C.qidx_d[0:32, h, tg * 512:(tg + 1) * 512], s[0:32, :])
        for tt_ in range(16):
            for kt in range(8):
                P.mm(pb[7][:, 0:8], C.hTo[:, kt, tt_ * 128:(tt_ + 1) * 128], wwi[:, kt, :], start=(kt == 0), stop=(kt == 7))
            P.cp("act", C.widx[:, tt_, :], pb[7][:, 0:8])


def stage_att(P, C):
    pb = C.pb
    with Stage(P):
        acc = P.sb("acc", [128, WIN]); notsel = P.sb("notsel", [128, WIN], BF16); bj = P.sb("bj", [128, WIN], BF16)
        tmp = [P.sb(f"atmp{i}", [128, 512]) for i in range(2)]
        qi = P.sb("qi", [32, 8, 128], BF16); ql = P.sb("ql", [128, 8, 128], BF16)
        npad = P.sb("npad_s", [128, 1]); P.dma("sp", npad[:, :], C.npad[C.j, :, :])
        padc = P.sb("padc", [128, 64]); P.dma("sp", padc[:, :], C.padcol[C.j, :, :])
        tri = P.sb("tri_s", [128, 128]); P.dma("sp", tri[:, :], C.tri[:, :])
        negI = P.sb("negI", [128, 4, 128], BF16)
        for j in range(4):
            P.ts("dve", negI[:, j, :], C.ident[:, :], NEG, None, ALU.mult)
        wuv = P.sb("wuv", [128, 512]); P.dma("sp", wuv[:, :], C.w_uv[:, :])
        wuvp = P.sb("wuvp", [128, 8, 128], BF16)
        P.memset("pool", wuvp[:, :, :], 0.0)
        for h in range(8):
            P.cp("dve", wuvp[:, h, 64 * (h % 2):64 * (h % 2) + 64], wuv[:, h * 64:(h + 1) * 64])
        pTs = [P.sb(f"pT{i}", [128, 512], BF16) for i in range(3)]
        ol = P.sb("ol", [128, 8, 128]); olT = P.sb("olT", [128, 8, 128], BF16)
        ab = P.sb("ab", [128, 4, 128], BF16)
        S = Small(P, "att")
        lo = S.col(); hi = S.col(); mid = S.col(); cnt = S.col(); c2 = S.col(); ge = S.col(); d1 = S.col(); d2 = S.col()
        rec = S.col(8)
        kk = 0
        for qb in range(TOWN // 128):
            nk = PRE // 128 + qb + 1
            NK = nk * 128
            qs = slice(qb * 128, (qb + 1) * 128)
            P.dma("sp", qi[:, :, :], C.qidx_d[0:32, :, qs])
            P.dma("sp", ql[:, :, :], C.qlat_d[:, :, qs])
            pad = C.pad
            nsp = (NK - pad + 511) // 512
            for h in range(8):
                for s in range(nsp):
                    n = min(512, NK - pad - 512 * s)
                    ks = slice(pad + 512 * s, pad + 512 * s + n)
                    ps = pb[kk % 2]; t = tmp[kk % 2]; kk += 1
                    P.mm(ps[:, 0:n], qi[0:32, h, :], C.kidxT[0:32, ks])
                    P.act(t[:, 0:n], ps[:, 0:n], AF.Relu)
                    if h == 0:
                        P.ts("dve", acc[:, ks], t[:, 0:n], C.widx[:, qb, 0:1], None, ALU.mult)
                    else:
                        P.stt(acc[:, ks], t[:, 0:n], C.widx[:, qb, h:h + 1], acc[:, ks], ALU.mult, ALU.add)
            P.red(hi[:, :], acc[:, pad:NK], ALU.max)
            P.red(lo[:, :], acc[:, pad:NK], ALU.min)
            P.tt("dve", acc[:, NK - 128:NK], acc[:, NK - 128:NK], tri[:, :], ALU.add)
            P.tt("dve", d2[:, :], hi[:, :], lo[:, :], ALU.subtract)
            for it in range(NBIS):
                P.ts("dve", mid[:, :], d2[:, :], 0.5 ** (it + 1), lo[:, :], ALU.mult, ALU.add)
                P.ts("dve", bj[:, pad:NK], acc[:, pad:NK], mid[:, :], None, ALU.is_ge, op1=ALU.add, accum=cnt[:, :])
                P.ts("dve", ge[:, :], cnt[:, :], 256.0, None, ALU.is_ge)
                P.ts("dve", d1[:, :], d2[:, :], 0.5 ** (it + 1), ge[:, :], ALU.mult, ALU.mult)
                P.tt("dve", lo[:, :], lo[:, :], d1[:, :], ALU.add)
            P.ts("dve", notsel[:, pad:NK], acc[:, pad:NK], lo[:, :], None, ALU.is_lt)
            kc0 = pad // 128
            groups = [(kc, hg) for kc in range(kc0, nk) for hg in range(2)]

            def emit_qk(i):
                kc, hg = groups[i]
                cs = slice(kc * 128, (kc + 1) * 128)
                pl = pb[2 + i % 2]
                P.mm(pl[:, :], C.ckvT[:, cs], ql[:, 4 * hg:4 * hg + 4, :], start=True, stop=False)
                P.mm(pl[:, :], notsel[:, cs], negI[:, :, :], start=False, stop=True)

            def emit_exp_pv(i):
                kc, hg = groups[i]
                pl = pb[2 + i % 2]; pT = pTs[i % 3]
                P.act(pT[:, :], pl[:, :], AF.Exp)
                for h4 in range(4):
                    h = 4 * hg + h4
                    po = pb[4 + h // 3]
                    P.mm(po[:, (h % 3) * 129:(h % 3) * 129 + 129], pT[:, h4 * 128:(h4 + 1) * 128], C.ckv_tok[:, kc, :],
                         start=(kc == kc0), stop=(kc == nk - 1))

            emit_qk(0)
            for i in range(len(groups)):
                if i + 1 < len(groups):
                    emit_qk(i + 1)
                emit_exp_pv(i)
            for h in range(8):
                po = pb[4 + h // 3]; o = (h % 3) * 129
                P.recip(rec[:, h:h + 1], po[:, o + 128:o + 129])
                P.ts("dve", ol[:, h, :], po[:, o:o + 128], rec[:, h:h + 1], None, ALU.mult)
            for h in range(8):
                P.tr(pb[7][:, (h % 4) * 128:(h % 4) * 128 + 128], ol[:, h, :], C.ident[:, :])
                P.cp("act", olT[:, h, :], pb[7][:, (h % 4) * 128:(h % 4) * 128 + 128])
            for m in range(4):
                ps = pb[kk % 2]; kk += 1
                P.mm(ps[:, 0:128], wuvp[:, 2 * m, :], olT[:, 2 * m, :], start=True, stop=False)
                P.mm(ps[:, 0:128], wuvp[:, 2 * m + 1, :], olT[:, 2 * m + 1, :], start=False, stop=True)
                P.cp("act", ab[:, m, :], ps[:, 0:128])
            P.dma("sp", V(C.brT, C.brT.h[0, :, qs].rearrange("(m p) q -> p m q", p=128)), ab[:, :, :])


def stage_merge_a(P, C):
    pb = C.pb
    with Stage(P):
        macc = P.sb("macc", [128, 8, TOWN])
        brt = P.sb("brt", [128, 4, TOWN], BF16)
        wbr = P.sb("wbr", [128, 4, D], BF16)
        wg = P.sb("wg", [128, 8, D], BF16)
        sg = [P.sb(f"sg{i}", [128, 512]) for i in range(2)]
        tm = [P.sb(f"mtm{i}", [128, 512]) for i in range(2)]
        k = 0
        for r in range(4):
            P.dma("sp", brt[:, :, :], V(C.brT, C.brT.h[r, :, :].rearrange("(kt k) t -> k kt t", k=128)))
            P.dma("pool", wbr[:, :, :], V(C.w_br, C.w_br.h[r, :, :].rearrange("(kt k) n -> k kt n", k=128)))
            P.dma("pool", wg[:, :, :], wview(C.w_in, C_GATE + r * D, D))
            for dt_ in range(8):
                ds_ = slice(dt_ * 128, (dt_ + 1) * 128)
                for tg in range(4):
                    ts_ = slice(tg * 512, (tg + 1) * 512)
                    pg = pb[k % 2]; pr = pb[2 + k % 2]; s = sg[k % 2]; t = tm[k % 2]; k += 1
                    for kt in range(8):
                        P.mm(pg[:, :], wg[:, kt, ds_], C.hTo[:, kt, ts_], start=(kt == 0), stop=(kt == 7))
                    for kt in range(4):
                        P.mm(pr[:, :], wbr[:, kt, ds_], brt[:, kt, ts_], start=(kt == 0), stop=(kt == 3))
                    P.act(s[:, :], pg[:, :], AF.Sigmoid)
                    if r == 0:
                        P.tt("dve", macc[:, dt_, ts_], s[:, :], pr[:, :], ALU.mult)
                    else:
                        P.tt("dve", t[:, :], s[:, :], pr[:, :], ALU.mult)
                        P.tt("pool", macc[:, dt_, ts_], macc[:, dt_, ts_], t[:, :], ALU.add)
        for dt_ in range(8):
            P.cp("act" if dt_ % 2 else "dve", brt[:, dt_ % 4, :], macc[:, dt_, :])
            P.dma("sp", C.mT_d[dt_ * 128:(dt_ + 1) * 128, :], brt[:, dt_ % 4, :])


def stage_merge_b(P, C):
    pb = C.pb
    with Stage(P):
        mT = P.sb("mTb", [128, 8, TOWN], BF16)
        P.dma("sp", mT[:, :, :], V(C.mT_d, C.mT_d.h[:, :].rearrange("(kt k) t -> k kt t", k=128)))
        wo = P.sb("wo", [128, 8, D], BF16)
        P.dma("pool", wo[:, :, :], V(C.w_o, C.w_o.h[:, :].rearrange("(kt k) n -> k kt n", k=128)))
        g1 = P.sb("g1", [128, D]); b1 = P.sb("b1", [128, D])
        P.dma("sp", g1[:, :], C.ln1g[:, :]); P.dma("sp", b1[:, :], C.ln1b[:, :])
        wr = P.sb("wr32", [128, 8, 32])
        P.dma("sp", wr[:, :, :], V(C.w_r, C.w_r.h[:, :].rearrange("(kt k) n -> k kt n", k=128)))
        wrh = P.sb("wrh", [128, 8, 32], BF16); wrl = P.sb("wrl", [128, 8, 32], BF16)
        P.cp("dve", wrh[:, :, :], wr[:, :, :])
        P.tt("dve", wrl[:, :, :], wr[:, :, :], wrh[:, :, :], ALU.subtract)
        brr = P.sb("brr", [128, 32]); P.dma("sp", brr[:, :], C.b_r[:, :])
        ho = [P.sb(f"ho{i}", [128, D]) for i in range(2)]
        xx = [P.sb(f"xx{i}", [128, D]) for i in range(2)]
        h1 = [P.sb(f"h1_{i}", [128, D]) for i in range(2)]
        h32 = [P.sb(f"h32_{i}", [128, 128], BF16) for i in range(3)]
        junk = P.sb("mjunk", [128, D])
        S = Small(P, "ln1")
        k = 0
        for tt_ in range(16):
            tsl = slice(tt_ * 128, (tt_ + 1) * 128)
            hot = ho[tt_ % 2]; x = xx[tt_ % 2]; h = h1[tt_ % 2]
            P.dma("sp", hot[:, :], C.hown[C.off + tt_ * 128:C.off + (tt_ + 1) * 128, :])
            for dh in range(2):
                ps = pb[dh]
                for kt in range(8):
                    P.mm(ps[:, :], mT[:, kt, tsl], wo[:, kt, dh * 512:(dh + 1) * 512], start=(kt == 0), stop=(kt == 7))
                P.stt(x[:, dh * 512:(dh + 1) * 512], hot[:, dh * 512:(dh + 1) * 512], ALPHA, ps[:, :], ALU.mult, ALU.add)
            ln_tok(P, x[:, :], h[:, :], g1[:, :], b1[:, :], S, junk[:, :])
            P.act(C.yacc[tt_][:, :], h[:, :], AF.Copy, scale=ALPHA)
            for kt in range(8):
                pt = pb[2 + k % 4]; hh = h32[k % 3]; k += 1
                P.tr(pt[:, 0:128], h[:, kt * 128:(kt + 1) * 128], C.ident[:, :])
                P.cp("act", C.h1T[:, kt, tsl], pt[:, 0:128])
                P.tt("dve", hh[:, :], pt[:, 0:128], C.h1T[:, kt, tsl], ALU.subtract)
                P.mm(pb[6][:, 0:32], C.h1T[:, kt, tsl], wrh[:, kt, :], start=(kt == 0), stop=False)
                P.mm(pb[6][:, 0:32], C.h1T[:, kt, tsl], wrl[:, kt, :], start=False, stop=False)
                P.mm(pb[6][:, 0:32], hh[:, :], wrh[:, kt, :], start=False, stop=(kt == 7))
            P.tt("dve", C.rlog[:, tt_, :], pb[6][:, 0:32], brr[:, :], ALU.add)


def emit_hT(P, C, o, dst, t0, k):
    if not hasattr(C, "tst") or C.tst_owner is not P.es:
        C.tst = [P.sb(f"tst{i}", [128, 8, 128]) for i in range(2)]
        C.tst_owner = P.es
    st = C.tst[k % 2]
    for kt in range(8):
        ps = C.pb[(kt // 4) + 2 * (k % 2)]
        P.tr(ps[:, (kt % 4) * 128:(kt % 4) * 128 + 128], o[:, kt * 128:(kt + 1) * 128], C.ident[:, :])
    for half in range(2):
        ps = C.pb[half + 2 * (k % 2)]
        P.cp("act" if half else "dve", st[:, 4 * half:4 * half + 4, :], ps[:, :])
    P.dma("sp", V(dst, dst.h[:, PRE + t0:PRE + t0 + 128].rearrange("(kt k) t -> k kt t", k=128)), st[:, :, :])


def stage_moe(P, C):
    pb = C.pb
    with Stage(P):
        S = Small(P, "moe")
        gates = P.sb("gates", [128, 16, 32])
        gT = P.sb("gT", [32, TOWN], BF16)
        bdn = P.sb("bdn", [32, D], BF16); P.dma("pool", bdn[:, :], C.b_dn[:, :])
        bup = P.sb("bup", [128, 32, 8, 2]); P.dma("sp", bup[:, :, :, :], C.b_up[:, :, :, :])
        top8 = P.sb("top8", [128, 8]); nmx = S.col(); ssum = S.col(); ee = P.sb("ree", [128, 32]); mk = P.sb("rmk", [128, 32])
        for tt_ in range(16):
            lg = C.rlog[:, tt_, :]
            P.max8(top8[:, :], lg)
            P.ts("dve", nmx[:, :], top8[:, 0:1], -1.0, None, ALU.mult)
            P.act(ee[:, :], lg, AF.Exp, bias=nmx[:, :])
            P.ts("dve", mk[:, :], lg, top8[:, 3:4], None, ALU.is_ge)
            P.tt("dve", ee[:, :], ee[:, :], mk[:, :], ALU.mult)
            P.red(ssum[:, :], ee[:, :], ALU.add)
            P.recip(ssum[:, :], ssum[:, :])
            P.ts("dve", gates[:, tt_, :], ee[:, :], ssum[:, :], None, ALU.mult)
            P.tr(pb[7][0:32, 0:128], gates[:, tt_, :], C.ident[:, :])
            P.cp("act", gT[0:32, tt_ * 128:(tt_ + 1) * 128], pb[7][0:32, 0:128])
        for tt_ in range(16):
            for dh in range(2):
                ps = pb[dh]
                P.mm(ps[:, :], gT[0:32, tt_ * 128:(tt_ + 1) * 128], bdn[0:32, dh * 512:(dh + 1) * 512])
                P.tt("dve", C.yacc[tt_][:, dh * 512:(dh + 1) * 512], C.yacc[tt_][:, dh * 512:(dh + 1) * 512], ps[:, :], ALU.add)
        with Stage(P):
            wup = [P.sb(f"wup{i}", [128, 8, 1024], BF16) for i in range(2)]
            wdn = [P.sb(f"wdn{i}", [128, 4, D], BF16) for i in range(2)]
            actTs = [P.sb(f"actT{i}", [128, 4, TOWN], BF16) for i in range(2)]
            gg = [P.sb(f"gg{i}", [128, 512]) for i in range(2)]
            ll = [P.sb(f"ll{i}", [128, 512]) for i in range(2)]
            sgm = [P.sb(f"sgm{i}", [128, 512]) for i in range(2)]
            cnt_ = {"k": 0, "kd": 0}

            def emit_up(uu):
                e, hf = uu // 2, uu % 2
                wu = wup[uu % 2]; wd = wdn[uu % 2]; actT = actTs[uu % 2]
                cu = getattr(C, "wc_up", None)
                if cu is None or C.first_q:
                    P.dma("pool", wu[:, :, :], V(C.w_up, C.w_up.h[e * D:(e + 1) * D, hf * 1024:(hf + 1) * 1024].rearrange("(kt k) n -> k kt n", k=128)))
                    P.dma("pool", wd[:, :, :], V(C.w_dn, C.w_dn.h[e * D + hf * 512:e * D + (hf + 1) * 512, :].rearrange("(ft f) n -> f ft n", f=128)))
                    if cu is not None:
                        P.dma("sp", C.wc_up[uu, :, :, :], wu[:, :, :])
                        P.dma("sp", C.wc_dn[uu, :, :, :], wd[:, :, :])
                else:
                    P.dma("sp", wu[:, :, :], C.wc_up[uu, :, :, :])
                    P.dma("sp", wd[:, :, :], C.wc_dn[uu, :, :, :])
                for f4 in range(4):
                    ft = hf * 4 + f4
                    for tg in range(4):
                        ts_ = slice(tg * 512, (tg + 1) * 512)
                        k = cnt_["k"]; cnt_["k"] += 1
                        pg = pb[(2 * k) % 6]; pl = pb[(2 * k) % 6 + 1]
                        g = gg[k % 2]; l = ll[k % 2]; s = sgm[k % 2]
                        for kt in range(8):
                            P.mm(pg[:, :], wu[:, kt, f4 * 256:f4 * 256 + 256:2], C.h1T[:, kt, ts_], start=(kt == 0), stop=(kt == 7))
                        for kt in range(8):
                            P.mm(pl[:, :], wu[:, kt, f4 * 256 + 1:f4 * 256 + 256:2], C.h1T[:, kt, ts_], start=(kt == 0), stop=(kt == 7))
                        P.ts("dve", g[:, :], pg[:, :], bup[:, e, ft, 0:1], 7.0, ALU.add, ALU.min)
                        P.act(s[:, :], g[:, :], AF.Sigmoid, scale=1.702)
                        P.ts("dve", l[:, :], pl[:, :], bup[:, e, ft, 1:2], 7.0, ALU.add, ALU.min)
                        P.ts("dve", l[:, :], l[:, :], -7.0, 1.0, ALU.max, ALU.add)
                        P.tt("pool", g[:, :], g[:, :], s[:, :], ALU.mult)
                        P.tt("dve", actT[:, f4, ts_], g[:, :], l[:, :], ALU.mult)

            def emit_down(uu):
                e = uu // 2
                wd = wdn[uu % 2]; actT = actTs[uu % 2]
                for tt_ in range(16):
                    tsl = slice(tt_ * 128, (tt_ + 1) * 128)
                    for dh in range(2):
                        kd = cnt_["kd"]; cnt_["kd"] += 1
                        pd = pb[6 + kd % 2]
                        for f4 in range(4):
                            P.mm(pd[:, :], actT[:, f4, tsl], wd[:, f4, dh * 512:(dh + 1) * 512], start=(f4 == 0), stop=(f4 == 3))
                        P.stt(C.yacc[tt_][:, dh * 512:(dh + 1) * 512], pd[:, :], gates[:, tt_, e:e + 1],
                              C.yacc[tt_][:, dh * 512:(dh + 1) * 512], ALU.mult, ALU.add)

            emit_up(0)
            for uu in range(64):
                if uu + 1 < 64:
                    emit_up(uu + 1)
                emit_down(uu)
        g2 = P.sb("g2", [128, D]); b2 = P.sb("b2", [128, D])
        P.dma("sp", g2[:, :], C.ln2g[:, :]); P.dma("sp", b2[:, :], C.ln2b[:, :])
        junk = P.sb("ojunk", [128, D])
        oo = [P.sb(f"oo{i}", [128, D]) for i in range(2)]
        for tt_ in range(16):
            o = oo[tt_ % 2]
            ln_tok(P, C.yacc[tt_][:, :], o[:, :], g2[:, :], b2[:, :], S, junk[:, :])
            P.dma("sp", C.out[C.off + tt_ * 128:C.off + (tt_ + 1) * 128, :], o[:, :])
            if getattr(C, "hT_next", None) is not None:
                emit_hT(P, C, o, C.hT_next, C.off + tt_ * 128, tt_)


STAGES_ALL = ("window", "ssm", "conv", "mem", "attproj", "att", "merge", "moe")
NQ = 4


def build_layer(stages=STAGES_ALL, dbg=(), quarters=(0, 1, 2, 3)):
    nc = bass.Bass("TRN2", target_bir_lowering=False)
    es = ExitStack()
    P = Prog(nc, es)
    C = Ctx()

    def inp(name, shape, dt=F32):
        t = P.dram(name, shape, dt, kind="ExternalInput")
        setattr(C, name, t)
        return t

    inp("hT", [D, PRE + T]); inp("hown", [T, D]); inp("npad", [4, 128, 1]); inp("padcol", [4, 128, 64])
    inp("memT", [D, 256]); inp("identin", [128, 128]); inp("tri", [128, 128])
    inp("w_in", [D, D_IN]); inp("kvg", [128, 1]); inp("kvgrow", [128, 128])
    inp("w_uk", [128, 512]); inp("w_uv", [128, 512]); inp("convw", [128, 4, 3]); inp("convb", [128, 4])
    inp("lamre", [128, 16]); inp("lamim", [128, 16]); inp("logdt", [128, 16])
    inp("bre", [128, 16, 16]); inp("bim", [128, 16, 16]); inp("cre", [128, 16, 16]); inp("cim", [128, 16, 16])
    inp("dskip", [128, 4]); inp("w_glu", [512, 512]); inp("bglu", [128, 4])
    inp("w_mem", [D, D]); inp("w_br", [4, 512, D]); inp("w_o", [D, D])
    inp("ln1g", [128, D]); inp("ln1b", [128, D]); inp("ln2g", [128, D]); inp("ln2b", [128, D])
    inp("w_r", [D, 32]); inp("b_r", [128, 32])
    inp("w_up", [32 * D, 2048]); inp("b_up", [128, 32, 8, 2]); inp("w_dn", [32 * D, D]); inp("b_dn", [32, D])
    C.out = P.dram("out", [T, D], F32, kind="ExternalOutput")

    def scratch(name, shape, dt):
        kind = "ExternalOutput" if name in dbg else "Internal"
        t = P.dram(name, shape, dt, kind=kind)
        setattr(C, name, t)
        return t

    scratch("uT", [512, WIN], F32); scratch("z32", [512, TOWN], F32); scratch("brT", [4, 512, TOWN], BF16)
    scratch("mT_d", [D, TOWN], BF16); scratch("qlat_d", [128, 8, TOWN], BF16); scratch("qidx_d", [32, 8, TOWN], BF16)

    C.pb = [P.ps(f"pb{i}", [128, 512], F32) for i in range(8)]
    C.ident = P.sb("ident", [128, 128]); P.dma("sp", C.ident[:, :], C.identin[:, :])
    C.ones_bf = P.sb("ones_bf", [128, 128], BF16); P.memset("dve", C.ones_bf[:, :], 1.0)

    for j in quarters:
        C.j = j
        C.off = j * TOWN
        C.pad = PRE - j * TOWN
        C.first_q = True
        with Stage(P):
            C.ckvT = P.sb(f"ckvT{j}", [128, WIN], BF16)
            C.ckv_tok = P.sb(f"ckv_tok{j}", [128, 64, 129], BF16)
            C.kidxT = P.sb(f"kidxT{j}", [32, WIN], BF16)
            C.widx = P.sb(f"widx{j}", [128, 16, 8])
            P.memset("dve", C.ckv_tok[:, :, 128:129], 1.0)
            if "window" in stages:
                stage_window(P, C)
            if "ssm" in stages:
                stage_ssm(P, C)
            with Stage(P):
                C.hTo = P.sb(f"hTo{j}", [128, 8, TOWN], BF16)
                P.dma("pool", C.hTo[:, :, :], V(C.hT, C.hT.h[:, C.off + PRE:C.off + WIN].rearrange("(kt k) t -> k kt t", k=128)))
                if "conv" in stages:
                    stage_conv(P, C)
                if "mem" in stages:
                    stage_mem(P, C)
                if "attproj" in stages:
                    stage_attproj(P, C)
            if "att" in stages:
                stage_att(P, C)
        with Stage(P):
            C.hTo = P.sb(f"hTo2{j}", [128, 8, TOWN], BF16)
            P.dma("pool", C.hTo[:, :, :], V(C.hT, C.hT.h[:, C.off + PRE:C.off + WIN].rearrange("(kt k) t -> k kt t", k=128)))
            if "merge" in stages or "merge_a" in stages:
                stage_merge_a(P, C)
        with Stage(P):
            C.yacc = [P.sb(f"yacc{j}_{i}", [128, D]) for i in range(16)]
            C.h1T = P.sb(f"h1T{j}", [128, 8, TOWN], BF16)
            C.rlog = P.sb(f"rlog{j}", [128, 16, 32])
            if "merge" in stages or "merge_b" in stages:
                stage_merge_b(P, C)
            if "moe" in stages:
                stage_moe(P, C)
    P.finish("sp", [C.out] + [getattr(C, n) for n in dbg])
    print("layer program instructions:", P.ninst, flush=True)
    es.close()
    return nc


def _rep(v, rows=128):
    return np.ascontiguousarray(np.broadcast_to(np.asarray(v, np.float32).reshape(1, -1), (rows, np.size(v))))


def _sm(a):
    a = np.asarray(a, np.float32)
    rest = a.shape[2:]
    return np.ascontiguousarray(a.reshape((16, 128) + rest).swapaxes(0, 1))


def layer_weights(inp, l):
    f = lambda k: np.asarray(inp[k][l], np.float32)
    w = {}
    w["w_in"] = np.ascontiguousarray(f("w_in"))
    w["kvg"] = np.ascontiguousarray(f("kv_norm_g").reshape(128, 1))
    w["kvgrow"] = _rep(f("kv_norm_g"))
    w["w_uk"] = np.ascontiguousarray(f("w_uk").reshape(128, 512))
    w["w_uv"] = np.ascontiguousarray(f("w_uv").reshape(128, 512))
    w["convw"] = np.ascontiguousarray(f("conv_w").T.reshape(4, 128, 3).transpose(1, 0, 2))
    w["convb"] = np.ascontiguousarray(f("conv_b").reshape(4, 128).T)
    w["lamre"] = _sm(f("lam_re")); w["lamim"] = _sm(f("lam_im"))
    w["logdt"] = _sm(np.broadcast_to(f("log_dt")[:, None], (32, 64)))
    w["bre"] = _sm(f("b_re")); w["bim"] = _sm(f("b_im"))
    w["cre"] = _sm(f("c_re").transpose(0, 2, 1)); w["cim"] = _sm(f("c_im").transpose(0, 2, 1))
    w["dskip"] = np.ascontiguousarray(f("d_skip").reshape(4, 128).T)
    w["w_glu"] = np.ascontiguousarray(f("w_glu"))
    w["bglu"] = np.ascontiguousarray(f("b_glu").reshape(4, 128).T)
    w["w_mem"] = np.ascontiguousarray(f("w_mem_kv"))
    w["w_br"] = np.ascontiguousarray(f("w_branch"))
    w["w_o"] = np.ascontiguousarray(f("w_o"))
    w["ln1g"] = _rep(f("ln1_g")); w["ln1b"] = _rep(f("ln1_b"))
    w["ln2g"] = _rep(f("ln2_g")); w["ln2b"] = _rep(f("ln2_b"))
    w["w_r"] = np.ascontiguousarray(f("w_router"))
    w["b_r"] = _rep(f("b_router"))
    w["w_up"] = np.ascontiguousarray(f("w_up").reshape(32 * D, 2048))
    w["b_up"] = np.ascontiguousarray(f("b_up").reshape(32, 8, 128, 2).transpose(2, 0, 1, 3))
    w["w_dn"] = np.ascontiguousarray(f("w_down").reshape(32 * D, D))
    w["b_dn"] = np.ascontiguousarray(f("b_down"))
    return w


def batch_inputs(h, mem, b):
    d = {}
    win = np.zeros((PRE + T, D), np.float32)
    win[PRE:] = h[b]
    d["hT"] = np.ascontiguousarray(win.T)
    d["hown"] = np.ascontiguousarray(h[b])
    npad = np.zeros((4, 128, 1), np.float32); padcol = np.zeros((4, 128, 64), np.float32)
    kidx = (np.arange(64)[None, :] * 128 + np.arange(128)[:, None])
    for j in range(4):
        pad = PRE - j * TOWN
        npad[j] = float(pad)
        padcol[j] = np.where(kidx < pad, NEG, 0.0)
    d["npad"] = npad; d["padcol"] = padcol
    d["memT"] = np.ascontiguousarray(np.asarray(mem[b], np.float32).T)
    d["identin"] = np.eye(128, dtype=np.float32)
    d["tri"] = np.where(np.arange(128)[None, :] > np.arange(128)[:, None], -BIG, 0.0).astype(np.float32)
    return d


def core_inputs(h, mem, c):
    b, j = c // 4, c % 4
    t0 = j * TOWN
    pad = PRE - t0
    win = np.zeros((WIN, D), np.float32)
    win[pad:] = h[b, 0:t0 + TOWN]
    d = {}
    d["hT"] = np.ascontiguousarray(win.T)
    d["hown"] = np.ascontiguousarray(h[b, t0:t0 + TOWN])
    d["npad"] = np.full((128, 1), float(pad), np.float32)
    kidx = (np.arange(64)[None, :] * 128 + np.arange(128)[:, None])
    d["padcol"] = np.where(kidx < pad, NEG, 0.0).astype(np.float32)
    d["memT"] = np.ascontiguousarray(np.asarray(mem[b], np.float32).T)
    d["identin"] = np.eye(128, dtype=np.float32)
    d["tri"] = np.where(np.arange(128)[None, :] > np.arange(128)[:, None], -BIG, 0.0).astype(np.float32)
    return d


_NC_CACHE = {}

W_NAMES = ("w_in", "kvg", "kvgrow", "w_uk", "w_uv", "convw", "convb", "lamre", "lamim", "logdt", "bre", "bim",
           "cre", "cim", "dskip", "w_glu", "bglu", "w_mem", "w_br", "w_o", "ln1g", "ln1b", "ln2g", "ln2b",
           "w_r", "b_r", "w_up", "b_up", "w_dn", "b_dn")
W_SHAPES = {"w_in": [D, D_IN], "kvg": [128, 1], "kvgrow": [128, 128], "w_uk": [128, 512], "w_uv": [128, 512],
            "convw": [128, 4, 3], "convb": [128, 4], "lamre": [128, 16], "lamim": [128, 16], "logdt": [128, 16],
            "bre": [128, 16, 16], "bim": [128, 16, 16], "cre": [128, 16, 16], "cim": [128, 16, 16],
            "dskip": [128, 4], "w_glu": [512, 512], "bglu": [128, 4], "w_mem": [D, D], "w_br": [4, 512, D],
            "w_o": [D, D], "ln1g": [128, D], "ln1b": [128, D], "ln2g": [128, D], "ln2b": [128, D],
            "w_r": [D, 32], "b_r": [128, 32], "w_up": [32 * D, 2048], "b_up": [128, 32, 8, 2],
            "w_dn": [32 * D, D], "b_dn": [32, D]}


def build_fused(depth=DEPTH, quarters=(0, 1, 2, 3)):
    nc = bass.Bass("TRN2", target_bir_lowering=False)
    es = ExitStack()
    P = Prog(nc, es)
    C = Ctx()
    x = P.dram("x", [T, D], F32, kind="ExternalInput")
    lng = P.dram("lng", [128, D], F32, kind="ExternalInput"); lnb = P.dram("lnb", [128, D], F32, kind="ExternalInput")
    C.npad = P.dram("npad", [4, 128, 1], F32, kind="ExternalInput")
    C.padcol = P.dram("padcol", [4, 128, 64], F32, kind="ExternalInput")
    C.memT = P.dram("memT", [D, 256], F32, kind="ExternalInput")
    C.identin = P.dram("identin", [128, 128], F32, kind="ExternalInput")
    C.tri = P.dram("tri", [128, 128], F32, kind="ExternalInput")
    WL = [{n: P.dram(f"{n}_{l}", W_SHAPES[n], F32, kind="ExternalInput") for n in W_NAMES} for l in range(depth)]
    out = P.dram("out", [T, D], F32, kind="ExternalOutput")
    hTb = [P.dram(f"hTbuf{i}", [D, PRE + T], F32) for i in range(2)]
    hb = [P.dram(f"hbuf{i}", [T, D], F32) for i in range(2)]
    for n, shp, dt in (("uT", [512, WIN], F32), ("z32", [512, TOWN], F32), ("brT", [4, 512, TOWN], BF16),
                       ("mT_d", [D, TOWN], BF16), ("qlat_d", [128, 8, TOWN], BF16), ("qidx_d", [32, 8, TOWN], BF16)):
        setattr(C, n, P.dram(n, shp, dt))
    C.wc_up = P.dram("wc_up", [64, 128, 8, 1024], BF16)
    C.wc_dn = P.dram("wc_dn", [64, 128, 4, D], BF16)
    C.pb = [P.ps(f"pb{i}", [128, 512], F32) for i in range(8)]
    C.ident = P.sb("ident", [128, 128]); P.dma("sp", C.ident[:, :], C.identin[:, :])
    C.ones_bf = P.sb("ones_bf", [128, 128], BF16); P.memset("dve", C.ones_bf[:, :], 1.0)
    with Stage(P):
        zt = P.sb("zt", [128, 2048]); P.memset("dve", zt[:, :], 0.0)
        for i in range(2):
            for r in range(8):
                for c in range(PRE // 2048):
                    P.dma("sp", hTb[i][r * 128:(r + 1) * 128, c * 2048:(c + 1) * 2048], zt[:, :])
        gs = P.sb("gs", [128, D]); bs = P.sb("bs", [128, D])
        P.dma("sp", gs[:, :], lng[:, :]); P.dma("sp", bs[:, :], lnb[:, :])
        S = Small(P, "lnin")
        junk = P.sb("junk", [128, D])
        xs = [P.sb(f"x{i}", [128, D]) for i in range(2)]
        os_ = [P.sb(f"o{i}", [128, D]) for i in range(2)]
        for i in range(T // 128):
            xt = xs[i % 2]; ot = os_[i % 2]
            P.dma("sp", xt[:, :], x[i * 128:(i + 1) * 128, :])
            ln_tok(P, xt[:, :], ot[:, :], gs[:, :], bs[:, :], S, junk[:, :])
            P.dma("sp", hb[0][i * 128:(i + 1) * 128, :], ot[:, :])
            emit_hT(P, C, ot, hTb[0], i * 128, i)
    for l in range(depth):
        for n in W_NAMES:
            setattr(C, n, WL[l][n])
        C.hT = hTb[l % 2]; C.hown = hb[l % 2]
        last = (l == depth - 1)
        C.out = out if last else hb[(l + 1) % 2]
        C.hT_next = None if last else hTb[(l + 1) % 2]
        for j in quarters:
            C.j = j
            C.off = j * TOWN
            C.pad = PRE - j * TOWN
            C.first_q = (j == quarters[0])
            with Stage(P):
                C.ckvT = P.sb("ckvT", [128, WIN], BF16)
                C.ckv_tok = P.sb("ckv_tok", [128, 64, 129], BF16)
                C.kidxT = P.sb("kidxT", [32, WIN], BF16)
                C.widx = P.sb("widx", [128, 16, 8])
                P.memset("dve", C.ckv_tok[:, :, 128:129], 1.0)
                stage_window(P, C)
                stage_ssm(P, C)
                with Stage(P):
                    C.hTo = P.sb("hTo", [128, 8, TOWN], BF16)
                    P.dma("pool", C.hTo[:, :, :], V(C.hT, C.hT.h[:, C.off + PRE:C.off + WIN].rearrange("(kt k) t -> k kt t", k=128)))
                    stage_conv(P, C)
                    stage_mem(P, C)
                    stage_attproj(P, C)
                stage_att(P, C)
            with Stage(P):
                C.hTo = P.sb("hTo2", [128, 8, TOWN], BF16)
                P.dma("pool", C.hTo[:, :, :], V(C.hT, C.hT.h[:, C.off + PRE:C.off + WIN].rearrange("(kt k) t -> k kt t", k=128)))
                stage_merge_a(P, C)
            with Stage(P):
                C.yacc = [P.sb(f"yacc{i}", [128, D]) for i in range(16)]
                C.h1T = P.sb("h1T", [128, 8, TOWN], BF16)
                C.rlog = P.sb("rlog", [128, 16, 32])
                stage_merge_b(P, C)
                stage_moe(P, C)
    P.finish("sp", [out])
    print("fused program instructions:", P.ninst, flush=True)
    es.close()
    return nc


def fused_inputs(inputs, b):
    x = np.asarray(inputs["x"], np.float32)
    d = {"x": np.ascontiguousarray(x[b]), "lng": _rep(inputs["ln_in_g"]), "lnb": _rep(inputs["ln_in_b"])}
    npad = np.zeros((4, 128, 1), np.float32); padcol = np.zeros((4, 128, 64), np.float32)
    kidx = (np.arange(64)[None, :] * 128 + np.arange(128)[:, None])
    for j in range(4):
        pad = PRE - j * TOWN
        npad[j] = float(pad)
        padcol[j] = np.where(kidx < pad, NEG, 0.0)
    d["npad"] = npad; d["padcol"] = padcol
    d["memT"] = np.ascontiguousarray(np.asarray(inputs["mem"], np.float32)[b].T)
    d["identin"] = np.eye(128, dtype=np.float32)
    d["tri"] = np.where(np.arange(128)[None, :] > np.arange(128)[:, None], -BIG, 0.0).astype(np.float32)
    return d


def kernel(**inputs):
    if "fused" not in _NC_CACHE:
        _NC_CACHE["fused"] = build_fused()
    maps = [fused_inputs(inputs, b) for b in range(2)]
    for l in range(DEPTH):
        w = layer_weights(inputs, l)
        for n in W_NAMES:
            for m in maps:
                m[f"{n}_{l}"] = w[n]
    res = run_bass_kernel_spmd(_NC_CACHE["fused"], maps, core_ids=[0, 1])
    return np.stack([np.asarray(r["out"]) for r in res.results]).astype(np.float32)
```
